# Optimizing a Trainium2 kernel written in Bass

```python
import math
import jax, jax.numpy as jnp
from jax import lax
import numpy as np

D_MODEL = 1024
BATCH = 8
SEQ = 2048
DEPTH = 4

CTX_LEN = 256
GRID_W = 64
HEAD_DIM = 64
BRANCH_WIDTH = D_MODEL // 2
N_BRANCH = 3
SSM_WIDTH = BRANCH_WIDTH
SSM_GROUP = 16
SSM_GROUPS = SSM_WIDTH // SSM_GROUP
SSM_STATE = 64
DT_MIN = 1e-3
DT_MAX = 1e-1
WIN_HEADS = BRANCH_WIDTH // HEAD_DIM
WIN_KV_HEADS = 2
WINDOW = 128
WIN_BLOCK = 128
NA_HEADS = BRANCH_WIDTH // HEAD_DIM
NA_KH = 8
NA_KW = 16
WIN_Q = WIN_HEADS * HEAD_DIM
WIN_KV = WIN_KV_HEADS * HEAD_DIM
NA_W = NA_HEADS * HEAD_DIM
IN_SPLITS = (SSM_WIDTH, WIN_Q, WIN_KV, WIN_KV, NA_W, NA_W, NA_W, N_BRANCH * D_MODEL)
IN_WIDTH = sum(IN_SPLITS)
N_EXPERTS = 32
TOP_K = 4
D_FF = D_MODEL
SWIGLU_ALPHA = 1.702
SWIGLU_LIMIT = 7.0
MOE_BLOCK = 128
ROPE_THETA = 10000.0
EPS = 1e-6
NEG_INF = -1e30

kernel_name = 'hybrid_ssm_window_natten_moe_dit'


def rms_norm(t, g):
    tf = t.astype(jnp.float32)
    y = tf * lax.rsqrt(jnp.mean(tf * tf, axis=-1, keepdims=True) + EPS) * g.astype(jnp.float32)
    return y.astype(t.dtype)


def rope_2d(t, row_pos, col_pos):
    half = t.shape[-1] // 2
    quarter = half // 2
    inv_freq = ROPE_THETA ** (-jnp.arange(quarter, dtype=jnp.float32) / quarter)

    def rotate(th, pos):
        ang = pos.astype(jnp.float32)[:, None] * inv_freq[None, :]
        cos = jnp.cos(ang)[None, :, None, :]
        sin = jnp.sin(ang)[None, :, None, :]
        t1 = th[..., :quarter].astype(jnp.float32)
        t2 = th[..., quarter:].astype(jnp.float32)
        return jnp.concatenate([t1 * cos - t2 * sin, t1 * sin + t2 * cos], axis=-1)

    out = jnp.concatenate([rotate(t[..., :half], row_pos), rotate(t[..., half:], col_pos)], axis=-1)
    return out.astype(t.dtype)


def complex_linear_combine(e1, e2):
    a1r, a1i, b1r, b1i = e1
    a2r, a2i, b2r, b2i = e2
    return (a2r * a1r - a2i * a1i,
            a2r * a1i + a2i * a1r,
            a2r * b1r - a2i * b1i + b2r,
            a2r * b1i + a2i * b1r + b2i)


def s5_mixer(u_l, u_c, need_ctx, lam_re, lam_im, log_dt, b_re, b_im, c_re, c_im, d_skip, w_glu):
    f32 = jnp.float32
    Bsz, L, P = u_l.shape
    C = u_c.shape[1]
    T = C + L
    lr = lam_re.astype(f32)
    li = lam_im.astype(f32)
    dt = jnp.exp(log_dt.astype(f32))[..., None]
    mag = jnp.exp(lr * dt)
    ar = mag * jnp.cos(li * dt)
    ai = mag * jnp.sin(li * dt)
    den = lr * lr + li * li
    fr = ((ar - 1.0) * lr + ai * li) / den
    fi = (ai * lr - (ar - 1.0) * li) / den
    br = b_re.astype(f32)
    bi = b_im.astype(f32)
    bbr = fr[..., None] * br - fi[..., None] * bi
    bbi = fr[..., None] * bi + fi[..., None] * br

    def scan_direction(dirn, seq):
        ug = seq.astype(f32).reshape(Bsz, T, SSM_GROUPS, SSM_GROUP)
        xr = jnp.einsum('btgp,gnp->tbgn', ug, bbr[dirn])
        xi = jnp.einsum('btgp,gnp->tbgn', ug, bbi[dirn])
        a_r = jnp.broadcast_to(ar[dirn][None, None], (T, 1, SSM_GROUPS, SSM_STATE))
        a_i = jnp.broadcast_to(ai[dirn][None, None], (T, 1, SSM_GROUPS, SSM_STATE))
        _, _, hr, hi = lax.associative_scan(complex_linear_combine, (a_r, a_i, xr, xi), axis=0)
        y = (jnp.einsum('tbgn,gpn->btgp', hr, c_re[dirn].astype(f32))
             - jnp.einsum('tbgn,gpn->btgp', hi, c_im[dirn].astype(f32)))
        return y.reshape(Bsz, T, P)

    y_f = scan_direction(0, jnp.concatenate([u_c, u_l], axis=1))
    y_b = scan_direction(1, jnp.concatenate([jnp.flip(u_c, 1), jnp.flip(u_l, 1)], axis=1))
    y_b_c = jnp.flip(y_b[:, :C], 1)
    y_b_l = jnp.flip(y_b[:, C:], 1)

    def readout(yf, yb, u):
        y = yf + yb + d_skip.astype(f32) * u.astype(f32)
        g = jax.nn.gelu(y).astype(u.dtype)
        return g * jax.nn.sigmoid(g @ w_glu)

    y_l = readout(y_f[:, C:], y_b_l, u_l)
    y_c = readout(y_f[:, :C], y_b_c, u_c) if need_ctx else None
    return y_l, y_c


def context_attention(q, k, v, sink):
    Bsz, C, H, d = q.shape
    hkv = k.shape[2]
    grp = H // hkv
    qg = q.reshape(Bsz, C, hkv, grp, d)
    s = jnp.einsum('bqhgd,bkhd->bhgqk', qg, k, preferred_element_type=jnp.float32) * (d ** -0.5)
    if sink is not None:
        s_sink = jnp.broadcast_to(sink.astype(jnp.float32).reshape(1, hkv, grp, 1, 1), s.shape[:-1] + (1,))
        s = jnp.concatenate([s, s_sink], axis=-1)
    p = jax.nn.softmax(s, axis=-1)[..., :C].astype(v.dtype)
    o = jnp.einsum('bhgqk,bkhd->bqhgd', p, v)
    return o.reshape(Bsz, C, H * d)


def window_attention(q, k, v, k_ctx, v_ctx, sink):
    Bsz, L, H, d = q.shape
    hkv = k.shape[2]
    grp = H // hkv
    nb = L // WIN_BLOCK
    C = k_ctx.shape[1]
    nk = 3 * WIN_BLOCK
    qb = q.reshape(Bsz, nb, WIN_BLOCK, hkv, grp, d)

    def band(t):
        tp = jnp.pad(t, ((0, 0), (WIN_BLOCK, WIN_BLOCK), (0, 0), (0, 0))).reshape(Bsz, nb + 2, WIN_BLOCK, hkv, d)
        return jnp.concatenate([tp[:, :-2], tp[:, 1:-1], tp[:, 2:]], axis=2)

    kb, vb = band(k), band(v)
    qpos = jnp.arange(L).reshape(nb, WIN_BLOCK)
    kpos = jnp.arange(nb)[:, None] * WIN_BLOCK - WIN_BLOCK + jnp.arange(nk)[None, :]
    valid = ((kpos[:, None, :] >= 0) & (kpos[:, None, :] < L)
             & (jnp.abs(qpos[:, :, None] - kpos[:, None, :]) <= WINDOW))
    scale = d ** -0.5
    s_loc = jnp.einsum('bnqhgd,bnkhd->bnhgqk', qb, kb, preferred_element_type=jnp.float32) * scale
    s_loc = jnp.where(valid[None, :, None, None], s_loc, NEG_INF)
    s_ctx = jnp.einsum('bnqhgd,bchd->bnhgqc', qb, k_ctx, preferred_element_type=jnp.float32) * scale
    s_sink = jnp.broadcast_to(sink.astype(jnp.float32).reshape(1, 1, hkv, grp, 1, 1), s_loc.shape[:-1] + (1,))
    p = jax.nn.softmax(jnp.concatenate([s_loc, s_ctx, s_sink], axis=-1), axis=-1).astype(v.dtype)
    o = (jnp.einsum('bnhgqk,bnkhd->bnqhgd', p[..., :nk], vb)
         + jnp.einsum('bnhgqc,bchd->bnqhgd', p[..., nk:nk + C], v_ctx))
    return o.reshape(Bsz, L, H * d)


def neighbourhood_attention(q, k, v, k_ctx, v_ctx, rpb):
    Bsz, L, H, d = q.shape
    rows = L // GRID_W
    kh = min(NA_KH, rows)
    C = k_ctx.shape[1]
    scale = d ** -0.5

    def grid(t):
        return t.reshape(Bsz, rows, GRID_W, H, d)

    qg, kg, vg = grid(q), grid(k), grid(v)
    r = jnp.arange(rows)
    row_idx = jnp.clip(r - kh // 2, 0, rows - kh)[:, None] + jnp.arange(kh)[None, :]
    k_rows = kg[:, row_idx]
    v_rows = vg[:, row_idx]
    col = jnp.arange(GRID_W)
    col_start = jnp.clip(col - NA_KW // 2, 0, GRID_W - NA_KW)
    col_in = (col[None, :] >= col_start[:, None]) & (col[None, :] < col_start[:, None] + NA_KW)
    d_row = row_idx - r[:, None] + (NA_KH - 1)
    d_col = jnp.clip(col[None, :] - col[:, None], 1 - NA_KW, NA_KW - 1) + (NA_KW - 1)
    bias = rpb.astype(jnp.float32)[:, d_row[:, None, :, None], d_col[None, :, None, :]]
    s = jnp.einsum('brchd,brkwhd->bhrckw', qg, k_rows, preferred_element_type=jnp.float32) * scale + bias[None]
    s = jnp.where(col_in[:, None, :], s, NEG_INF)
    nloc = kh * GRID_W
    s = s.reshape(Bsz, H, rows, GRID_W, nloc)
    s_ctx = jnp.einsum('brchd,bshd->bhrcs', qg, k_ctx, preferred_element_type=jnp.float32) * scale
    p = jax.nn.softmax(jnp.concatenate([s, s_ctx], axis=-1), axis=-1).astype(v.dtype)
    p_loc = p[..., :nloc].reshape(Bsz, H, rows, GRID_W, kh, GRID_W)
    o = (jnp.einsum('bhrckw,brkwhd->brchd', p_loc, v_rows)
         + jnp.einsum('bhrcs,bshd->brchd', p[..., nloc:nloc + C], v_ctx))
    return o.reshape(Bsz, L, H * d)


def token_mixing(h_l, h_c, row_pos, col_pos, need_ctx, w_in, lam_re, lam_im, log_dt, b_re, b_im,
                 c_re, c_im, d_skip, w_glu, win_qn, win_kn, win_sink, na_qn, na_kn, na_rpb, w_branch, w_out):
    split_pts = [int(s) for s in np.cumsum(IN_SPLITS)[:-1]]
    a_l, wq_l, wk_l, wv_l, nq_l, nk_l, nv_l, gate_l = jnp.split(h_l @ w_in, split_pts, axis=-1)
    a_c, wq_c, wk_c, wv_c, nq_c, nk_c, nv_c, gate_c = jnp.split(h_c @ w_in, split_pts, axis=-1)

    def heads(t, n):
        return t.reshape(t.shape[0], t.shape[1], n, HEAD_DIM)

    y_a_l, y_a_c = s5_mixer(a_l, a_c, need_ctx, lam_re, lam_im, log_dt, b_re, b_im, c_re, c_im, d_skip, w_glu)

    q_b = rope_2d(rms_norm(heads(wq_l, WIN_HEADS), win_qn), row_pos, col_pos)
    k_b = rope_2d(rms_norm(heads(wk_l, WIN_KV_HEADS), win_kn), row_pos, col_pos)
    v_b = heads(wv_l, WIN_KV_HEADS)
    k_bc = rms_norm(heads(wk_c, WIN_KV_HEADS), win_kn)
    v_bc = heads(wv_c, WIN_KV_HEADS)
    y_b_l = window_attention(q_b, k_b, v_b, k_bc, v_bc, win_sink)

    q_n = rms_norm(heads(nq_l, NA_HEADS), na_qn)
    k_n = rms_norm(heads(nk_l, NA_HEADS), na_kn)
    v_n = heads(nv_l, NA_HEADS)
    k_nc = rms_norm(heads(nk_c, NA_HEADS), na_kn)
    v_nc = heads(nv_c, NA_HEADS)
    y_n_l = neighbourhood_attention(q_n, k_n, v_n, k_nc, v_nc, na_rpb)

    def merge(ys, gates):
        stacked = jnp.stack(ys, axis=2)
        proj = jnp.einsum('btip,ipd->btid', stacked, w_branch)
        g = jax.nn.sigmoid(gates.reshape(gates.shape[0], gates.shape[1], N_BRANCH, D_MODEL))
        return jnp.sum(g * proj, axis=2) @ w_out

    m_l = merge([y_a_l, y_b_l, y_n_l], gate_l)
    if not need_ctx:
        return m_l, None
    y_b_c = context_attention(rms_norm(heads(wq_c, WIN_HEADS), win_qn), k_bc, v_bc, win_sink)
    y_n_c = context_attention(rms_norm(heads(nq_c, NA_HEADS), na_qn), k_nc, v_nc, None)
    m_c = merge([y_a_c, y_b_c, y_n_c], gate_c)
    return m_l, m_c


def moe_ffn(h, router_w, router_b, w_gate_up, b_gate_up, w_down, b_down):
    T, D = h.shape
    logits = (h @ router_w + router_b).astype(jnp.float32)
    top_val, top_idx = lax.top_k(logits, TOP_K)
    gates = jax.nn.softmax(top_val, axis=-1)
    TK = T * TOP_K
    flat_e = top_idx.reshape(TK)
    flat_tok = jnp.arange(TK, dtype=jnp.int32) // TOP_K
    order = jnp.argsort(flat_e)
    sorted_e = flat_e[order]
    sorted_tok = flat_tok[order]
    counts = jnp.bincount(flat_e, length=N_EXPERTS)
    padded = (counts + MOE_BLOCK - 1) // MOE_BLOCK * MOE_BLOCK
    seg_end = jnp.cumsum(padded)
    pad_start = seg_end - padded
    start = jnp.cumsum(counts) - counts
    dest = pad_start[sorted_e] + jnp.arange(TK, dtype=jnp.int32) - start[sorted_e]
    n_blocks = -(-TK // MOE_BLOCK) + N_EXPERTS
    buf_tok = jnp.full((n_blocks * MOE_BLOCK,), T, jnp.int32).at[dest].set(sorted_tok)
    block_expert = jnp.minimum(jnp.searchsorted(seg_end, jnp.arange(n_blocks) * MOE_BLOCK, side='right'),
                               N_EXPERTS - 1)
    h_pad = jnp.concatenate([h, jnp.zeros((1, D), h.dtype)], axis=0)
    xb = h_pad[buf_tok].reshape(n_blocks, MOE_BLOCK, D)

    def expert_block(args):
        xblk, e = args
        gu = xblk @ w_gate_up[e] + b_gate_up[e]
        g = jnp.minimum(gu[:, :D_FF], SWIGLU_LIMIT)
        u = jnp.clip(gu[:, D_FF:], -SWIGLU_LIMIT, SWIGLU_LIMIT)
        act = (u + 1.0) * (g * jax.nn.sigmoid(SWIGLU_ALPHA * g))
        return act @ w_down[e] + b_down[e]

    yb = lax.map(expert_block, (xb, block_expert)).reshape(n_blocks * MOE_BLOCK, D)
    y = yb[dest] * gates.reshape(TK)[order][:, None].astype(h.dtype)
    return jax.ops.segment_sum(y, sorted_tok, num_segments=T)


def setup_inputs(seed: int = 0) -> dict:
    key = jax.random.key(seed)
    ks = list(jax.random.split(key, 32))
    f32 = jnp.float32

    def nrm(i, shape, s):
        return jax.random.normal(ks[i], shape, f32) * s

    G, N, Pg = SSM_GROUPS, SSM_STATE, SSM_GROUP
    lam_im = jnp.broadcast_to(jnp.pi * jnp.arange(N, dtype=f32), (DEPTH, 2, G, N))
    return {
        'x': nrm(0, (BATCH, SEQ, D_MODEL), 1.0),
        'c': nrm(1, (BATCH, D_MODEL), 1.0),
        'ctx': nrm(2, (BATCH, CTX_LEN, D_MODEL), 1.0),
        'c_ctx': nrm(3, (D_MODEL,), 1.0),
        'w_mod': nrm(4, (DEPTH, D_MODEL, 6 * D_MODEL), 0.5 * D_MODEL ** -0.5),
        'b_mod': nrm(5, (DEPTH, 6 * D_MODEL), 0.02),
        'norm_mix': 1.0 + nrm(6, (DEPTH, D_MODEL), 0.05),
        'norm_ffn': 1.0 + nrm(7, (DEPTH, D_MODEL), 0.05),
        'w_in': nrm(8, (DEPTH, D_MODEL, IN_WIDTH), D_MODEL ** -0.5),
        'ssm_lam_re': -0.5 + nrm(9, (DEPTH, 2, G, N), 0.02),
        'ssm_lam_im': lam_im,
        'ssm_log_dt': jax.random.uniform(ks[10], (DEPTH, 2, G), f32, math.log(DT_MIN), math.log(DT_MAX)),
        'ssm_b_re': nrm(11, (DEPTH, 2, G, N, Pg), Pg ** -0.5),
        'ssm_b_im': nrm(12, (DEPTH, 2, G, N, Pg), Pg ** -0.5),
        'ssm_c_re': nrm(13, (DEPTH, 2, G, Pg, N), N ** -0.5),
        'ssm_c_im': nrm(14, (DEPTH, 2, G, Pg, N), N ** -0.5),
        'ssm_d': nrm(15, (DEPTH, SSM_WIDTH), 1.0),
        'ssm_w_glu': nrm(16, (DEPTH, SSM_WIDTH, SSM_WIDTH), SSM_WIDTH ** -0.5),
        'win_q_norm': 1.0 + nrm(17, (DEPTH, HEAD_DIM), 0.05),
        'win_k_norm': 1.0 + nrm(18, (DEPTH, HEAD_DIM), 0.05),
        'win_sink': nrm(19, (DEPTH, WIN_HEADS), 0.5),
        'na_q_norm': 1.0 + nrm(20, (DEPTH, HEAD_DIM), 0.05),
        'na_k_norm': 1.0 + nrm(21, (DEPTH, HEAD_DIM), 0.05),
        'na_rpb': nrm(22, (DEPTH, NA_HEADS, 2 * NA_KH - 1, 2 * NA_KW - 1), 0.5),
        'w_branch': nrm(23, (DEPTH, N_BRANCH, BRANCH_WIDTH, D_MODEL), BRANCH_WIDTH ** -0.5),
        'w_out': nrm(24, (DEPTH, D_MODEL, D_MODEL), D_MODEL ** -0.5),
        'router_w': nrm(25, (DEPTH, D_MODEL, N_EXPERTS), D_MODEL ** -0.5),
        'router_b': nrm(26, (DEPTH, N_EXPERTS), 0.01),
        'w_gate_up': nrm(27, (DEPTH, N_EXPERTS, D_MODEL, 2 * D_FF), D_MODEL ** -0.5),
        'b_gate_up': nrm(28, (DEPTH, N_EXPERTS, 2 * D_FF), 0.01),
        'w_down': nrm(29, (DEPTH, N_EXPERTS, D_FF, D_MODEL), D_FF ** -0.5),
        'b_down': nrm(30, (DEPTH, N_EXPERTS, D_MODEL), 0.01),
    }


def reference(x, c, ctx, c_ctx, w_mod, b_mod, norm_mix, norm_ffn, w_in, ssm_lam_re, ssm_lam_im, ssm_log_dt,
              ssm_b_re, ssm_b_im, ssm_c_re, ssm_c_im, ssm_d, ssm_w_glu, win_q_norm, win_k_norm, win_sink,
              na_q_norm, na_k_norm, na_rpb, w_branch, w_out, router_w, router_b, w_gate_up, b_gate_up,
              w_down, b_down):
    Bsz, L, D = x.shape
    C = ctx.shape[1]
    pos = jnp.arange(L)
    row_pos = pos // GRID_W
    col_pos = pos % GRID_W
    lat, cx = x, ctx
    for li in range(DEPTH):
        need_ctx = li < DEPTH - 1
        mod_l = jnp.split((jax.nn.silu(c) @ w_mod[li] + b_mod[li])[:, None, :], 6, axis=-1)
        mod_c = jnp.split(jax.nn.silu(c_ctx) @ w_mod[li] + b_mod[li], 6, axis=-1)
        h_l = rms_norm(lat, norm_mix[li]) * (1.0 + mod_l[1]) + mod_l[0]
        h_c = rms_norm(cx, norm_mix[li]) * (1.0 + mod_c[1]) + mod_c[0]
        m_l, m_c = token_mixing(h_l, h_c, row_pos, col_pos, need_ctx, w_in[li], ssm_lam_re[li], ssm_lam_im[li],
                                ssm_log_dt[li], ssm_b_re[li], ssm_b_im[li], ssm_c_re[li], ssm_c_im[li], ssm_d[li],
                                ssm_w_glu[li], win_q_norm[li], win_k_norm[li], win_sink[li], na_q_norm[li],
                                na_k_norm[li], na_rpb[li], w_branch[li], w_out[li])
        lat = lat + mod_l[2] * m_l
        f_l = rms_norm(lat, norm_ffn[li]) * (1.0 + mod_l[4]) + mod_l[3]
        if need_ctx:
            cx = cx + mod_c[2] * m_c
            f_c = rms_norm(cx, norm_ffn[li]) * (1.0 + mod_c[4]) + mod_c[3]
            toks = jnp.concatenate([f_l.reshape(Bsz * L, D), f_c.reshape(Bsz * C, D)], axis=0)
        else:
            toks = f_l.reshape(Bsz * L, D)
        y = moe_ffn(toks, router_w[li], router_b[li], w_gate_up[li], b_gate_up[li], w_down[li], b_down[li])
        lat = lat + mod_l[5] * y[:Bsz * L].reshape(Bsz, L, D)
        if need_ctx:
            cx = cx + mod_c[5] * y[Bsz * L:].reshape(Bsz, C, D)
    return lat
```

```python
import numpy as np
from contextlib import ExitStack
import concourse.bass as bass
import concourse.mybir as mybir
from concourse.bass_utils import run_bass_kernel_spmd

F32 = mybir.dt.float32
BF = mybir.dt.bfloat16
ALU = mybir.AluOpType
AF = mybir.ActivationFunctionType

D = 1024; L_LAT = 2048; C_CTX = 256; T = 2304; NT = 18; DEPTH = 4
NE = 32; INW = 5888
CH = [(0, 256)] + [(256 + 512 * j, 512) for j in range(4)]
EPS = 1e-6
ND = 6


def bl(ap, n):
    return bass.AP(ap.tensor, ap.offset, list(ap.ap) + [(0, n)])


def bmid(ap, n):
    a = list(ap.ap)
    return bass.AP(ap.tensor, ap.offset, [a[0], (0, n)] + a[1:])


class Sched:
    def __init__(s, nc, block, stack):
        s.nc = nc; s.block = block
        s.engs = {'pe': nc.tensor, 'act': nc.scalar, 'dve': nc.vector, 'pool': nc.gpsimd, 'sp': nc.sync}
        s.tl = {e: stack.enter_context(nc.semaphore('tl_' + e)) for e in ['pe', 'act', 'dve', 'pool']}
        s.cnt = {e: 0 for e in s.tl}
        s.dsem = {q: [stack.enter_context(nc.semaphore('d_%s%d' % (q, i))) for i in range(ND)] for q in ['sp', 'pool']}
        s.dval = {q: [0] * ND for q in s.dsem}; s.dnext = {q: 0 for q in s.dsem}
        s.waited = {e: {} for e in s.engs}
        s.lw = {}; s.rd = {}
        s.pend = {e: [] for e in s.engs}
        s.n = 0

    def _waits(s, eng, R, W):
        deps = {}
        def need(tok):
            if tok is None: return
            name, sem, val, src = tok
            if src == eng and eng in ('pe', 'sp'): return
            if deps.get(name, (None, 0))[1] < val: deps[name] = (sem, val)
        for k in R: need(s.lw.get(k))
        for k in W:
            need(s.lw.get(k))
            for t in s.rd.get(k, ()): need(t)
        out = []
        for name, (sem, val) in deps.items():
            if s.waited[eng].get(name, 0) < val:
                s.waited[eng][name] = val; out.append((sem, val))
        return out

    def _mark(s, tok, R, W):
        for k in R: s.rd.setdefault(k, []).append(tok)
        for k in W: s.lw[k] = tok; s.rd[k] = []

    def add(s, eng, fn, R=(), W=()):
        waits = s._waits(eng, R, W)
        s.cnt[eng] += 1
        tok = ('tl_' + eng, s.tl[eng], s.cnt[eng], eng)
        s.pend[eng].append((waits, fn, (s.tl[eng], 1)))
        s._mark(tok, R, W); s.n += 1

    def dma(s, q, out, in_, R=(), W=(), **kw):
        waits = s._waits(q, R, W)
        i = s.dnext[q]; s.dnext[q] = (i + 1) % ND
        sem = s.dsem[q][i]; name = 'd_%s%d' % (q, i)
        if s.dval[q][i] > s.waited[q].get(name, 0):
            s.waited[q][name] = s.dval[q][i]; waits.append((sem, s.dval[q][i]))
        s.dval[q][i] += 16
        tok = (name, sem, s.dval[q][i], 'dma')
        s.pend[q].append((waits, lambda e: e.dma_start(out=out, in_=in_, **kw), (sem, 16)))
        s._mark(tok, R, W); s.n += 1

    def barrier(s):
        for eng in s.engs:
            waits = []
            for e2 in s.tl:
                name = 'tl_' + e2
                if s.cnt[e2] > s.waited[eng].get(name, 0) and not (e2 == eng == 'pe'):
                    s.waited[eng][name] = s.cnt[e2]; waits.append((s.tl[e2], s.cnt[e2]))
            for q in s.dsem:
                for i in range(ND):
                    name = 'd_%s%d' % (q, i)
                    if s.dval[q][i] > s.waited[eng].get(name, 0):
                        s.waited[eng][name] = s.dval[q][i]; waits.append((s.dsem[q][i], s.dval[q][i]))
            if waits: s.pend[eng].append((waits, None, None))
        s.flush()

    def wait_all(s, eng, keys):
        waits = s._waits(eng, keys, ())
        s.pend[eng].append((waits, None, None))

    def flush(s):
        for ename, lst in s.pend.items():
            if not lst: continue
            def body(e, lst=lst):
                for waits, fn, inc in lst:
                    for sem, val in waits: e.wait_ge(sem, val)
                    if fn is not None:
                        ins = fn(e)
                        ins.then_inc(inc[0], inc[1])
            getattr(s.block, {'pe': 'tensor', 'act': 'scalar', 'dve': 'vector', 'pool': 'gpsimd', 'sp': 'sync'}[ename])(body)
            s.pend[ename] = []


def na_start(r): return min(max(r - 4, 0), 24)

def na_tiles(i):
    rows = set()
    for r in (2 * i, 2 * i + 1):
        st = na_start(r); rows.update(range(st, st + 8))
    return sorted(set(r // 2 for r in rows))

def na_mask(i, j):
    m = np.zeros((128, 128), np.float32)
    k = np.arange(128); rk = k // 64; ck = k % 64
    for q in range(128):
        rq = q // 64; cq = q % 64
        st = na_start(2 * i + rq)
        row_ok = (2 * j + rk >= st) & (2 * j + rk <= st + 7)
        cs = min(max(cq - 8, 0), 48)
        col_ok = (ck >= cs) & (ck < cs + 16)
        m[:, q] = (row_ok & col_ok)
    return m

_NA_MASKS = None
def na_mask_table():
    global _NA_MASKS
    if _NA_MASKS is None:
        uniq = []; idx = {}
        for i in range(16):
            for j in na_tiles(i):
                m = na_mask(i, j)
                for u, mm in enumerate(uniq):
                    if np.array_equal(mm, m): idx[(i, j)] = u; break
                else:
                    uniq.append(m); idx[(i, j)] = len(uniq) - 1
        _NA_MASKS = (np.stack(uniq), idx)
    return _NA_MASKS


def host_consts():
    bf = mybir.dt.np(BF)
    c = {}
    c['ident_f'] = np.eye(128, dtype=np.float32)
    c['ident_b'] = np.eye(128, dtype=np.float32).astype(bf)
    bo = np.zeros((128, 128), np.float32); bo[:64, :64] = 1; bo[64:, 64:] = 1
    c['blockones'] = bo.astype(bf)
    c['ones_f'] = np.ones((128, 128), np.float32)
    R = np.zeros((64, 64), np.float32)
    for base in (0, 32):
        for i in range(16):
            R[base + i, base + 16 + i] = -1.0
            R[base + 16 + i, base + i] = 1.0
    R2 = np.zeros((128, 128), np.float32); R2[:64, :64] = R; R2[64:, 64:] = R
    c['rotT'] = np.ascontiguousarray(R2.T).astype(bf)
    pos = np.arange(L_LAT); row = pos // 64; col = pos % 64
    inv = (10000.0 ** (-np.arange(16, dtype=np.float32) / 16)).astype(np.float32)
    ang = np.zeros((64, L_LAT), np.float32)
    for dd in range(64):
        p = row if dd < 32 else col
        ang[dd] = p.astype(np.float32) * inv[dd % 16]
    c['cosT'] = np.concatenate([np.cos(ang), np.cos(ang)], 0).astype(bf)
    c['sinT'] = np.concatenate([np.sin(ang), np.sin(ang)], 0).astype(bf)
    b = np.arange(128)[:, None]; a = np.arange(128)[None, :]
    c['wmask'] = np.stack([(a <= b), (b <= a)]).astype(np.float32).astype(bf)
    c['namask'] = na_mask_table()[0].astype(bf)
    mz = np.zeros((4, 128, 128), np.float32); my = np.zeros((4, 128, 128), np.float32)
    for gl in range(4):
        for g2 in range(2):
            mz[gl, 64 * g2:64 * g2 + 64, 32 * gl + 16 * g2: 32 * gl + 16 * g2 + 16] = 1
            my[gl, 32 * gl + 16 * g2: 32 * gl + 16 * g2 + 16, 64 * g2:64 * g2 + 64] = 1
    c['maskZ'] = mz; c['maskY'] = my
    return c


def host_layout(inp, b):
    o = {}
    cb = inp['c'][b].reshape(8, 128).T; cc = inp['c_ctx'].reshape(8, 128).T
    o['cT'] = np.ascontiguousarray(np.stack([cb, cc], -1))
    o['bmodT'] = np.ascontiguousarray(inp['b_mod'].reshape(DEPTH, 48, 128).transpose(0, 2, 1))
    o['gmix'] = np.ascontiguousarray(inp['norm_mix'].reshape(DEPTH, 8, 128).transpose(0, 2, 1))
    o['gffn'] = np.ascontiguousarray(inp['norm_ffn'].reshape(DEPTH, 8, 128).transpose(0, 2, 1))
    def p2(x):
        return np.ascontiguousarray(x.reshape(DEPTH, 2, 16, 2, 64).transpose(0, 3, 4, 1, 2).reshape(DEPTH, 128, 32))
    o['lamre'] = p2(inp['ssm_lam_re']); o['lamim'] = p2(inp['ssm_lam_im'])
    o['logdt'] = p2(np.broadcast_to(inp['ssm_log_dt'][..., None], (DEPTH, 2, 32, 64)))
    def pb(x):
        return np.ascontiguousarray(x.reshape(DEPTH, 2, 16, 2, 64, 16).transpose(0, 3, 4, 1, 2, 5).reshape(DEPTH, 128, 32, 16))
    o['bre'] = pb(inp['ssm_b_re']); o['bim'] = pb(inp['ssm_b_im'])
    def pc(x):
        return np.ascontiguousarray(x.reshape(DEPTH, 2, 4, 8, 16, 64).transpose(0, 3, 4, 1, 2, 5).reshape(DEPTH, 128, 8, 64))
    o['cre'] = pc(inp['ssm_c_re']); o['cim'] = pc(inp['ssm_c_im'])
    o['dskip'] = np.ascontiguousarray(inp['ssm_d'].reshape(DEPTH, 4, 128).transpose(0, 2, 1))
    def hd(x): return np.ascontiguousarray(np.tile(x, (1, 2))[:, :, None])
    o['gwq'] = hd(inp['win_q_norm']); o['gwk'] = hd(inp['win_k_norm'])
    o['gnq'] = hd(inp['na_q_norm']); o['gnk'] = hd(inp['na_k_norm'])
    o['sink'] = np.ascontiguousarray(np.broadcast_to(inp['win_sink'][:, None, :], (DEPTH, 128, 8)))
    k = np.arange(128); rk = k // 64; ck = k % 64
    rpb = inp['na_rpb']
    dc = np.clip(ck[:, None] - ck[None, :], -15, 15) + 15
    B = np.zeros((DEPTH, 7, 128, 8, 128), np.float32)
    for di, dl in enumerate(range(-3, 4)):
        dr = np.clip(2 * dl + rk[:, None] - rk[None, :] + 7, 0, 14)
        dcf = dc[ck[:, None], ck[None, :]]
        B[:, di] = rpb[:, :, dr, dcf].transpose(0, 2, 1, 3)
    o['nabias'] = B
    o['rbias'] = np.ascontiguousarray(np.broadcast_to(inp['router_b'][:, None, :], (DEPTH, 128, NE)))
    o['bguT'] = np.ascontiguousarray(inp['b_gate_up'].reshape(DEPTH, NE, 16, 128).transpose(0, 3, 1, 2))
    return o


SMALL_SHAPES = {
    'cT': [128, 8, 2], 'bmodT': [DEPTH, 128, 48], 'gmix': [DEPTH, 128, 8], 'gffn': [DEPTH, 128, 8],
    'lamre': [DEPTH, 128, 32], 'lamim': [DEPTH, 128, 32], 'logdt': [DEPTH, 128, 32],
    'bre': [DEPTH, 128, 32, 16], 'bim': [DEPTH, 128, 32, 16], 'cre': [DEPTH, 128, 8, 64], 'cim': [DEPTH, 128, 8, 64],
    'dskip': [DEPTH, 128, 4], 'gwq': [DEPTH, 128, 1], 'gwk': [DEPTH, 128, 1], 'gnq': [DEPTH, 128, 1], 'gnk': [DEPTH, 128, 1],
    'sink': [DEPTH, 128, 8], 'nabias': [DEPTH, 7, 128, 8, 128], 'rbias': [DEPTH, 128, NE], 'bguT': [DEPTH, 128, NE, 16],
}
BIG = {'w_mod': [DEPTH, D, 6 * D], 'w_in': [DEPTH, D, INW], 'ssm_w_glu': [DEPTH, 512, 512],
       'w_branch': [DEPTH, 3, 512, D], 'w_out': [DEPTH, D, D], 'router_w': [DEPTH, D, NE],
       'w_gate_up': [DEPTH, NE, D, 2 * D], 'w_down': [DEPTH, NE, D, D], 'b_down': [DEPTH, NE, D]}


def build(nlayers=DEPTH, dbg=None, stop_after=None):
    nc = bass.Bass("TRN2", target_bir_lowering=False)
    consts = host_consts()
    shapes = {'x': ([L_LAT, D], F32), 'ctx': ([C_CTX, D], F32)}
    for k_, v in consts.items(): shapes[k_] = (list(v.shape), BF if v.dtype != np.float32 else F32)
    for k_, v in SMALL_SHAPES.items(): shapes[k_] = (v, F32)
    for k_, v in BIG.items(): shapes[k_] = ([nlayers] + v[1:], F32)
    class LazyI(dict):
        def __missing__(self, name):
            shp, dt = shapes[name]
            self[name] = nc.dram_tensor(name, list(shp), dt, kind="ExternalInput").ap()
            return self[name]
    I = LazyI()
    yout = nc.dram_tensor('y', [L_LAT, D], F32, kind="ExternalOutput").ap()
    xd = nc.dram_tensor('xd', [T, D], F32, kind="Internal").ap()
    dbg_out = {}
    if dbg:
        for k_, shp in dbg.items():
            dbg_out[k_] = nc.dram_tensor('dbg_' + k_, list(shp), F32, kind="ExternalOutput").ap()

    with ExitStack() as st:
        sbn = [0]
        def sb(name, shape, dt=F32, stack=None):
            sbn[0] += 1
            return (stack or st).enter_context(nc.sbuf_tensor('s%d_%s' % (sbn[0], name), list(shape), dt))
        ident_f = sb('ident_f', [128, 128]); ident_b = sb('ident_b', [128, 128], BF)
        blockones = sb('blockones', [128, 128], BF); ones_f = sb('ones_f', [128, 128]); rotT = sb('rotT', [128, 128], BF)
        cosT = sb('cosT', [128, L_LAT], BF); sinT = sb('sinT', [128, L_LAT], BF)
        wmask = sb('wmask', [128, 2, 128], BF)
        nmk, nidx = na_mask_table(); NM = nmk.shape[0]
        namask = sb('namask', [128, NM, 128], BF)
        maskZ = sb('maskZ', [128, 4, 128]); maskY = sb('maskY', [128, 4, 128])
        epsT = sb('epsT', [128, 1]); halfpi = sb('halfpi', [128, 1]); hm = sb('hm', [128, 2])
        sc = sb('sc', [128, 8, 2])
        modc = sb('modc', [128, 48, 2])
        s1 = sb('s1', [128, 8, 2]); s0 = sb('s0', [128, 8, 2])
        modb = sb('modb', [128, 2, D])
        hT = sb('hT', [128, 8, T], BF)
        ps = [st.enter_context(nc.psum_tensor('ps%d' % i, [128, 512], F32)) for i in range(6)]
        pT = [st.enter_context(nc.psum_tensor('pT%d' % i, [128, 1024], BF)) for i in range(2)]
        block = st.enter_context(nc.Block())
        S = Sched(nc, block, st)
        psn = [0]
        def PS():
            i = psn[0] % 6; psn[0] += 1
            return ps[i], 'ps%d' % i
        ptn = [0]
        def PT_():
            i = ptn[0] % 2; ptn[0] += 1
            return pT[i], 'pT%d' % i

        def act(out, in_, func, R, W, **kw): S.add('act', lambda e: e.activation(out=out, in_=in_, func=func, **kw), R, W)
        def ts(eng, out, in0, s1_, s2_, op0, op1, R, W):
            if op1 is None: S.add(eng, lambda e: e.tensor_scalar(out=out, in0=in0, scalar1=s1_, scalar2=None, op0=op0), R, W)
            else: S.add(eng, lambda e: e.tensor_scalar(out=out, in0=in0, scalar1=s1_, scalar2=s2_, op0=op0, op1=op1), R, W)
        def tt(eng, out, in0, in1, op, R, W): S.add(eng, lambda e: e.tensor_tensor(out=out, in0=in0, in1=in1, op=op), R, W)
        def stt(out, in0, scalar, in1, op0, op1, R, W):
            S.add('dve', lambda e: e.scalar_tensor_tensor(out=out, in0=in0, scalar=scalar, in1=in1, op0=op0, op1=op1), R, W)
        def mm(out, lhsT, rhs, start, stop, R, W): S.add('pe', lambda e: e.matmul(out, lhsT, rhs, start=start, stop=stop), R, W)
        def tr(out, in_, ident, R, W): S.add('pe', lambda e: e.transpose(out, in_, ident), R, W)
        def cp(eng, out, in_, R, W):
            if eng == 'act': S.add('act', lambda e: e.copy(out=out, in_=in_), R, W)
            else: S.add(eng, lambda e: e.tensor_copy(out=out, in_=in_), R, W)
        def ms(eng, ap, val, W): S.add(eng, lambda e: e.memset(ap, val), (), W)
        def rec(out, in_, R, W): S.add('dve', lambda e: e.reciprocal(out=out, in_=in_), R, W)

        for k_, t_ in [('ident_f', ident_f), ('ident_b', ident_b), ('blockones', blockones), ('ones_f', ones_f), ('rotT', rotT),
                       ('cosT', cosT), ('sinT', sinT), ('maskZ', None), ('maskY', None), ('wmask', None), ('namask', None)]:
            if t_ is not None: S.dma('sp', t_[:], I[k_], (), (k_,))
        S.dma('sp', maskZ[:], I['maskZ'].rearrange("g p c -> p g c"), (), ('maskZ',))
        S.dma('sp', maskY[:], I['maskY'].rearrange("g p c -> p g c"), (), ('maskY',))
        S.dma('sp', wmask[:], I['wmask'].rearrange("g p c -> p g c"), (), ('wmask',))
        S.dma('sp', namask[:], I['namask'].rearrange("g p c -> p g c"), (), ('namask',))
        ms('dve', hm[:], 0.0, ('hm',)); ms('dve', hm[0:64, 0:1], 1.0, ('hm',)); ms('dve', hm[64:128, 1:2], 1.0, ('hm',))
        ms('dve', epsT[:], EPS, ('epsT',)); ms('dve', halfpi[:], float(np.pi / 2), ('halfpi',))
        S.dma('sp', xd[0:C_CTX, :], I['ctx'], (), ('xd0', 'xd1'))
        S.dma('sp', xd[C_CTX:T, :], I['x'], (), tuple('xd%d' % t for t in range(2, NT)))
        S.dma('sp', sc[:], I['cT'], (), ('sc',))
        act(sc[:], sc[:], AF.Silu, ('sc',), ('sc',))
        S.flush()

        for li in range(nlayers):
            with ExitStack() as ph:
                wm = [sb('wm%d' % i, [128, 8, 512], F32, ph) for i in range(2)]
                bmod = sb('bmod', [128, 48], F32, ph)
                S.dma('sp', bmod[:], I['bmodT'][li], (), ('bmod',))
                wv = I['w_mod'][li].rearrange("(kc p) n -> p kc n", p=128)
                for blk in range(12):
                    w = wm[blk % 2]; wk = 'wm%d' % (blk % 2)
                    S.dma('sp', w[:], wv[:, :, blk * 512:(blk + 1) * 512], (), (wk,))
                    p_, pk = PS()
                    for j in range(4):
                        for kc in range(8):
                            mm(p_[:, 2 * j:2 * j + 2], w[:, kc, j * 128:(j + 1) * 128], sc[:, kc, :], kc == 0, kc == 7, (wk, 'sc'), (pk,))
                    tt('dve', modc[:, blk * 4:blk * 4 + 4, :], p_[:, 0:8].rearrange("p (j c) -> p j c", c=2),
                       bl(bmod[:, blk * 4:blk * 4 + 4], 2), ALU.add, (pk, 'bmod'), ('modc',))
                S.barrier()
            gcol = sb('gcol', [128, 8], F32, st) if li == 0 else gcol
            if stop_after == 'adaln': break

            def mod_scale_shift(gname, jsc, jsh):
                S.dma('sp', gcol[:], I[gname][li], (), ('gcol',))
                ts('dve', s1[:], modc[:, 8 * jsc:8 * jsc + 8, :], 1.0, None, ALU.add, None, ('modc',), ('s1',))
                tt('dve', s1[:], s1[:], bl(gcol[:], 2), ALU.mult, ('s1', 'gcol'), ('s1',))
                cp('dve', s0[:], modc[:, 8 * jsh:8 * jsh + 8, :], ('modc',), ('s0',))

            def mod_bcast(jg):
                with ExitStack() as ph:
                    dg = sb('dg', [128, 128], F32, ph)
                    for v in range(2):
                        for kc in range(8):
                            ts('dve', dg[:], ident_f[:], modc[:, 8 * jg + kc, v:v + 1], None, ALU.mult, None, ('ident_f', 'modc'), ('dg',))
                            p_, pk = PS()
                            mm(p_[:, 0:128], ones_f[:], dg[:], True, True, ('ones_f', 'dg'), (pk,))
                            cp('act', modb[:, v, kc * 128:(kc + 1) * 128], p_[:, 0:128], (pk,), ('modb',))
                    S.barrier()

            def norm_stage():
                with ExitStack() as ph:
                    xt = [sb('xt%d' % i, [128, D], F32, ph) for i in range(2)]
                    junk = sb('junk', [128, D], BF, ph); xn = [sb('xn%d' % i, [128, D], BF, ph) for i in range(2)]
                    ss = sb('ss', [128, NT], F32, ph)
                    for tt_ in range(NT):
                        b_ = tt_ % 2; v = 1 if tt_ < 2 else 0
                        S.dma('sp', xt[b_][:], xd[tt_ * 128:(tt_ + 1) * 128, :], ('xd%d' % tt_,), ('xt%d' % b_,))
                        act(junk[:], xt[b_][:], AF.Square, ('xt%d' % b_,), ('junk', 'ss%d' % tt_), accum_out=ss[:, tt_:tt_ + 1])
                        act(ss[:, tt_:tt_ + 1], ss[:, tt_:tt_ + 1], AF.Sqrt, ('ss%d' % tt_, 'epsT'), ('ss%d' % tt_,), bias=epsT[:], scale=1.0 / D)
                        rec(ss[:, tt_:tt_ + 1], ss[:, tt_:tt_ + 1], ('ss%d' % tt_,), ('ss%d' % tt_,))
                        ts('dve', xn[b_][:], xt[b_][:], ss[:, tt_:tt_ + 1], None, ALU.mult, None, ('xt%d' % b_, 'ss%d' % tt_), ('xn%d' % b_,))
                        p_, pk = PT_()
                        for kc in range(8):
                            tr(p_[:, kc * 128:(kc + 1) * 128], xn[b_][:, kc * 128:(kc + 1) * 128], ident_b[:], ('xn%d' % b_, 'ident_b'), (pk,))
                        for kc in range(8):
                            ts('dve', hT[:, kc, tt_ * 128:(tt_ + 1) * 128], p_[:, kc * 128:(kc + 1) * 128], s1[:, kc, v:v + 1], s0[:, kc, v:v + 1],
                               ALU.mult, ALU.add, (pk, 's1', 's0'), ('hT',))
                    S.barrier()

            win = I['w_in'][li].rearrange("(kc p) n -> p kc n", p=128)

            def load_w(wt, wkey, view, c0, n, dst0=0):
                S.dma('pool', wt[:, :, dst0:dst0 + n], view[:, :, c0:c0 + n], (), (wkey,))

            def proj_fm(wt, wkey, wc0, consumer, src=None, nk=8, srckey='hT'):
                src = hT if src is None else src
                for (c0, n) in CH:
                    p_, pk = PS()
                    for kc in range(nk):
                        mm(p_[:, 0:n], wt[:, kc, wc0:wc0 + 128], src[:, kc, c0:c0 + n], kc == 0, kc == nk - 1, (wkey,) + (srckey if isinstance(srckey, tuple) else (srckey,)), (pk,))
                    consumer(p_[:, 0:n], pk, c0, n)

            mod_scale_shift('gmix', 1, 0)
            norm_stage()
            if stop_after == 'norm': break
            with ExitStack() as mx:
                yT0 = sb('yT0', [128, 4, T], BF, mx)
                yTbox = [None]
                class _YT:
                    def __getitem__(self, idx):
                        p, m, sl = idx
                        if isinstance(m, slice):
                            if m.start >= 4: return yTbox[0][p, m.start - 4:m.stop - 4, sl]
                            return yT0[p, m, sl]
                        if m >= 4: return yTbox[0][p, m - 4, sl]
                        return yT0[p, m, sl]
                yT = _YT()
                with ExitStack() as ph:
                    uT = sb('uT', [128, 4, T], BF, ph)
                    acc = sb('acc', [128, 4, T], F32, ph)
                    dsk = sb('dsk', [128, 4], F32, ph)
                    wa_scope = ExitStack()
                    wa = sb('wa', [128, 8, 512], BF, wa_scope)
                    S.dma('sp', dsk[:], I['dskip'][li], (), ('dsk',))
                    load_w(wa, 'wa', win, 0, 512)
                    for m in range(4):
                        def cons(p_, pk, c0, n, m=m):
                            import os
                            dbgv = int(os.environ.get('S5DBG', '0'))
                            if dbgv in (0, 2): cp('act', uT[:, m, c0:c0 + n], p_, (pk,), ('uT%d' % m,))
                            if dbgv in (0, 3): ts('dve', acc[:, m, c0:c0 + n], uT[:, m, c0:c0 + n], dsk[:, m:m + 1], None, ALU.mult, None, ('uT%d' % m, 'dsk'), ('acc%d' % m,))
                        proj_fm(wa, 'wa', m * 128, cons)
                    S.barrier(); wa_scope.close()
                    if stop_after == 's5a': break
                    P = {k_: sb('sp_' + k_, [128, 32], F32, ph) for k_ in ['lr', 'li', 'dt', 'mag', 'c', 's', 't1', 't2', 'ar', 'ai', 'fr', 'fi', 'den']}
                    S.dma('sp', P['lr'][:], I['lamre'][li], (), ('p_lr',)); S.dma('sp', P['li'][:], I['lamim'][li], (), ('p_li',))
                    S.dma('sp', P['dt'][:], I['logdt'][li], (), ('p_dt',))
                    K_ = ('sp',)
                    act(P['dt'][:], P['dt'][:], AF.Exp, ('p_dt',), ('p_dt',))
                    tt('dve', P['t1'][:], P['lr'][:], P['dt'][:], ALU.mult, ('p_lr', 'p_dt'), K_)
                    act(P['mag'][:], P['t1'][:], AF.Exp, K_, K_)
                    tt('dve', P['t2'][:], P['li'][:], P['dt'][:], ALU.mult, ('p_li', 'p_dt'), K_)
                    act(P['s'][:], P['t2'][:], AF.Sin, K_, K_, scale=1.0 / 16)
                    act(P['c'][:], P['t2'][:], AF.Sin, K_ + ('halfpi',), K_, scale=1.0 / 16, bias=halfpi[:])
                    for _ in range(4):
                        tt('dve', P['t1'][:], P['c'][:], P['c'][:], ALU.mult, K_, K_)
                        tt('dve', P['t2'][:], P['s'][:], P['s'][:], ALU.mult, K_, K_)
                        tt('dve', P['s'][:], P['s'][:], P['c'][:], ALU.mult, K_, K_)
                        ts('dve', P['s'][:], P['s'][:], 2.0, None, ALU.mult, None, K_, K_)
                        tt('dve', P['c'][:], P['t1'][:], P['t2'][:], ALU.subtract, K_, K_)
                    tt('dve', P['ar'][:], P['mag'][:], P['c'][:], ALU.mult, K_, K_)
                    tt('dve', P['ai'][:], P['mag'][:], P['s'][:], ALU.mult, K_, K_)
                    tt('dve', P['den'][:], P['lr'][:], P['lr'][:], ALU.mult, K_, K_)
                    tt('dve', P['t1'][:], P['li'][:], P['li'][:], ALU.mult, K_, K_)
                    tt('dve', P['den'][:], P['den'][:], P['t1'][:], ALU.add, K_, K_)
                    rec(P['den'][:], P['den'][:], K_, K_)
                    ts('dve', P['t1'][:], P['ar'][:], -1.0, None, ALU.add, None, K_, K_)
                    tt('dve', P['fr'][:], P['t1'][:], P['lr'][:], ALU.mult, K_, K_)
                    tt('dve', P['t2'][:], P['ai'][:], P['li'][:], ALU.mult, K_, K_)
                    tt('dve', P['fr'][:], P['fr'][:], P['t2'][:], ALU.add, K_, K_)
                    tt('dve', P['fr'][:], P['fr'][:], P['den'][:], ALU.mult, K_, K_)
                    tt('dve', P['fi'][:], P['ai'][:], P['lr'][:], ALU.mult, K_, K_)
                    tt('dve', P['t2'][:], P['t1'][:], P['li'][:], ALU.mult, K_, K_)
                    tt('dve', P['fi'][:], P['fi'][:], P['t2'][:], ALU.subtract, K_, K_)
                    tt('dve', P['fi'][:], P['fi'][:], P['den'][:], ALU.mult, K_, K_)
                    pwr = sb('pwr', [128, 12, 32], F32, ph); pwi = sb('pwi', [128, 12, 32], F32, ph); npwi = sb('npwi', [128, 12, 32], F32, ph)
                    cp('dve', pwr[:, 0, :], P['ar'][:], K_, K_); cp('dve', pwi[:, 0, :], P['ai'][:], K_, K_)
                    for i in range(1, 12):
                        tt('dve', P['t1'][:], pwr[:, i - 1, :], pwr[:, i - 1, :], ALU.mult, K_, K_)
                        tt('dve', P['t2'][:], pwi[:, i - 1, :], pwi[:, i - 1, :], ALU.mult, K_, K_)
                        tt('dve', pwr[:, i, :], P['t1'][:], P['t2'][:], ALU.subtract, K_, K_)
                        tt('dve', P['t1'][:], pwr[:, i - 1, :], pwi[:, i - 1, :], ALU.mult, K_, K_)
                        ts('dve', pwi[:, i, :], P['t1'][:], 2.0, None, ALU.mult, None, K_, K_)
                    ts('dve', npwi[:], pwi[:], -1.0, None, ALU.mult, None, K_, K_)
                    br = sb('br', [128, 32, 16], F32, ph); bi = sb('bi', [128, 32, 16], F32, ph)
                    bbr = sb('bbr', [128, 32, 16], F32, ph); bbi = sb('bbi', [128, 32, 16], F32, ph); tb = sb('tb', [128, 32, 16], F32, ph)
                    S.dma('sp', br[:], I['bre'][li], (), K_); S.dma('sp', bi[:], I['bim'][li], (), K_)
                    frb = bl(P['fr'][:], 16); fib = bl(P['fi'][:], 16)
                    tt('dve', bbr[:], br[:], frb, ALU.mult, K_, K_); tt('dve', tb[:], bi[:], fib, ALU.mult, K_, K_)
                    tt('dve', bbr[:], bbr[:], tb[:], ALU.subtract, K_, K_)
                    tt('dve', bbi[:], bi[:], frb, ALU.mult, K_, K_); tt('dve', tb[:], br[:], fib, ALU.mult, K_, K_)
                    tt('dve', bbi[:], bbi[:], tb[:], ALU.add, K_, K_)
                    cn_r = sb('cn_r', [128, 8, 64], F32, ph); cn_i = sb('cn_i', [128, 8, 64], F32, ph)
                    S.dma('sp', cn_r[:], I['cre'][li], (), K_); S.dma('sp', cn_i[:], I['cim'][li], (), K_)
                    ts('dve', cn_i[:], cn_i[:], -1.0, None, ALU.mult, None, K_, K_)
                    Zt = sb('Zt', [128, 128], F32, ph)
                    BTr = sb('BTr', [128, 128], BF, ph); BTi = sb('BTi', [128, 128], BF, ph)
                    CTr = sb('CTr', [128, 128], F32, ph); CTi = sb('CTi', [128, 128], F32, ph)
                    KA = [sb('ks%d' % i, [128, T], F32, ph) for i in range(4)]
                    S.barrier()
                    if stop_after == 's5b': break
                    for d_ in range(2):
                        if stop_after == 's5c' and d_ == 1: break
                        for gp in range(16):
                            if stop_after == 's5c' and gp == 1: break
                            col = d_ * 16 + gp; tile_ = gp // 4; gl = gp % 4
                            for src_, dst_, dk in [(bbr, BTr, 'BTr'), (bbi, BTi, 'BTi')]:
                                tt('dve', Zt[:].rearrange("p (a q) -> p a q", q=16), bmid(src_[:, col, :], 8),
                                   maskZ[:, gl, :].rearrange("p (a q) -> p a q", q=16), ALU.mult, K_ + ('maskZ',), ('Zt',))
                                p_, pk = PS()
                                tr(p_[:, 0:128], Zt[:], ident_f[:], ('Zt', 'ident_f'), (pk,))
                                cp('act', dst_[:], p_[:, 0:128], (pk,), (dk,))
                            for src_, dst_, dk in [(cn_r, CTr, 'CTr'), (cn_i, CTi, 'CTi')]:
                                tt('dve', Zt[:].rearrange("p (a n) -> p a n", n=64), bmid(src_[:, d_ * 4 + tile_, :], 2),
                                   maskY[:, gl, :].rearrange("p (a n) -> p a n", n=64), ALU.mult, K_ + ('maskY',), ('Zt',))
                                p_, pk = PS()
                                tr(p_[:, 0:128], Zt[:], ident_f[:], ('Zt', 'ident_f'), (pk,))
                                cp('act', dst_[:], p_[:, 0:128], (pk,), (dk,))
                            def pos(c0, n):
                                if d_ == 0: return c0
                                return (c0 - C_CTX) if c0 >= C_CTX else L_LAT
                            for (c0, n) in CH:
                                for lh, dst_, lk, dk in [(BTr, KA[0], 'BTr', 'ka0'), (BTi, KA[1], 'BTi', 'ka1')]:
                                    p_, pk = PS()
                                    mm(p_[:, 0:n], lh[:], uT[:, tile_, c0:c0 + n], True, True, (lk, 'uT%d' % tile_), (pk,))
                                    cp('act', dst_[:, pos(c0, n):pos(c0, n) + n], p_[:, 0:n], (pk,), (dk,))
                            cur = 0
                            for i in range(12):
                                k = 1 << i
                                sr, si = KA[cur], KA[cur + 1]; dr_, di_ = KA[2 - cur], KA[3 - cur]
                                skr, ski = 'ka%d' % cur, 'ka%d' % (cur + 1); dkr, dki = 'ka%d' % (2 - cur), 'ka%d' % (3 - cur)
                                cr = pwr[:, i, col:col + 1]; ci = pwi[:, i, col:col + 1]; nci = npwi[:, i, col:col + 1]
                                if d_ == 0: dst_sl = slice(k, T); src_sl = slice(0, T - k); keep = slice(0, k)
                                else: dst_sl = slice(0, T - k); src_sl = slice(k, T); keep = slice(T - k, T)
                                stt(dr_[:, dst_sl], sr[:, src_sl], cr, sr[:, dst_sl], ALU.mult, ALU.add, (skr,) + K_, (dkr,))
                                stt(dr_[:, dst_sl], si[:, src_sl], nci, dr_[:, dst_sl], ALU.mult, ALU.add, (ski, dkr) + K_, (dkr,))
                                stt(di_[:, dst_sl], si[:, src_sl], cr, si[:, dst_sl], ALU.mult, ALU.add, (ski,) + K_, (dki,))
                                stt(di_[:, dst_sl], sr[:, src_sl], ci, di_[:, dst_sl], ALU.mult, ALU.add, (skr, dki) + K_, (dki,))
                                cp('pool', dr_[:, keep], sr[:, keep], (skr,), (dkr,))
                                cp('pool', di_[:, keep], si[:, keep], (ski,), (dki,))
                                cur = 2 - cur
                            hr, hi = KA[cur], KA[cur + 1]; hkr, hki = 'ka%d' % cur, 'ka%d' % (cur + 1)
                            for (c0, n) in CH:
                                p_, pk = PS()
                                mm(p_[:, 0:n], CTr[:], hr[:, pos(c0, n):pos(c0, n) + n], True, False, ('CTr', hkr), (pk,))
                                mm(p_[:, 0:n], CTi[:], hi[:, pos(c0, n):pos(c0, n) + n], False, True, ('CTi', hki), (pk,))
                                tt('dve', acc[:, tile_, c0:c0 + n], acc[:, tile_, c0:c0 + n], p_[:, 0:n], ALU.add, (pk, 'acc%d' % tile_), ('acc%d' % tile_,))
                            S.flush()
                    if dbg and 'acc' in dbg_out and li == 0:
                        S.dma('sp', dbg_out['acc'].rearrange("(m p) t -> p (m t)", p=128), acc[:].rearrange("p m t -> p (m t)"), ['acc%d' % m for m in range(4)], ())
                    wg = sb('wg', [128, 4, 512], BF, ph)
                    S.dma('pool', wg[:], I['ssm_w_glu'][li].rearrange("(kc p) n -> p kc n", p=128), (), ('wg',))
                    t3 = KA[0]; gT = uT
                    for m in range(4):
                        a_ = acc[:, m, :]
                        act(t3[:], a_, AF.Square, ('acc%d' % m,), ('ka0',))
                        ts('dve', t3[:], t3[:], 0.044715, 1.0, ALU.mult, ALU.add, ('ka0',), ('ka0',))
                        tt('dve', t3[:], t3[:], a_, ALU.mult, ('ka0', 'acc%d' % m), ('ka0',))
                        act(t3[:], t3[:], AF.Sigmoid, ('ka0',), ('ka0',), scale=1.5957691216057308)
                        tt('dve', gT[:, m, :], t3[:], a_, ALU.mult, ('ka0', 'acc%d' % m), ('uT%d' % m,))
                    for m in range(4):
                        def cons(p_, pk, c0, n, m=m):
                            act(KA[1][:, c0:c0 + n], p_, AF.Sigmoid, (pk,), ('ka1',))
                            tt('dve', yT[:, m, c0:c0 + n], gT[:, m, c0:c0 + n], KA[1][:, c0:c0 + n], ALU.mult, ('ka1', 'uT%d' % m), ('yT%d' % m,))
                        proj_fm(wg, 'wg', m * 128, cons, src=gT, nk=4, srckey=('uT0', 'uT1', 'uT2', 'uT3'))
                    S.barrier()

                if stop_after == 's5': break
                yTbox[0] = sb('yT2', [128, 8, T], BF, mx)
                def qk_prep(dst, dkey, wt, wkey, wc0, gcol_ap, gkey, rope, tmp):
                    raw, sq, rs, qn, t1 = tmp
                    for (c0, n) in CH:
                        p_, pk = PS()
                        for kc in range(8):
                            mm(p_[:, 0:n], wt[:, kc, wc0:wc0 + 128], hT[:, kc, c0:c0 + n], kc == 0, kc == 7, (wkey, 'hT'), (pk,))
                        cp('act', raw[:, 0:n], p_[:, 0:n], (pk,), ('raw',))
                        act(sq[:, 0:n], p_[:, 0:n], AF.Square, (pk,), ('sq',))
                        p2, pk2 = PS()
                        mm(p2[:, 0:n], blockones[:], sq[:, 0:n], True, True, ('blockones', 'sq'), (pk2,))
                        act(rs[:, 0:n], p2[:, 0:n], AF.Sqrt, (pk2, 'epsT'), ('rs',), bias=epsT[:], scale=1.0 / 64)
                        rec(rs[:, 0:n], rs[:, 0:n], ('rs',), ('rs',))
                        if not (rope and c0 >= C_CTX):
                            stt(dst[:, c0:c0 + n], raw[:, 0:n], gcol_ap, rs[:, 0:n], ALU.mult, ALU.mult, ('raw', 'rs', gkey), (dkey,))
                        else:
                            l0 = c0 - C_CTX
                            stt(qn[:, 0:n], raw[:, 0:n], gcol_ap, rs[:, 0:n], ALU.mult, ALU.mult, ('raw', 'rs', gkey), ('qn',))
                            p3, pk3 = PS()
                            mm(p3[:, 0:n], rotT[:], qn[:, 0:n], True, True, ('rotT', 'qn'), (pk3,))
                            tt('dve', t1[:, 0:n], qn[:, 0:n], cosT[:, l0:l0 + n], ALU.mult, ('qn', 'cosT'), ('t1',))
                            tt('dve', raw[:, 0:n], p3[:, 0:n], sinT[:, l0:l0 + n], ALU.mult, (pk3, 'sinT'), ('raw',))
                            tt('pool', dst[:, c0:c0 + n], t1[:, 0:n], raw[:, 0:n], ALU.add, ('t1', 'raw'), (dkey,))

                def attention(kind, hg):
                    with ExitStack() as ph:
                        tmp = (sb('raw', [128, 512], F32, ph), sb('sq', [128, 512], BF, ph), sb('rs', [128, 512], F32, ph),
                               sb('qn', [128, 512], BF, ph), sb('t1', [128, 512], F32, ph))
                        gq = sb('gq', [128, 1], F32, ph); gk = sb('gk', [128, 1], F32, ph)
                        S.dma('sp', gq[:], I['gwq' if kind == 'w' else 'gnq'][li], (), ('gq',))
                        S.dma('sp', gk[:], I['gwk' if kind == 'w' else 'gnk'][li], (), ('gk',))
                        ts('dve', gq[:], gq[:], 0.125, None, ALU.mult, None, ('gq',), ('gq',))
                        wq_ = sb('wq_', [128, 8, 256], BF, ph)
                        qT = sb('qT', [128, 2, T], BF, ph)
                        nkt = 1 if kind == 'w' else 2
                        nv = 1 if kind == 'w' else 4
                        wk_ = sb('wk_', [128, 8, 128 * nkt], BF, ph); kT = sb('kT', [128, nkt, T], BF, ph)
                        wv_ = sb('wv_', [128, 8, 64 * nv], BF, ph); vaug = sb('vaug', [128, NT, nv, 65], BF, ph)
                        if kind == 'w':
                            load_w(wq_, 'wq_', win, 512 + 256 * hg, 256)
                            load_w(wk_, 'wk_', win, 1024 + 64 * hg, 64, 0); load_w(wk_, 'wk_', win, 1024 + 64 * hg, 64, 64)
                            load_w(wv_, 'wv_', win, 1152 + 64 * hg, 64)
                        else:
                            load_w(wq_, 'wq_', win, 1280 + 256 * hg, 256)
                            load_w(wk_, 'wk_', win, 1792 + 256 * hg, 256)
                            load_w(wv_, 'wv_', win, 2304 + 256 * hg, 256)
                        for m in range(2):
                            qk_prep(qT[:, m, :], 'qT', wq_, 'wq_', m * 128, gq[:, 0:1], 'gq', kind == 'w', tmp)
                        for m in range(nkt):
                            qk_prep(kT[:, m, :], 'kT', wk_, 'wk_', m * 128, gk[:, 0:1], 'gk', kind == 'w', tmp)
                        kTm = sb('kTm', [128, nkt, 2, T], BF, ph)
                        for m in range(nkt):
                            for par in range(2):
                                ts('dve', kTm[:, m, par, :], kT[:, m, :], hm[:, par:par + 1], None, ALU.mult, None, ('kT', 'hm'), ('kTm',))
                        ms('pool', vaug[:, :, :, 64:65], 1.0, ('vaug',))
                        for t_ in range(NT):
                            p_, pk = PS()
                            for kc in range(8):
                                mm(p_[:, 0:64 * nv], hT[:, kc, t_ * 128:(t_ + 1) * 128], wv_[:, kc, :], kc == 0, kc == 7, ('hT', 'wv_'), (pk,))
                            cp('act', vaug[:, t_, :, 0:64], p_[:, 0:64 * nv].rearrange("p (v d) -> p v d", d=64), (pk,), ('vaug',))
                        esink = sb('esink', [128, 8], F32, ph)
                        if kind == 'w':
                            S.dma('sp', esink[:], I['sink'][li], (), ('esink',))
                            act(esink[:], esink[:], AF.Exp, ('esink',), ('esink',))
                        else:
                            nab = sb('nab', [128, 7, 4, 128], BF, ph)
                            for di in range(7):
                                S.dma('pool', nab[:, di, :, :], I['nabias'][li, di, :, 4 * hg:4 * hg + 4, :], (), ('nab',))
                        import os
                        adbg = int(os.environ.get('ATTDBG', '0'))
                        if adbg == 1:
                            S.barrier(); return
                        NSLOT = 7
                        PTs = sb('PTs', [128, NSLOT, 4, 128], BF, ph)
                        ytok = sb('ytok', [128, 4, 64], BF, ph); tot = sb('tot', [128, 4], F32, ph)
                        S.flush()
                        ybase = (4 if kind == 'w' else 8) + 2 * hg
                        for tq in range(NT):
                            keys = []
                            if tq < 2: keys = [(0, None, None), (1, None, None)]
                            else:
                                i = tq - 2
                                keys = [(0, None, None), (1, None, None)]
                                if kind == 'w':
                                    for j in (i - 1, i, i + 1):
                                        if 0 <= j < 16:
                                            keys.append((2 + j, None if j == i else (wmask[:, 0, :] if j < i else wmask[:, 1, :]), None))
                                else:
                                    for j in na_tiles(i):
                                        keys.append((2 + j, namask[:, nidx[(i, j)], :], nab[:, j - i + 3, :, :]))
                            assert len(keys) <= NSLOT
                            for sl, (tk, mask, bias) in enumerate(keys):
                                p_, pk = PS()
                                if bias is not None:
                                    mm(p_[:, 0:512], ident_b[:], bias.rearrange("p h q -> p (h q)"), True, False, ('ident_b', 'nab'), (pk,))
                                for h in range(4):
                                    if adbg == 4 and h % 2 == 1: continue
                                    half = (h % 2) * 64
                                    kt = kTm[:, 0 if kind == 'w' else h // 2, h % 2, tk * 128:(tk + 1) * 128]
                                    mm(p_[:, h * 128:(h + 1) * 128], kt, qT[:, h // 2, tq * 128:(tq + 1) * 128],
                                       bias is None, h == 3 or bias is None, ('kTm', 'qT'), (pk,))
                                act(PTs[:, sl, :, :].rearrange("p h q -> p (h q)"), p_[:, 0:512], AF.Exp, (pk,), ('PT%d' % sl,))
                                if mask is not None and adbg not in (3, 4):
                                    tt('pool', PTs[:, sl, :, :], PTs[:, sl, :, :], bmid(mask, 4), ALU.mult, ('PT%d' % sl, 'wmask', 'namask'), ('PT%d' % sl,))
                            if adbg in (2, 3, 4):
                                continue
                            po, pok = PS()
                            for h in range(4):
                                for sl, (tk, mask, bias) in enumerate(keys):
                                    mm(po[:, h * 65:(h + 1) * 65], PTs[:, sl, h, :], vaug[:, tk, 0 if kind == 'w' else h, :],
                                       sl == 0, sl == len(keys) - 1, ('PT%d' % sl, 'vaug'), (pok,))
                            pov = po[:, 0:260].rearrange("p (h e) -> p h e", e=65)
                            if kind == 'w':
                                tt('dve', tot[:], pov[:, :, 64], esink[:, 4 * hg:4 * hg + 4], ALU.add, (pok, 'esink'), ('tot',))
                            else:
                                cp('dve', tot[:], pov[:, :, 64], (pok,), ('tot',))
                            rec(tot[:], tot[:], ('tot',), ('tot',))
                            tt('dve', ytok[:], pov[:, :, 0:64], bl(tot[:], 64), ALU.mult, (pok, 'tot'), ('ytok',))
                            pt_, ptk = PT_()
                            for m in range(2):
                                tr(pt_[:, m * 128:(m + 1) * 128], ytok[:, 2 * m:2 * m + 2, :].rearrange("p h d -> p (h d)"), ident_b[:], ('ytok', 'ident_b'), (ptk,))
                            cp('act', yT[:, ybase:ybase + 2, tq * 128:(tq + 1) * 128], pt_[:, 0:256].rearrange("p (m t) -> p m t", t=128), (ptk,), ('yT%d' % ybase, 'yT%d' % (ybase + 1)))
                            if tq % 6 == 5: S.flush()
                        S.barrier()

                for kind in ('w', 'n'):
                    for hg in range(2):
                        attention(kind, hg)
                if dbg and 'yT' in dbg_out and li == 0:
                    with ExitStack() as ph:
                        yf = sb('yf', [128, 12, T], F32, ph) if False else None

                if stop_after == 'attn': break
                mod_bcast(2)
                with ExitStack() as ph:
                    wbr = sb('wbr', [128, 12, D], BF, ph)
                    S.dma('pool', wbr[:], I['w_branch'][li].rearrange("i (kc p) d -> p (i kc) d", p=128), (), ('wbr',))
                    wo = sb('wo', [128, 8, D], BF, ph)
                    S.dma('pool', wo[:], I['w_out'][li].rearrange("(kc p) d -> p kc d", p=128), (), ('wo',))
                    wgt = [sb('wgt%d' % i, [128, 8, 384], BF, ph) for i in range(2)]
                    sT = sb('sT', [128, 8, 512], BF, ph)
                    sg = [sb('sg%d' % i, [128, 512], F32, ph) for i in range(3)]
                    xr = [sb('xr%d' % i, [128, D], F32, ph) for i in range(2)]
                    tmpm = sb('tmpm', [128, 512], F32, ph)
                    it = 0
                    for (c0, n) in CH:
                        for dt_ in range(8):
                            w = wgt[it % 2]; wk = 'wgt%d' % (it % 2); it += 1
                            for i in range(3):
                                load_w(w, wk, win, 2816 + i * 1024 + dt_ * 128, 128, i * 128)
                            for i in range(3):
                                pg, pgk = PS()
                                for kc in range(8):
                                    mm(pg[:, 0:n], w[:, kc, i * 128:(i + 1) * 128], hT[:, kc, c0:c0 + n], kc == 0, kc == 7, (wk, 'hT'), (pgk,))
                                act(sg[i][:, 0:n], pg[:, 0:n], AF.Sigmoid, (pgk,), ('sg%d' % i,))
                                pp, ppk = PS()
                                for kc in range(4):
                                    mm(pp[:, 0:n], wbr[:, i * 4 + kc, dt_ * 128:(dt_ + 1) * 128], yT[:, i * 4 + kc, c0:c0 + n], kc == 0, kc == 3,
                                       ('wbr',) + tuple('yT%d' % q for q in range(12)), (ppk,))
                                tt('dve', sg[i][:, 0:n], sg[i][:, 0:n], pp[:, 0:n], ALU.mult, (ppk, 'sg%d' % i), ('sg%d' % i,))
                            tt('pool', sg[0][:, 0:n], sg[0][:, 0:n], sg[1][:, 0:n], ALU.add, ('sg0', 'sg1'), ('sg0',))
                            tt('pool', sT[:, dt_, 0:n], sg[0][:, 0:n], sg[2][:, 0:n], ALU.add, ('sg0', 'sg2'), ('sT',))
                        for tl_ in range(n // 128):
                            tt_ = c0 // 128 + tl_; b_ = tt_ % 2; v = 1 if tt_ < 2 else 0
                            S.dma('sp', xr[b_][:], xd[tt_ * 128:(tt_ + 1) * 128, :], ('xd%d' % tt_,), ('xr%d' % b_,))
                            for hf in range(2):
                                p_, pk = PS()
                                for kc in range(8):
                                    mm(p_[:, 0:512], sT[:, kc, tl_ * 128:(tl_ + 1) * 128], wo[:, kc, hf * 512:(hf + 1) * 512], kc == 0, kc == 7, ('sT', 'wo'), (pk,))
                                tt('dve', tmpm[:], p_[:, 0:512], modb[:, v, hf * 512:(hf + 1) * 512], ALU.mult, (pk, 'modb'), ('tmpm',))
                                tt('pool', xr[b_][:, hf * 512:(hf + 1) * 512], xr[b_][:, hf * 512:(hf + 1) * 512], tmpm[:], ALU.add, ('tmpm', 'xr%d' % b_), ('xr%d' % b_,))
                            S.dma('sp', xd[tt_ * 128:(tt_ + 1) * 128, :], xr[b_][:], ('xr%d' % b_,), ('xd%d' % tt_,))
                    S.barrier()

            if stop_after == 'merge': break
            mod_scale_shift('gffn', 4, 3)
            norm_stage()
            mod_bcast(5)
            with ExitStack() as ph:
                macc = sb('macc', [128, NT, D], F32, ph)
                G = sb('G', [128, NT, NE], F32, ph)
                rw = sb('rw', [128, 8, NE], BF, ph); rb = sb('rb', [128, NE], F32, ph)
                S.dma('pool', rw[:], I['router_w'][li].rearrange("(kc p) e -> p kc e", p=128), (), ('rw',))
                S.dma('sp', rb[:], I['rbias'][li], (), ('rb',))
                lg = sb('lg', [128, NE], F32, ph); m8 = sb('m8', [128, 8], F32, ph); mk = sb('mk', [128, NE], F32, ph)
                nmx = sb('nmx', [128, 1], F32, ph); sm = sb('sm', [128, 1], F32, ph)
                ms('pool', macc[:], 0.0, tuple('macc%d' % t for t in range(NT)))
                for t_ in range(NT):
                    p_, pk = PS()
                    for kc in range(8):
                        mm(p_[:, 0:NE], hT[:, kc, t_ * 128:(t_ + 1) * 128], rw[:, kc, :], kc == 0, kc == 7, ('hT', 'rw'), (pk,))
                    tt('dve', lg[:], p_[:, 0:NE], rb[:], ALU.add, (pk, 'rb'), ('lg',))
                    S.add('dve', lambda e: e.max(out=m8[:], in_=lg[:]), ('lg',), ('m8',))
                    ts('dve', mk[:], lg[:], m8[:, 3:4], None, ALU.is_ge, None, ('lg', 'm8'), ('mk',))
                    ts('dve', nmx[:], m8[:, 0:1], -1.0, None, ALU.mult, None, ('m8',), ('nmx',))
                    act(lg[:], lg[:], AF.Exp, ('lg', 'nmx'), ('lg',), bias=nmx[:])
                    tt('dve', lg[:], lg[:], mk[:], ALU.mult, ('lg', 'mk'), ('lg',))
                    S.add('dve', lambda e: e.reduce_sum(out=sm[:], in_=lg[:], axis=mybir.AxisListType.X), ('lg',), ('sm',))
                    rec(sm[:], sm[:], ('sm',), ('sm',))
                    ts('dve', G[:, t_, :], lg[:], sm[:, 0:1], None, ALU.mult, None, ('lg', 'sm'), ('G',))
                S.flush()
                wgu = sb('wgu', [128, 8, 2 * D], BF, ph); wdn = sb('wdn', [128, 8, D], BF, ph)
                bgu = sb('bgu', [128, NE, 16], F32, ph); bd = sb('bd', [1, D], BF, ph); onesr = sb('onesr', [1, 128], BF, ph)
                S.dma('sp', bgu[:], I['bguT'][li], (), ('bgu',))
                ts('dve', bgu[:, :, 8:16], bgu[:, :, 8:16], 1.0, None, ALU.add, None, ('bgu',), ('bgu',))
                ms('dve', onesr[:], 1.0, ('onesr',))
                actT = sb('actT', [128, 8, 512], BF, ph)
                g1 = sb('g1', [128, 512], F32, ph); sgm = sb('sgm', [128, 512], F32, ph); u1 = sb('u1', [128, 512], F32, ph)
                for e_ in range(NE):
                    wgv = I['w_gate_up'][li, e_].rearrange("(kc p) n -> p kc n", p=128)
                    for kc in range(8):
                        S.dma('pool', wgu[:, kc, :], wgv[:, kc, :], (), ('wgu',))
                    S.dma('pool', wdn[:], I['w_down'][li, e_].rearrange("(kc p) n -> p kc n", p=128), (), ('wdn',))
                    S.dma('pool', bd[:], I['b_down'][li, e_:e_ + 1, :], (), ('bd',))
                    for (c0, n) in CH:
                        for m in range(8):
                            pg, pgk = PS()
                            for kc in range(8):
                                mm(pg[:, 0:n], wgu[:, kc, m * 128:(m + 1) * 128], hT[:, kc, c0:c0 + n], kc == 0, kc == 7, ('wgu', 'hT'), (pgk,))
                            pu, puk = PS()
                            for kc in range(8):
                                mm(pu[:, 0:n], wgu[:, kc, D + m * 128:D + (m + 1) * 128], hT[:, kc, c0:c0 + n], kc == 0, kc == 7, ('wgu', 'hT'), (puk,))
                            ts('dve', g1[:, 0:n], pg[:, 0:n], bgu[:, e_, m:m + 1], 7.0, ALU.add, ALU.min, (pgk, 'bgu'), ('g1',))
                            act(sgm[:, 0:n], g1[:, 0:n], AF.Sigmoid, ('g1',), ('sgm',), scale=1.702)
                            tt('pool', g1[:, 0:n], g1[:, 0:n], sgm[:, 0:n], ALU.mult, ('g1', 'sgm'), ('g1',))
                            ts('dve', u1[:, 0:n], pu[:, 0:n], bgu[:, e_, 8 + m:9 + m], 8.0, ALU.add, ALU.min, (puk, 'bgu'), ('u1',))
                            stt(actT[:, m, 0:n], u1[:, 0:n], -6.0, g1[:, 0:n], ALU.max, ALU.mult, ('u1', 'g1'), ('actT',))
                        for tl_ in range(n // 128):
                            tt_ = c0 // 128 + tl_
                            for hf in range(2):
                                p_, pk = PS()
                                for kc in range(8):
                                    mm(p_[:, 0:512], actT[:, kc, tl_ * 128:(tl_ + 1) * 128], wdn[:, kc, hf * 512:(hf + 1) * 512], kc == 0, False, ('actT', 'wdn'), (pk,))
                                mm(p_[:, 0:512], onesr[:], bd[:, hf * 512:(hf + 1) * 512], False, True, ('onesr', 'bd'), (pk,))
                                sl_ = macc[:, tt_, hf * 512:(hf + 1) * 512]
                                stt(sl_, p_[:, 0:512], G[:, tt_, e_:e_ + 1], sl_, ALU.mult, ALU.add, (pk, 'G', 'macc%d' % tt_), ('macc%d' % tt_,))
                    S.flush()
                S.barrier()
                xr = [wgu[:, i, :].bitcast(F32) for i in range(2)]
                for t_ in range(NT):
                    b_ = t_ % 2; v = 1 if t_ < 2 else 0
                    S.dma('sp', xr[b_], xd[t_ * 128:(t_ + 1) * 128, :], ('xd%d' % t_,), ('xq%d' % b_,))
                    tt('dve', macc[:, t_, :], macc[:, t_, :], modb[:, v, :], ALU.mult, ('macc%d' % t_, 'modb'), ('macc%d' % t_,))
                    tt('pool', xr[b_], xr[b_], macc[:, t_, :], ALU.add, ('xq%d' % b_, 'macc%d' % t_), ('xq%d' % b_,))
                    if li == nlayers - 1:
                        if t_ >= 2:
                            S.dma('sp', yout[(t_ - 2) * 128:(t_ - 1) * 128, :], xr[b_], ('xq%d' % b_,), ('yout%d' % t_,))
                    else:
                        S.dma('sp', xd[t_ * 128:(t_ + 1) * 128, :], xr[b_], ('xq%d' % b_,), ('xd%d' % t_,))
                S.barrier()
        S.barrier()
    return nc, consts, list(I.keys())


def make_in_maps(inputs, cores, nc_consts, names=None):
    maps = []
    big = {k_: np.ascontiguousarray(inputs[k_], dtype=np.float32) for k_ in BIG if names is None or k_ in names}
    for b in cores:
        m = {'x': np.ascontiguousarray(inputs['x'][b]), 'ctx': np.ascontiguousarray(inputs['ctx'][b])}
        m.update(nc_consts)
        m.update(host_layout(inputs, b))
        m.update(big)
        if names is not None: m = {k_: v for k_, v in m.items() if k_ in names}
        maps.append(m)
    return maps


def kernel(**inputs):
    inputs = {k_: np.asarray(v) for k_, v in inputs.items()}
    nc, consts, names = build(DEPTH)
    maps = make_in_maps(inputs, list(range(8)), consts, names)
    res = run_bass_kernel_spmd(nc, maps, core_ids=list(range(8)))
    return np.stack([r['y'] for r in res.results], 0).astype(np.float32)
```

```python
import numpy as np
from contextlib import ExitStack
import concourse.bass as bass
import concourse.mybir as mybir
from concourse.bass_utils import run_bass_kernel_spmd

F32 = mybir.dt.float32
BF = mybir.dt.bfloat16
ALU = mybir.AluOpType
AF = mybir.ActivationFunctionType

D = 1024; L_LAT = 2048; C_CTX = 256; T = 2304; NT = 18; DEPTH = 4
NE = 32; INW = 5888
CH = [(0, 256)] + [(256 + 512 * j, 512) for j in range(4)]
EPS = 1e-6
ND = 12
SAME_ENGINE_SYNC = True


def bl(ap, n):
    return bass.AP(ap.tensor, ap.offset, list(ap.ap) + [(0, n)])


def bmid(ap, n):
    a = list(ap.ap)
    return bass.AP(ap.tensor, ap.offset, [a[0], (0, n)] + a[1:])


class Sched:
    def __init__(s, nc, block, stack):
        s.nc = nc; s.block = block
        s.engs = {'pe': nc.tensor, 'act': nc.scalar, 'dve': nc.vector, 'pool': nc.gpsimd, 'sp': nc.sync}
        s.tl = {e: stack.enter_context(nc.semaphore('tl_' + e)) for e in ['pe', 'act', 'dve', 'pool']}
        s.cnt = {e: 0 for e in s.tl}
        s.dsem = {q: [stack.enter_context(nc.semaphore('d_%s%d' % (q, i))) for i in range(ND)] for q in ['sp', 'pool']}
        s.dval = {q: [0] * ND for q in s.dsem}; s.dnext = {q: 0 for q in s.dsem}
        s.waited = {e: {} for e in s.engs}
        s.lw = {}; s.rd = {}
        s.pend = {e: [] for e in s.engs}
        s.n = 0

    def _waits(s, eng, R, W):
        deps = {}
        def need(tok):
            if tok is None: return
            name, sem, val, src = tok
            if src == eng and (eng in ('pe', 'sp') or not SAME_ENGINE_SYNC): return
            if deps.get(name, (None, 0))[1] < val: deps[name] = (sem, val)
        for k in R: need(s.lw.get(k))
        for k in W:
            need(s.lw.get(k))
            for t in s.rd.get(k, ()): need(t)
        out = []
        for name, (sem, val) in deps.items():
            if s.waited[eng].get(name, 0) < val:
                s.waited[eng][name] = val; out.append((sem, val))
        return out

    def _mark(s, tok, R, W):
        for k in R: s.rd.setdefault(k, []).append(tok)
        for k in W: s.lw[k] = tok; s.rd[k] = []

    def add(s, eng, fn, R=(), W=()):
        waits = s._waits(eng, R, W)
        s.cnt[eng] += 1
        tok = ('tl_' + eng, s.tl[eng], s.cnt[eng], eng)
        s.pend[eng].append((waits, fn, (s.tl[eng], 1)))
        s._mark(tok, R, W); s.n += 1

    def dma(s, q, out, in_, R=(), W=(), **kw):
        waits = s._waits(q, R, W)
        i = s.dnext[q]; s.dnext[q] = (i + 1) % ND
        sem = s.dsem[q][i]; name = 'd_%s%d' % (q, i)
        if s.dval[q][i] > s.waited[q].get(name, 0):
            s.waited[q][name] = s.dval[q][i]; waits.append((sem, s.dval[q][i]))
        s.dval[q][i] += 16
        tok = (name, sem, s.dval[q][i], 'dma')
        s.pend[q].append((waits, lambda e: e.dma_start(out=out, in_=in_, **kw), (sem, 16)))
        s._mark(tok, R, W); s.n += 1

    def barrier(s):
        for eng in s.engs:
            waits = []
            for e2 in s.tl:
                name = 'tl_' + e2
                if s.cnt[e2] > s.waited[eng].get(name, 0) and not (e2 == eng == 'pe'):
                    s.waited[eng][name] = s.cnt[e2]; waits.append((s.tl[e2], s.cnt[e2]))
            for q in s.dsem:
                for i in range(ND):
                    name = 'd_%s%d' % (q, i)
                    if s.dval[q][i] > s.waited[eng].get(name, 0):
                        s.waited[eng][name] = s.dval[q][i]; waits.append((s.dsem[q][i], s.dval[q][i]))
            if waits: s.pend[eng].append((waits, None, None))
        s.flush()

    def wait_all(s, eng, keys):
        waits = s._waits(eng, keys, ())
        s.pend[eng].append((waits, None, None))

    def flush(s):
        for ename, lst in s.pend.items():
            if not lst: continue
            def body(e, lst=lst):
                for waits, fn, inc in lst:
                    for sem, val in waits: e.wait_ge(sem, val)
                    if fn is not None:
                        ins = fn(e)
                        ins.then_inc(inc[0], inc[1])
            getattr(s.block, {'pe': 'tensor', 'act': 'scalar', 'dve': 'vector', 'pool': 'gpsimd', 'sp': 'sync'}[ename])(body)
            s.pend[ename] = []


def na_start(r): return min(max(r - 4, 0), 24)

def na_tiles(i):
    rows = set()
    for r in (2 * i, 2 * i + 1):
        st = na_start(r); rows.update(range(st, st + 8))
    return sorted(set(r // 2 for r in rows))

def na_mask(i, j):
    m = np.zeros((128, 128), np.float32)
    k = np.arange(128); rk = k // 64; ck = k % 64
    for q in range(128):
        rq = q // 64; cq = q % 64
        st = na_start(2 * i + rq)
        row_ok = (2 * j + rk >= st) & (2 * j + rk <= st + 7)
        cs = min(max(cq - 8, 0), 48)
        col_ok = (ck >= cs) & (ck < cs + 16)
        m[:, q] = (row_ok & col_ok)
    return m

_NA_MASKS = None
def na_mask_table():
    global _NA_MASKS
    if _NA_MASKS is None:
        uniq = []; idx = {}
        for i in range(16):
            for j in na_tiles(i):
                m = na_mask(i, j)
                for u, mm in enumerate(uniq):
                    if np.array_equal(mm, m): idx[(i, j)] = u; break
                else:
                    uniq.append(m); idx[(i, j)] = len(uniq) - 1
        _NA_MASKS = (np.stack(uniq), idx)
    return _NA_MASKS


def host_consts():
    bf = mybir.dt.np(BF)
    c = {}
    c['ident_f'] = np.eye(128, dtype=np.float32)
    c['ident_b'] = np.eye(128, dtype=np.float32).astype(bf)
    bo = np.zeros((128, 128), np.float32); bo[:64, :64] = 1; bo[64:, 64:] = 1
    c['blockones'] = bo.astype(bf)
    c['ones_f'] = np.ones((128, 128), np.float32)
    R = np.zeros((64, 64), np.float32)
    for base in (0, 32):
        for i in range(16):
            R[base + i, base + 16 + i] = -1.0
            R[base + 16 + i, base + i] = 1.0
    R2 = np.zeros((128, 128), np.float32); R2[:64, :64] = R; R2[64:, 64:] = R
    c['rotT'] = np.ascontiguousarray(R2.T).astype(bf)
    pos = np.arange(L_LAT); row = pos // 64; col = pos % 64
    inv = (10000.0 ** (-np.arange(16, dtype=np.float32) / 16)).astype(np.float32)
    ang = np.zeros((64, L_LAT), np.float32)
    for dd in range(64):
        p = row if dd < 32 else col
        ang[dd] = p.astype(np.float32) * inv[dd % 16]
    c['cosT'] = np.concatenate([np.cos(ang), np.cos(ang)], 0).astype(bf)
    c['sinT'] = np.concatenate([np.sin(ang), np.sin(ang)], 0).astype(bf)
    b = np.arange(128)[:, None]; a = np.arange(128)[None, :]
    c['wmask'] = np.stack([(a <= b), (b <= a)]).astype(np.float32).astype(bf)
    c['namask'] = na_mask_table()[0].astype(bf)
    mz = np.zeros((4, 128, 128), np.float32); my = np.zeros((4, 128, 128), np.float32)
    for gl in range(4):
        for g2 in range(2):
            mz[gl, 64 * g2:64 * g2 + 64, 32 * gl + 16 * g2: 32 * gl + 16 * g2 + 16] = 1
            my[gl, 32 * gl + 16 * g2: 32 * gl + 16 * g2 + 16, 64 * g2:64 * g2 + 64] = 1
    c['maskZ'] = mz; c['maskY'] = my
    return c


def host_layout(inp, b):
    o = {}
    cb = inp['c'][b].reshape(8, 128).T; cc = inp['c_ctx'].reshape(8, 128).T
    o['cT'] = np.ascontiguousarray(np.stack([cb, cc], -1))
    o['bmodT'] = np.ascontiguousarray(inp['b_mod'].reshape(DEPTH, 48, 128).transpose(0, 2, 1))
    o['gmix'] = np.ascontiguousarray(inp['norm_mix'].reshape(DEPTH, 8, 128).transpose(0, 2, 1))
    o['gffn'] = np.ascontiguousarray(inp['norm_ffn'].reshape(DEPTH, 8, 128).transpose(0, 2, 1))
    def p2(x):
        return np.ascontiguousarray(x.reshape(DEPTH, 2, 16, 2, 64).transpose(0, 3, 4, 1, 2).reshape(DEPTH, 128, 32))
    o['lamre'] = p2(inp['ssm_lam_re']); o['lamim'] = p2(inp['ssm_lam_im'])
    o['logdt'] = p2(np.broadcast_to(inp['ssm_log_dt'][..., None], (DEPTH, 2, 32, 64)))
    def pb(x):
        return np.ascontiguousarray(x.reshape(DEPTH, 2, 16, 2, 64, 16).transpose(0, 3, 4, 1, 2, 5).reshape(DEPTH, 128, 32, 16))
    o['bre'] = pb(inp['ssm_b_re']); o['bim'] = pb(inp['ssm_b_im'])
    def pc(x):
        return np.ascontiguousarray(x.reshape(DEPTH, 2, 4, 8, 16, 64).transpose(0, 3, 4, 1, 2, 5).reshape(DEPTH, 128, 8, 64))
    o['cre'] = pc(inp['ssm_c_re']); o['cim'] = pc(inp['ssm_c_im'])
    o['dskip'] = np.ascontiguousarray(inp['ssm_d'].reshape(DEPTH, 4, 128).transpose(0, 2, 1))
    def hd(x): return np.ascontiguousarray(np.tile(x, (1, 2))[:, :, None])
    o['gwq'] = hd(inp['win_q_norm']); o['gwk'] = hd(inp['win_k_norm'])
    o['gnq'] = hd(inp['na_q_norm']); o['gnk'] = hd(inp['na_k_norm'])
    o['sink'] = np.ascontiguousarray(np.broadcast_to(inp['win_sink'][:, None, :], (DEPTH, 128, 8)))
    k = np.arange(128); rk = k // 64; ck = k % 64
    rpb = inp['na_rpb']
    dc = np.clip(ck[:, None] - ck[None, :], -15, 15) + 15
    B = np.zeros((DEPTH, 7, 128, 8, 128), np.float32)
    for di, dl in enumerate(range(-3, 4)):
        dr = np.clip(2 * dl + rk[:, None] - rk[None, :] + 7, 0, 14)
        dcf = dc[ck[:, None], ck[None, :]]
        B[:, di] = rpb[:, :, dr, dcf].transpose(0, 2, 1, 3)
    o['nabias'] = B
    o['rbias'] = np.ascontiguousarray(np.broadcast_to(inp['router_b'][:, None, :], (DEPTH, 128, NE)))
    o['bguT'] = np.ascontiguousarray(inp['b_gate_up'].reshape(DEPTH, NE, 16, 128).transpose(0, 3, 1, 2))
    return o


SMALL_SHAPES = {
    'cT': [128, 8, 2], 'bmodT': [DEPTH, 128, 48], 'gmix': [DEPTH, 128, 8], 'gffn': [DEPTH, 128, 8],
    'lamre': [DEPTH, 128, 32], 'lamim': [DEPTH, 128, 32], 'logdt': [DEPTH, 128, 32],
    'bre': [DEPTH, 128, 32, 16], 'bim': [DEPTH, 128, 32, 16], 'cre': [DEPTH, 128, 8, 64], 'cim': [DEPTH, 128, 8, 64],
    'dskip': [DEPTH, 128, 4], 'gwq': [DEPTH, 128, 1], 'gwk': [DEPTH, 128, 1], 'gnq': [DEPTH, 128, 1], 'gnk': [DEPTH, 128, 1],
    'sink': [DEPTH, 128, 8], 'nabias': [DEPTH, 7, 128, 8, 128], 'rbias': [DEPTH, 128, NE], 'bguT': [DEPTH, 128, NE, 16],
}
BIG = {'w_mod': [DEPTH, D, 6 * D], 'w_in': [DEPTH, D, INW], 'ssm_w_glu': [DEPTH, 512, 512],
       'w_branch': [DEPTH, 3, 512, D], 'w_out': [DEPTH, D, D], 'router_w': [DEPTH, D, NE],
       'w_gate_up': [DEPTH, NE, D, 2 * D], 'w_down': [DEPTH, NE, D, D], 'b_down': [DEPTH, NE, D]}


def build(nlayers=DEPTH, dbg=None, stop_after=None):
    nc = bass.Bass("TRN2", target_bir_lowering=False, dynamic_dma_scratch_size=4096)
    consts = host_consts()
    shapes = {'x': ([L_LAT, D], F32), 'ctx': ([C_CTX, D], F32)}
    for k_, v in consts.items(): shapes[k_] = (list(v.shape), BF if v.dtype != np.float32 else F32)
    for k_, v in SMALL_SHAPES.items(): shapes[k_] = (v, F32)
    for k_, v in BIG.items(): shapes[k_] = ([nlayers] + v[1:], F32)
    class LazyI(dict):
        def __missing__(self, name):
            shp, dt = shapes[name]
            self[name] = nc.dram_tensor(name, list(shp), dt, kind="ExternalInput").ap()
            return self[name]
    I = LazyI()
    yout = nc.dram_tensor('y', [L_LAT, D], F32, kind="ExternalOutput").ap()
    xd = nc.dram_tensor('xd', [T, D], F32, kind="Internal").ap()
    dbg_out = {}
    if dbg:
        for k_, shp in dbg.items():
            dbg_out[k_] = nc.dram_tensor('dbg_' + k_, list(shp), F32, kind="ExternalOutput").ap()

    with ExitStack() as st:
        sbn = [0]
        def sb(name, shape, dt=F32, stack=None):
            sbn[0] += 1
            return (stack or st).enter_context(nc.sbuf_tensor('s%d_%s' % (sbn[0], name), list(shape), dt))
        ident_f = sb('ident_f', [128, 128]); ident_b = sb('ident_b', [128, 128], BF)
        blockones = sb('blockones', [128, 128], BF); ones_f = sb('ones_f', [128, 128])
        nmk, nidx = na_mask_table(); NM = nmk.shape[0]
        epsT = sb('epsT', [128, 1]); halfpi = sb('halfpi', [128, 1]); hm = sb('hm', [128, 2])
        sc = sb('sc', [128, 8, 2])
        modc = sb('modc', [128, 48, 2])
        s1 = sb('s1', [128, 8, 2]); s0 = sb('s0', [128, 8, 2])
        hT = sb('hT', [128, 8, T], BF)
        ps = [st.enter_context(nc.psum_tensor('ps%d' % i, [128, 512], F32)) for i in range(6)]
        pT = [st.enter_context(nc.psum_tensor('pT%d' % i, [128, 1024], BF)) for i in range(2)]
        block = st.enter_context(nc.Block())
        S = Sched(nc, block, st)
        psn = [0]
        def PS():
            i = psn[0] % 6; psn[0] += 1
            return ps[i], 'ps%d' % i
        ptn = [0]
        def PT_():
            i = ptn[0] % 2; ptn[0] += 1
            return pT[i], 'pT%d' % i

        def act(out, in_, func, R, W, **kw): S.add('act', lambda e: e.activation(out=out, in_=in_, func=func, **kw), R, W)
        def ts(eng, out, in0, s1_, s2_, op0, op1, R, W):
            if op1 is None: S.add(eng, lambda e: e.tensor_scalar(out=out, in0=in0, scalar1=s1_, scalar2=None, op0=op0), R, W)
            else: S.add(eng, lambda e: e.tensor_scalar(out=out, in0=in0, scalar1=s1_, scalar2=s2_, op0=op0, op1=op1), R, W)
        def tt(eng, out, in0, in1, op, R, W): S.add(eng, lambda e: e.tensor_tensor(out=out, in0=in0, in1=in1, op=op), R, W)
        def stt(out, in0, scalar, in1, op0, op1, R, W):
            S.add('dve', lambda e: e.scalar_tensor_tensor(out=out, in0=in0, scalar=scalar, in1=in1, op0=op0, op1=op1), R, W)
        def mm(out, lhsT, rhs, start, stop, R, W): S.add('pe', lambda e: e.matmul(out, lhsT, rhs, start=start, stop=stop), R, W)
        def tr(out, in_, ident, R, W): S.add('pe', lambda e: e.transpose(out, in_, ident), R, W)
        def cp(eng, out, in_, R, W):
            if eng == 'act': S.add('act', lambda e: e.copy(out=out, in_=in_), R, W)
            else: S.add(eng, lambda e: e.tensor_copy(out=out, in_=in_), R, W)
        def ms(eng, ap, val, W): S.add(eng, lambda e: e.memset(ap, val), (), W)
        def rec(out, in_, R, W): S.add('dve', lambda e: e.reciprocal(out=out, in_=in_), R, W)

        for k_, t_ in [('ident_f', ident_f), ('ident_b', ident_b), ('blockones', blockones), ('ones_f', ones_f)]:
            S.dma('sp', t_[:], I[k_], (), (k_,))
        ms('dve', hm[:], 0.0, ('hm',)); ms('dve', hm[0:64, 0:1], 1.0, ('hm',)); ms('dve', hm[64:128, 1:2], 1.0, ('hm',))
        ms('dve', epsT[:], EPS, ('epsT',)); ms('dve', halfpi[:], float(np.pi / 2), ('halfpi',))
        S.dma('sp', xd[0:C_CTX, :], I['ctx'], (), ('xd0', 'xd1'))
        S.dma('sp', xd[C_CTX:T, :], I['x'], (), tuple('xd%d' % t for t in range(2, NT)))
        S.dma('sp', sc[:], I['cT'], (), ('sc',))
        act(sc[:], sc[:], AF.Silu, ('sc',), ('sc',))
        S.flush()

        for li in range(nlayers):
            with ExitStack() as ph:
                wm = [sb('wm%d' % i, [128, 8, 512], F32, ph) for i in range(2)]
                bmod = sb('bmod', [128, 48], F32, ph)
                S.dma('sp', bmod[:], I['bmodT'][li], (), ('bmod',))
                wv = I['w_mod'][li].rearrange("(kc p) n -> p kc n", p=128)
                for blk in range(12):
                    w = wm[blk % 2]; wk = 'wm%d' % (blk % 2)
                    S.dma('sp', w[:], wv[:, :, blk * 512:(blk + 1) * 512], (), (wk,))
                    p_, pk = PS()
                    for j in range(4):
                        for kc in range(8):
                            mm(p_[:, 2 * j:2 * j + 2], w[:, kc, j * 128:(j + 1) * 128], sc[:, kc, :], kc == 0, kc == 7, (wk, 'sc'), (pk,))
                    tt('dve', modc[:, blk * 4:blk * 4 + 4, :], p_[:, 0:8].rearrange("p (j c) -> p j c", c=2),
                       bl(bmod[:, blk * 4:blk * 4 + 4], 2), ALU.add, (pk, 'bmod'), ('modc',))
                S.barrier()
            gcol = sb('gcol', [128, 8], F32, st) if li == 0 else gcol
            if stop_after == 'adaln': break

            def mod_scale_shift(gname, jsc, jsh):
                S.dma('sp', gcol[:], I[gname][li], (), ('gcol',))
                ts('dve', s1[:], modc[:, 8 * jsc:8 * jsc + 8, :], 1.0, None, ALU.add, None, ('modc',), ('s1',))
                tt('dve', s1[:], s1[:], bl(gcol[:], 2), ALU.mult, ('s1', 'gcol'), ('s1',))
                cp('dve', s0[:], modc[:, 8 * jsh:8 * jsh + 8, :], ('modc',), ('s0',))

            def mod_bcast(jg, modb):
                with ExitStack() as ph:
                    dg = sb('dg', [128, 128], F32, ph)
                    for v in range(2):
                        for kc in range(8):
                            ts('dve', dg[:], ident_f[:], modc[:, 8 * jg + kc, v:v + 1], None, ALU.mult, None, ('ident_f', 'modc'), ('dg',))
                            p_, pk = PS()
                            mm(p_[:, 0:128], ones_f[:], dg[:], True, True, ('ones_f', 'dg'), (pk,))
                            cp('act', modb[:, v, kc * 128:(kc + 1) * 128], p_[:, 0:128], (pk,), ('modb',))
                    S.barrier()

            def norm_stage():
                with ExitStack() as ph:
                    xt = [sb('xt%d' % i, [128, D], F32, ph) for i in range(2)]
                    junk = sb('junk', [128, D], BF, ph); xn = [sb('xn%d' % i, [128, D], BF, ph) for i in range(2)]
                    ss = sb('ss', [128, NT], F32, ph)
                    for tt_ in range(NT):
                        b_ = tt_ % 2; v = 1 if tt_ < 2 else 0
                        S.dma('sp', xt[b_][:], xd[tt_ * 128:(tt_ + 1) * 128, :], ('xd%d' % tt_,), ('xt%d' % b_,))
                        act(junk[:], xt[b_][:], AF.Square, ('xt%d' % b_,), ('junk', 'ss%d' % tt_), accum_out=ss[:, tt_:tt_ + 1])
                        act(ss[:, tt_:tt_ + 1], ss[:, tt_:tt_ + 1], AF.Sqrt, ('ss%d' % tt_, 'epsT'), ('ss%d' % tt_,), bias=epsT[:], scale=1.0 / D)
                        rec(ss[:, tt_:tt_ + 1], ss[:, tt_:tt_ + 1], ('ss%d' % tt_,), ('ss%d' % tt_,))
                        ts('dve', xn[b_][:], xt[b_][:], ss[:, tt_:tt_ + 1], None, ALU.mult, None, ('xt%d' % b_, 'ss%d' % tt_), ('xn%d' % b_,))
                        p_, pk = PT_()
                        for kc in range(8):
                            tr(p_[:, kc * 128:(kc + 1) * 128], xn[b_][:, kc * 128:(kc + 1) * 128], ident_b[:], ('xn%d' % b_, 'ident_b'), (pk,))
                        for kc in range(8):
                            ts('dve', hT[:, kc, tt_ * 128:(tt_ + 1) * 128], p_[:, kc * 128:(kc + 1) * 128], s1[:, kc, v:v + 1], s0[:, kc, v:v + 1],
                               ALU.mult, ALU.add, (pk, 's1', 's0'), ('hT',))
                    S.barrier()

            win = I['w_in'][li].rearrange("(kc p) n -> p kc n", p=128)

            def load_w(wt, wkey, view, c0, n, dst0=0):
                S.dma('pool', wt[:, :, dst0:dst0 + n], view[:, :, c0:c0 + n], (), (wkey,))

            def proj_fm(wt, wkey, wc0, consumer, src=None, nk=8, srckey='hT'):
                src = hT if src is None else src
                for (c0, n) in CH:
                    p_, pk = PS()
                    for kc in range(nk):
                        mm(p_[:, 0:n], wt[:, kc, wc0:wc0 + 128], src[:, kc, c0:c0 + n], kc == 0, kc == nk - 1, (wkey,) + (srckey if isinstance(srckey, tuple) else (srckey,)), (pk,))
                    consumer(p_[:, 0:n], pk, c0, n)

            mod_scale_shift('gmix', 1, 0)
            norm_stage()
            if stop_after == 'norm': break
            with ExitStack() as mx:
                rotT = sb('rotT', [128, 128], BF, mx)
                cosT = sb('cosT', [128, L_LAT], BF, mx); sinT = sb('sinT', [128, L_LAT], BF, mx)
                wmask = sb('wmask', [128, 2, 128], BF, mx); namask = sb('namask', [128, NM, 128], BF, mx)
                maskZ = sb('maskZ', [128, 4, 128], F32, mx); maskY = sb('maskY', [128, 4, 128], F32, mx)
                for k_, t_ in [('rotT', rotT), ('cosT', cosT), ('sinT', sinT)]:
                    S.dma('sp', t_[:], I[k_], (), (k_,))
                S.dma('sp', maskZ[:], I['maskZ'].rearrange("g p c -> p g c"), (), ('maskZ',))
                S.dma('sp', maskY[:], I['maskY'].rearrange("g p c -> p g c"), (), ('maskY',))
                S.dma('sp', wmask[:], I['wmask'].rearrange("g p c -> p g c"), (), ('wmask',))
                S.dma('sp', namask[:], I['namask'].rearrange("g p c -> p g c"), (), ('namask',))
                yT0 = sb('yT0', [128, 4, T], BF, mx)
                yTbox = [None]
                class _YT:
                    def __getitem__(self, idx):
                        p, m, sl = idx
                        if isinstance(m, slice):
                            if m.start >= 4: return yTbox[0][p, m.start - 4:m.stop - 4, sl]
                            return yT0[p, m, sl]
                        if m >= 4: return yTbox[0][p, m - 4, sl]
                        return yT0[p, m, sl]
                yT = _YT()
                with ExitStack() as ph:
                    uT = sb('uT', [128, 4, T], BF, ph)
                    acc = sb('acc', [128, 4, T], F32, ph)
                    dsk = sb('dsk', [128, 4], F32, ph)
                    wa_scope = ExitStack()
                    wa = sb('wa', [128, 8, 512], BF, wa_scope)
                    S.dma('sp', dsk[:], I['dskip'][li], (), ('dsk',))
                    load_w(wa, 'wa', win, 0, 512)
                    for m in range(4):
                        def cons(p_, pk, c0, n, m=m):
                            import os
                            dbgv = int(os.environ.get('S5DBG', '0'))
                            if dbgv in (0, 2): cp('act', uT[:, m, c0:c0 + n], p_, (pk,), ('uT%d' % m,))
                            if dbgv in (0, 3): ts('dve', acc[:, m, c0:c0 + n], uT[:, m, c0:c0 + n], dsk[:, m:m + 1], None, ALU.mult, None, ('uT%d' % m, 'dsk'), ('acc%d' % m,))
                        proj_fm(wa, 'wa', m * 128, cons)
                    S.barrier(); wa_scope.close()
                    if stop_after == 's5a': break
                    P = {k_: sb('sp_' + k_, [128, 32], F32, ph) for k_ in ['lr', 'li', 'dt', 'mag', 'c', 's', 't1', 't2', 'ar', 'ai', 'fr', 'fi', 'den']}
                    S.dma('sp', P['lr'][:], I['lamre'][li], (), ('p_lr',)); S.dma('sp', P['li'][:], I['lamim'][li], (), ('p_li',))
                    S.dma('sp', P['dt'][:], I['logdt'][li], (), ('p_dt',))
                    K_ = ('sp',)
                    act(P['dt'][:], P['dt'][:], AF.Exp, ('p_dt',), ('p_dt',))
                    tt('dve', P['t1'][:], P['lr'][:], P['dt'][:], ALU.mult, ('p_lr', 'p_dt'), K_)
                    act(P['mag'][:], P['t1'][:], AF.Exp, K_, K_)
                    tt('dve', P['t2'][:], P['li'][:], P['dt'][:], ALU.mult, ('p_li', 'p_dt'), K_)
                    act(P['s'][:], P['t2'][:], AF.Sin, K_, K_, scale=1.0 / 16)
                    act(P['c'][:], P['t2'][:], AF.Sin, K_ + ('halfpi',), K_, scale=1.0 / 16, bias=halfpi[:])
                    for _ in range(4):
                        tt('dve', P['t1'][:], P['c'][:], P['c'][:], ALU.mult, K_, K_)
                        tt('dve', P['t2'][:], P['s'][:], P['s'][:], ALU.mult, K_, K_)
                        tt('dve', P['s'][:], P['s'][:], P['c'][:], ALU.mult, K_, K_)
                        ts('dve', P['s'][:], P['s'][:], 2.0, None, ALU.mult, None, K_, K_)
                        tt('dve', P['c'][:], P['t1'][:], P['t2'][:], ALU.subtract, K_, K_)
                    tt('dve', P['ar'][:], P['mag'][:], P['c'][:], ALU.mult, K_, K_)
                    tt('dve', P['ai'][:], P['mag'][:], P['s'][:], ALU.mult, K_, K_)
                    tt('dve', P['den'][:], P['lr'][:], P['lr'][:], ALU.mult, K_, K_)
                    tt('dve', P['t1'][:], P['li'][:], P['li'][:], ALU.mult, K_, K_)
                    tt('dve', P['den'][:], P['den'][:], P['t1'][:], ALU.add, K_, K_)
                    rec(P['den'][:], P['den'][:], K_, K_)
                    ts('dve', P['t1'][:], P['ar'][:], -1.0, None, ALU.add, None, K_, K_)
                    tt('dve', P['fr'][:], P['t1'][:], P['lr'][:], ALU.mult, K_, K_)
                    tt('dve', P['t2'][:], P['ai'][:], P['li'][:], ALU.mult, K_, K_)
                    tt('dve', P['fr'][:], P['fr'][:], P['t2'][:], ALU.add, K_, K_)
                    tt('dve', P['fr'][:], P['fr'][:], P['den'][:], ALU.mult, K_, K_)
                    tt('dve', P['fi'][:], P['ai'][:], P['lr'][:], ALU.mult, K_, K_)
                    tt('dve', P['t2'][:], P['t1'][:], P['li'][:], ALU.mult, K_, K_)
                    tt('dve', P['fi'][:], P['fi'][:], P['t2'][:], ALU.subtract, K_, K_)
                    tt('dve', P['fi'][:], P['fi'][:], P['den'][:], ALU.mult, K_, K_)
                    pwr = sb('pwr', [128, 12, 32], F32, ph); pwi = sb('pwi', [128, 12, 32], F32, ph); npwi = sb('npwi', [128, 12, 32], F32, ph)
                    cp('dve', pwr[:, 0, :], P['ar'][:], K_, K_); cp('dve', pwi[:, 0, :], P['ai'][:], K_, K_)
                    for i in range(1, 12):
                        tt('dve', P['t1'][:], pwr[:, i - 1, :], pwr[:, i - 1, :], ALU.mult, K_, K_)
                        tt('dve', P['t2'][:], pwi[:, i - 1, :], pwi[:, i - 1, :], ALU.mult, K_, K_)
                        tt('dve', pwr[:, i, :], P['t1'][:], P['t2'][:], ALU.subtract, K_, K_)
                        tt('dve', P['t1'][:], pwr[:, i - 1, :], pwi[:, i - 1, :], ALU.mult, K_, K_)
                        ts('dve', pwi[:, i, :], P['t1'][:], 2.0, None, ALU.mult, None, K_, K_)
                    ts('dve', npwi[:], pwi[:], -1.0, None, ALU.mult, None, K_, K_)
                    br = sb('br', [128, 32, 16], F32, ph); bi = sb('bi', [128, 32, 16], F32, ph)
                    bbr = sb('bbr', [128, 32, 16], F32, ph); bbi = sb('bbi', [128, 32, 16], F32, ph); tb = sb('tb', [128, 32, 16], F32, ph)
                    S.dma('sp', br[:], I['bre'][li], (), K_); S.dma('sp', bi[:], I['bim'][li], (), K_)
                    frb = bl(P['fr'][:], 16); fib = bl(P['fi'][:], 16)
                    tt('dve', bbr[:], br[:], frb, ALU.mult, K_, K_); tt('dve', tb[:], bi[:], fib, ALU.mult, K_, K_)
                    tt('dve', bbr[:], bbr[:], tb[:], ALU.subtract, K_, K_)
                    tt('dve', bbi[:], bi[:], frb, ALU.mult, K_, K_); tt('dve', tb[:], br[:], fib, ALU.mult, K_, K_)
                    tt('dve', bbi[:], bbi[:], tb[:], ALU.add, K_, K_)
                    cn_r = sb('cn_r', [128, 8, 64], F32, ph); cn_i = sb('cn_i', [128, 8, 64], F32, ph)
                    S.dma('sp', cn_r[:], I['cre'][li], (), K_); S.dma('sp', cn_i[:], I['cim'][li], (), K_)
                    ts('dve', cn_i[:], cn_i[:], -1.0, None, ALU.mult, None, K_, K_)
                    Zt = sb('Zt', [128, 128], F32, ph)
                    BTr = sb('BTr', [128, 128], BF, ph); BTi = sb('BTi', [128, 128], BF, ph)
                    CTr = sb('CTr', [128, 128], F32, ph); CTi = sb('CTi', [128, 128], F32, ph)
                    KA = [sb('ks%d' % i, [128, T], F32, ph) for i in range(4)]
                    S.barrier()
                    if stop_after == 's5b': break
                    for d_ in range(2):
                        if stop_after == 's5c' and d_ == 1: break
                        for gp in range(16):
                            if stop_after == 's5c' and gp == 1: break
                            col = d_ * 16 + gp; tile_ = gp // 4; gl = gp % 4
                            for src_, dst_, dk in [(bbr, BTr, 'BTr'), (bbi, BTi, 'BTi')]:
                                tt('dve', Zt[:].rearrange("p (a q) -> p a q", q=16), bmid(src_[:, col, :], 8),
                                   maskZ[:, gl, :].rearrange("p (a q) -> p a q", q=16), ALU.mult, K_ + ('maskZ',), ('Zt',))
                                p_, pk = PS()
                                tr(p_[:, 0:128], Zt[:], ident_f[:], ('Zt', 'ident_f'), (pk,))
                                cp('act', dst_[:], p_[:, 0:128], (pk,), (dk,))
                            for src_, dst_, dk in [(cn_r, CTr, 'CTr'), (cn_i, CTi, 'CTi')]:
                                tt('dve', Zt[:].rearrange("p (a n) -> p a n", n=64), bmid(src_[:, d_ * 4 + tile_, :], 2),
                                   maskY[:, gl, :].rearrange("p (a n) -> p a n", n=64), ALU.mult, K_ + ('maskY',), ('Zt',))
                                p_, pk = PS()
                                tr(p_[:, 0:128], Zt[:], ident_f[:], ('Zt', 'ident_f'), (pk,))
                                cp('act', dst_[:], p_[:, 0:128], (pk,), (dk,))
                            def pos(c0, n):
                                if d_ == 0: return c0
                                return (c0 - C_CTX) if c0 >= C_CTX else L_LAT
                            for (c0, n) in CH:
                                for lh, dst_, lk, dk in [(BTr, KA[0], 'BTr', 'ka0'), (BTi, KA[1], 'BTi', 'ka1')]:
                                    p_, pk = PS()
                                    mm(p_[:, 0:n], lh[:], uT[:, tile_, c0:c0 + n], True, True, (lk, 'uT%d' % tile_), (pk,))
                                    cp('act', dst_[:, pos(c0, n):pos(c0, n) + n], p_[:, 0:n], (pk,), (dk,))
                            cur = 0
                            for i in range(12):
                                k = 1 << i
                                sr, si = KA[cur], KA[cur + 1]; dr_, di_ = KA[2 - cur], KA[3 - cur]
                                skr, ski = 'ka%d' % cur, 'ka%d' % (cur + 1); dkr, dki = 'ka%d' % (2 - cur), 'ka%d' % (3 - cur)
                                cr = pwr[:, i, col:col + 1]; ci = pwi[:, i, col:col + 1]; nci = npwi[:, i, col:col + 1]
                                if d_ == 0: dst_sl = slice(k, T); src_sl = slice(0, T - k); keep = slice(0, k)
                                else: dst_sl = slice(0, T - k); src_sl = slice(k, T); keep = slice(T - k, T)
                                stt(dr_[:, dst_sl], sr[:, src_sl], cr, sr[:, dst_sl], ALU.mult, ALU.add, (skr,) + K_, (dkr,))
                                stt(dr_[:, dst_sl], si[:, src_sl], nci, dr_[:, dst_sl], ALU.mult, ALU.add, (ski, dkr) + K_, (dkr,))
                                stt(di_[:, dst_sl], si[:, src_sl], cr, si[:, dst_sl], ALU.mult, ALU.add, (ski,) + K_, (dki,))
                                stt(di_[:, dst_sl], sr[:, src_sl], ci, di_[:, dst_sl], ALU.mult, ALU.add, (skr, dki) + K_, (dki,))
                                cp('pool', dr_[:, keep], sr[:, keep], (skr,), (dkr,))
                                cp('pool', di_[:, keep], si[:, keep], (ski,), (dki,))
                                cur = 2 - cur
                            hr, hi = KA[cur], KA[cur + 1]; hkr, hki = 'ka%d' % cur, 'ka%d' % (cur + 1)
                            for (c0, n) in CH:
                                p_, pk = PS()
                                mm(p_[:, 0:n], CTr[:], hr[:, pos(c0, n):pos(c0, n) + n], True, False, ('CTr', hkr), (pk,))
                                mm(p_[:, 0:n], CTi[:], hi[:, pos(c0, n):pos(c0, n) + n], False, True, ('CTi', hki), (pk,))
                                tt('dve', acc[:, tile_, c0:c0 + n], acc[:, tile_, c0:c0 + n], p_[:, 0:n], ALU.add, (pk, 'acc%d' % tile_), ('acc%d' % tile_,))
                            S.flush()
                    if dbg and 'acc' in dbg_out and li == 0:
                        S.dma('sp', dbg_out['acc'].rearrange("(m p) t -> p (m t)", p=128), acc[:].rearrange("p m t -> p (m t)"), ['acc%d' % m for m in range(4)], ())
                    wg = sb('wg', [128, 4, 512], BF, ph)
                    S.dma('pool', wg[:], I['ssm_w_glu'][li].rearrange("(kc p) n -> p kc n", p=128), (), ('wg',))
                    t3 = KA[0]; gT = uT
                    for m in range(4):
                        a_ = acc[:, m, :]
                        act(t3[:], a_, AF.Square, ('acc%d' % m,), ('ka0',))
                        ts('dve', t3[:], t3[:], 0.044715, 1.0, ALU.mult, ALU.add, ('ka0',), ('ka0',))
                        tt('dve', t3[:], t3[:], a_, ALU.mult, ('ka0', 'acc%d' % m), ('ka0',))
                        act(t3[:], t3[:], AF.Sigmoid, ('ka0',), ('ka0',), scale=1.5957691216057308)
                        tt('dve', gT[:, m, :], t3[:], a_, ALU.mult, ('ka0', 'acc%d' % m), ('uT%d' % m,))
                    for m in range(4):
                        def cons(p_, pk, c0, n, m=m):
                            act(KA[1][:, c0:c0 + n], p_, AF.Sigmoid, (pk,), ('ka1',))
                            tt('dve', yT[:, m, c0:c0 + n], gT[:, m, c0:c0 + n], KA[1][:, c0:c0 + n], ALU.mult, ('ka1', 'uT%d' % m), ('yT%d' % m,))
                        proj_fm(wg, 'wg', m * 128, cons, src=gT, nk=4, srckey=('uT0', 'uT1', 'uT2', 'uT3'))
                    S.barrier()

                if stop_after == 's5': break
                yTbox[0] = sb('yT2', [128, 8, T], BF, mx)
                def qk_prep(dst, dkey, wt, wkey, wc0, gcol_ap, gkey, rope, tmp):
                    raw, sq, rs, qn, t1 = tmp
                    for (c0, n) in CH:
                        p_, pk = PS()
                        for kc in range(8):
                            mm(p_[:, 0:n], wt[:, kc, wc0:wc0 + 128], hT[:, kc, c0:c0 + n], kc == 0, kc == 7, (wkey, 'hT'), (pk,))
                        cp('act', raw[:, 0:n], p_[:, 0:n], (pk,), ('raw',))
                        act(sq[:, 0:n], p_[:, 0:n], AF.Square, (pk,), ('sq',))
                        p2, pk2 = PS()
                        mm(p2[:, 0:n], blockones[:], sq[:, 0:n], True, True, ('blockones', 'sq'), (pk2,))
                        act(rs[:, 0:n], p2[:, 0:n], AF.Sqrt, (pk2, 'epsT'), ('rs',), bias=epsT[:], scale=1.0 / 64)
                        rec(rs[:, 0:n], rs[:, 0:n], ('rs',), ('rs',))
                        if not (rope and c0 >= C_CTX):
                            stt(dst[:, c0:c0 + n], raw[:, 0:n], gcol_ap, rs[:, 0:n], ALU.mult, ALU.mult, ('raw', 'rs', gkey), (dkey,))
                        else:
                            l0 = c0 - C_CTX
                            stt(qn[:, 0:n], raw[:, 0:n], gcol_ap, rs[:, 0:n], ALU.mult, ALU.mult, ('raw', 'rs', gkey), ('qn',))
                            p3, pk3 = PS()
                            mm(p3[:, 0:n], rotT[:], qn[:, 0:n], True, True, ('rotT', 'qn'), (pk3,))
                            tt('dve', t1[:, 0:n], qn[:, 0:n], cosT[:, l0:l0 + n], ALU.mult, ('qn', 'cosT'), ('t1',))
                            tt('dve', raw[:, 0:n], p3[:, 0:n], sinT[:, l0:l0 + n], ALU.mult, (pk3, 'sinT'), ('raw',))
                            tt('pool', dst[:, c0:c0 + n], t1[:, 0:n], raw[:, 0:n], ALU.add, ('t1', 'raw'), (dkey,))

                def attention(kind, hg):
                    with ExitStack() as ph:
                        tmp = (sb('raw', [128, 512], F32, ph), sb('sq', [128, 512], BF, ph), sb('rs', [128, 512], F32, ph),
                               sb('qn', [128, 512], BF, ph), sb('t1', [128, 512], F32, ph))
                        gq = sb('gq', [128, 1], F32, ph); gk = sb('gk', [128, 1], F32, ph)
                        S.dma('sp', gq[:], I['gwq' if kind == 'w' else 'gnq'][li], (), ('gq',))
                        S.dma('sp', gk[:], I['gwk' if kind == 'w' else 'gnk'][li], (), ('gk',))
                        ts('dve', gq[:], gq[:], 0.125, None, ALU.mult, None, ('gq',), ('gq',))
                        wq_ = sb('wq_', [128, 8, 256], BF, ph)
                        qT = sb('qT', [128, 2, T], BF, ph)
                        nkt = 1 if kind == 'w' else 2
                        nv = 1 if kind == 'w' else 4
                        wk_ = sb('wk_', [128, 8, 128 * nkt], BF, ph); kT = sb('kT', [128, nkt, T], BF, ph)
                        wv_ = sb('wv_', [128, 8, 64 * nv], BF, ph); vaug = sb('vaug', [128, NT, nv, 65], BF, ph)
                        if kind == 'w':
                            load_w(wq_, 'wq_', win, 512 + 256 * hg, 256)
                            load_w(wk_, 'wk_', win, 1024 + 64 * hg, 64, 0); load_w(wk_, 'wk_', win, 1024 + 64 * hg, 64, 64)
                            load_w(wv_, 'wv_', win, 1152 + 64 * hg, 64)
                        else:
                            load_w(wq_, 'wq_', win, 1280 + 256 * hg, 256)
                            load_w(wk_, 'wk_', win, 1792 + 256 * hg, 256)
                            load_w(wv_, 'wv_', win, 2304 + 256 * hg, 256)
                        for m in range(2):
                            qk_prep(qT[:, m, :], 'qT', wq_, 'wq_', m * 128, gq[:, 0:1], 'gq', kind == 'w', tmp)
                        for m in range(nkt):
                            qk_prep(kT[:, m, :], 'kT', wk_, 'wk_', m * 128, gk[:, 0:1], 'gk', kind == 'w', tmp)
                        kTm = sb('kTm', [128, nkt, 2, T], BF, ph)
                        for m in range(nkt):
                            for par in range(2):
                                ts('dve', kTm[:, m, par, :], kT[:, m, :], hm[:, par:par + 1], None, ALU.mult, None, ('kT', 'hm'), ('kTm',))
                        ms('pool', vaug[:, :, :, 64:65], 1.0, ('vaug',))
                        for t_ in range(NT):
                            p_, pk = PS()
                            for kc in range(8):
                                mm(p_[:, 0:64 * nv], hT[:, kc, t_ * 128:(t_ + 1) * 128], wv_[:, kc, :], kc == 0, kc == 7, ('hT', 'wv_'), (pk,))
                            cp('act', vaug[:, t_, :, 0:64], p_[:, 0:64 * nv].rearrange("p (v d) -> p v d", d=64), (pk,), ('vaug',))
                        esink = sb('esink', [128, 8], F32, ph)
                        if kind == 'w':
                            S.dma('sp', esink[:], I['sink'][li], (), ('esink',))
                            act(esink[:], esink[:], AF.Exp, ('esink',), ('esink',))
                        else:
                            nab = sb('nab', [128, 7, 4, 128], BF, ph)
                            for di in range(7):
                                S.dma('pool', nab[:, di, :, :], I['nabias'][li, di, :, 4 * hg:4 * hg + 4, :], (), ('nab',))
                        import os
                        adbg = int(os.environ.get('ATTDBG', '0'))
                        if adbg == 1:
                            S.barrier(); return
                        NSLOT = 7
                        PTs = sb('PTs', [128, NSLOT, 4, 128], BF, ph)
                        ytok = sb('ytok', [128, 4, 64], BF, ph); tot = sb('tot', [128, 4], F32, ph)
                        S.flush()
                        ybase = (4 if kind == 'w' else 8) + 2 * hg
                        for tq in range(NT):
                            keys = []
                            if tq < 2: keys = [(0, None, None), (1, None, None)]
                            else:
                                i = tq - 2
                                keys = [(0, None, None), (1, None, None)]
                                if kind == 'w':
                                    for j in (i - 1, i, i + 1):
                                        if 0 <= j < 16:
                                            keys.append((2 + j, None if j == i else (wmask[:, 0, :] if j < i else wmask[:, 1, :]), None))
                                else:
                                    for j in na_tiles(i):
                                        keys.append((2 + j, namask[:, nidx[(i, j)], :], nab[:, j - i + 3, :, :]))
                            assert len(keys) <= NSLOT
                            for sl, (tk, mask, bias) in enumerate(keys):
                                p_, pk = PS()
                                if bias is not None:
                                    mm(p_[:, 0:512], ident_b[:], bias.rearrange("p h q -> p (h q)"), True, False, ('ident_b', 'nab'), (pk,))
                                for h in range(4):
                                    if adbg == 4 and h % 2 == 1: continue
                                    half = (h % 2) * 64
                                    kt = kTm[:, 0 if kind == 'w' else h // 2, h % 2, tk * 128:(tk + 1) * 128]
                                    mm(p_[:, h * 128:(h + 1) * 128], kt, qT[:, h // 2, tq * 128:(tq + 1) * 128],
                                       bias is None, h == 3 or bias is None, ('kTm', 'qT'), (pk,))
                                act(PTs[:, sl, :, :].rearrange("p h q -> p (h q)"), p_[:, 0:512], AF.Exp, (pk,), ('PT%d' % sl,))
                                if mask is not None and adbg not in (3, 4):
                                    tt('pool', PTs[:, sl, :, :], PTs[:, sl, :, :], bmid(mask, 4), ALU.mult, ('PT%d' % sl, 'wmask', 'namask'), ('PT%d' % sl,))
                            if adbg in (2, 3, 4):
                                continue
                            po, pok = PS()
                            for h in range(4):
                                for sl, (tk, mask, bias) in enumerate(keys):
                                    mm(po[:, h * 65:(h + 1) * 65], PTs[:, sl, h, :], vaug[:, tk, 0 if kind == 'w' else h, :],
                                       sl == 0, sl == len(keys) - 1, ('PT%d' % sl, 'vaug'), (pok,))
                            pov = po[:, 0:260].rearrange("p (h e) -> p h e", e=65)
                            if kind == 'w':
                                tt('dve', tot[:], pov[:, :, 64], esink[:, 4 * hg:4 * hg + 4], ALU.add, (pok, 'esink'), ('tot',))
                            else:
                                cp('dve', tot[:], pov[:, :, 64], (pok,), ('tot',))
                            rec(tot[:], tot[:], ('tot',), ('tot',))
                            tt('dve', ytok[:], pov[:, :, 0:64], bl(tot[:], 64), ALU.mult, (pok, 'tot'), ('ytok',))
                            pt_, ptk = PT_()
                            for m in range(2):
                                tr(pt_[:, m * 128:(m + 1) * 128], ytok[:, 2 * m:2 * m + 2, :].rearrange("p h d -> p (h d)"), ident_b[:], ('ytok', 'ident_b'), (ptk,))
                            cp('act', yT[:, ybase:ybase + 2, tq * 128:(tq + 1) * 128], pt_[:, 0:256].rearrange("p (m t) -> p m t", t=128), (ptk,), ('yT%d' % ybase, 'yT%d' % (ybase + 1)))
                            if tq % 6 == 5: S.flush()
                        S.barrier()

                for kind in ('w', 'n'):
                    for hg in range(2):
                        attention(kind, hg)
                if dbg and 'yT' in dbg_out and li == 0:
                    with ExitStack() as ph:
                        yf = sb('yf', [128, 12, T], F32, ph) if False else None

                if stop_after == 'attn': break
                with ExitStack() as ph:
                    modb = sb('modb', [128, 2, D], F32, ph)
                    mod_bcast(2, modb)
                    wbr = sb('wbr', [128, 12, D], BF, ph)
                    S.dma('pool', wbr[:], I['w_branch'][li].rearrange("i (kc p) d -> p (i kc) d", p=128), (), ('wbr',))
                    wo = sb('wo', [128, 8, D], BF, ph)
                    S.dma('pool', wo[:], I['w_out'][li].rearrange("(kc p) d -> p kc d", p=128), (), ('wo',))
                    wgt = [sb('wgt%d' % i, [128, 8, 384], BF, ph) for i in range(2)]
                    sT = sb('sT', [128, 8, 512], BF, ph)
                    sg = [sb('sg%d' % i, [128, 512], F32, ph) for i in range(3)]
                    xr = [sb('xr%d' % i, [128, D], F32, ph) for i in range(2)]
                    tmpm = sb('tmpm', [128, 512], F32, ph)
                    it = 0
                    for (c0, n) in CH:
                        for dt_ in range(8):
                            w = wgt[it % 2]; wk = 'wgt%d' % (it % 2); it += 1
                            for i in range(3):
                                load_w(w, wk + '_%d' % i, win, 2816 + i * 1024 + dt_ * 128, 128, i * 128)
                            for i in range(3):
                                pg, pgk = PS()
                                for kc in range(8):
                                    mm(pg[:, 0:n], w[:, kc, i * 128:(i + 1) * 128], hT[:, kc, c0:c0 + n], kc == 0, kc == 7, (wk + '_%d' % i, 'hT'), (pgk,))
                                act(sg[i][:, 0:n], pg[:, 0:n], AF.Sigmoid, (pgk,), ('sg%d' % i,))
                                pp, ppk = PS()
                                for kc in range(4):
                                    mm(pp[:, 0:n], wbr[:, i * 4 + kc, dt_ * 128:(dt_ + 1) * 128], yT[:, i * 4 + kc, c0:c0 + n], kc == 0, kc == 3,
                                       ('wbr',) + tuple('yT%d' % q for q in range(12)), (ppk,))
                                tt('dve', sg[i][:, 0:n], sg[i][:, 0:n], pp[:, 0:n], ALU.mult, (ppk, 'sg%d' % i), ('sg%d' % i,))
                            tt('pool', sg[0][:, 0:n], sg[0][:, 0:n], sg[1][:, 0:n], ALU.add, ('sg0', 'sg1'), ('sg0',))
                            tt('pool', sT[:, dt_, 0:n], sg[0][:, 0:n], sg[2][:, 0:n], ALU.add, ('sg0', 'sg2'), ('sT',))
                        for tl_ in range(n // 128):
                            tt_ = c0 // 128 + tl_; b_ = tt_ % 2; v = 1 if tt_ < 2 else 0
                            S.dma('sp', xr[b_][:], xd[tt_ * 128:(tt_ + 1) * 128, :], ('xd%d' % tt_,), ('xr%d' % b_,))
                            for hf in range(2):
                                p_, pk = PS()
                                for kc in range(8):
                                    mm(p_[:, 0:512], sT[:, kc, tl_ * 128:(tl_ + 1) * 128], wo[:, kc, hf * 512:(hf + 1) * 512], kc == 0, kc == 7, ('sT', 'wo'), (pk,))
                                tt('dve', tmpm[:], p_[:, 0:512], modb[:, v, hf * 512:(hf + 1) * 512], ALU.mult, (pk, 'modb'), ('tmpm',))
                                tt('pool', xr[b_][:, hf * 512:(hf + 1) * 512], xr[b_][:, hf * 512:(hf + 1) * 512], tmpm[:], ALU.add, ('tmpm', 'xr%d' % b_), ('xr%d' % b_,))
                            S.dma('sp', xd[tt_ * 128:(tt_ + 1) * 128, :], xr[b_][:], ('xr%d' % b_,), ('xd%d' % tt_,))
                    S.barrier()

            if stop_after == 'merge': break
            mod_scale_shift('gffn', 4, 3)
            norm_stage()
            with ExitStack() as ph:
                macc = sb('macc', [128, NT, D], F32, ph)
                G = sb('G', [128, NT, NE], F32, ph)
                with ExitStack() as ph1:
                    rw = sb('rw', [128, 8, NE], BF, ph1); rb = sb('rb', [128, NE], F32, ph1)
                    S.dma('pool', rw[:], I['router_w'][li].rearrange("(kc p) e -> p kc e", p=128), (), ('rw',))
                    S.dma('sp', rb[:], I['rbias'][li], (), ('rb',))
                    lg = sb('lg', [128, NE], F32, ph1); m8 = sb('m8', [128, 8], F32, ph1); mk = sb('mk', [128, NE], F32, ph1)
                    nmx = sb('nmx', [128, 1], F32, ph1); sm = sb('sm', [128, 1], F32, ph1)
                    ms('pool', macc[:], 0.0, tuple('macc%d' % t for t in range(NT)))
                    for t_ in range(NT):
                        p_, pk = PS()
                        for kc in range(8):
                            mm(p_[:, 0:NE], hT[:, kc, t_ * 128:(t_ + 1) * 128], rw[:, kc, :], kc == 0, kc == 7, ('hT', 'rw'), (pk,))
                        tt('dve', lg[:], p_[:, 0:NE], rb[:], ALU.add, (pk, 'rb'), ('lg',))
                        S.add('dve', lambda e: e.max(out=m8[:], in_=lg[:]), ('lg',), ('m8',))
                        ts('dve', mk[:], lg[:], m8[:, 3:4], None, ALU.is_ge, None, ('lg', 'm8'), ('mk',))
                        ts('dve', nmx[:], m8[:, 0:1], -1.0, None, ALU.mult, None, ('m8',), ('nmx',))
                        act(lg[:], lg[:], AF.Exp, ('lg', 'nmx'), ('lg',), bias=nmx[:])
                        tt('dve', lg[:], lg[:], mk[:], ALU.mult, ('lg', 'mk'), ('lg',))
                        S.add('dve', lambda e: e.reduce_sum(out=sm[:], in_=lg[:], axis=mybir.AxisListType.X), ('lg',), ('sm',))
                        rec(sm[:], sm[:], ('sm',), ('sm',))
                        ts('dve', G[:, t_, :], lg[:], sm[:, 0:1], None, ALU.mult, None, ('lg', 'sm'), ('G',))
                    S.barrier()
                with ExitStack() as ph2:
                    wgu = [sb('wgu%d' % i, [128, 8, 2 * D], BF, ph2) for i in range(2)]
                    wdn = sb('wdn', [128, 8, D], BF, ph2)
                    bgu = sb('bgu', [128, NE, 16], F32, ph2)
                    S.dma('sp', bgu[:], I['bguT'][li], (), ('bgu',))
                    ts('dve', bgu[:, :, 8:16], bgu[:, :, 8:16], 1.0, None, ALU.add, None, ('bgu',), ('bgu',))
                    actT = [sb('actT%d' % i, [128, 8, 512], BF, ph2) for i in range(2)]
                    g1 = [sb('g1%d' % i, [128, 512], F32, ph2) for i in range(2)]; sgm = sb('sgm', [128, 512], BF, ph2); u1 = sb('u1', [128, 512], F32, ph2)
                    CHM = [(256 + 512 * j, 512) for j in range(4)] + [(0, 256)]

                    def load_gu(e_):
                        wgv = I['w_gate_up'][li, e_].rearrange("(kc p) n -> p kc n", p=128)
                        for kc in range(8):
                            S.dma('pool', wgu[e_ % 2][:, kc, :], wgv[:, kc, :], (), ('wgu%d_%d' % (e_ % 2, kc),))

                    def load_dn(e_):
                        S.dma('pool', wdn[:], I['w_down'][li, e_].rearrange("(kc p) n -> p kc n", p=128), (), ('wdn',))

                    def GU(e_, ci):
                        c0, n = CHM[ci]; a_ = actT[ci % 2]; ak = 'actT%d' % (ci % 2)
                        w = wgu[e_ % 2]; wk = 'wgu%d' % (e_ % 2)
                        for m in range(8):
                            pg, pgk = PS()
                            for kc in range(8):
                                mm(pg[:, 0:n], w[:, kc, m * 128:(m + 1) * 128], hT[:, kc, c0:c0 + n], kc == 0, kc == 7, (wk + '_%d' % kc, 'hT'), (pgk,))
                            pu, puk = PS()
                            for kc in range(8):
                                mm(pu[:, 0:n], w[:, kc, D + m * 128:D + (m + 1) * 128], hT[:, kc, c0:c0 + n], kc == 0, kc == 7, (wk + '_%d' % kc, 'hT'), (puk,))
                            b_ = m % 2
                            ts('dve', g1[b_][:, 0:n], pg[:, 0:n], bgu[:, e_, m:m + 1], 7.0, ALU.add, ALU.min, (pgk, 'bgu'), ('g1%d' % b_,))
                            act(sgm[:, 0:n], g1[b_][:, 0:n], AF.Sigmoid, ('g1%d' % b_,), ('sgm',), scale=1.702)
                            if m > 0:
                                q_ = (m - 1) % 2
                                stt(a_[:, m - 1, 0:n], u1[:, 0:n], -6.0, g1[q_][:, 0:n], ALU.max, ALU.mult, ('u1', 'g1%d' % q_), (ak,))
                            ts('dve', u1[:, 0:n], pu[:, 0:n], bgu[:, e_, 8 + m:9 + m], 8.0, ALU.add, ALU.min, (puk, 'bgu'), ('u1',))
                            tt('pool', g1[b_][:, 0:n], g1[b_][:, 0:n], sgm[:, 0:n], ALU.mult, ('g1%d' % b_, 'sgm'), ('g1%d' % b_,))
                        stt(a_[:, 7, 0:n], u1[:, 0:n], -6.0, g1[1][:, 0:n], ALU.max, ALU.mult, ('u1', 'g11'), (ak,))

                    def DN(e_, ci):
                        c0, n = CHM[ci]; a_ = actT[ci % 2]; ak = 'actT%d' % (ci % 2)
                        for tl_ in range(n // 128):
                            tt_ = c0 // 128 + tl_
                            for hf in range(2):
                                p_, pk = PS()
                                for kc in range(8):
                                    mm(p_[:, 0:512], a_[:, kc, tl_ * 128:(tl_ + 1) * 128], wdn[:, kc, hf * 512:(hf + 1) * 512], kc == 0, kc == 7, (ak, 'wdn'), (pk,))
                                sl_ = macc[:, tt_, hf * 512:(hf + 1) * 512]
                                stt(sl_, p_[:, 0:512], G[:, tt_, e_:e_ + 1], sl_, ALU.mult, ALU.add, (pk, 'G', 'macc%d' % tt_), ('macc%d' % tt_,))

                    load_gu(0); load_dn(0)
                    for e_ in range(NE):
                        if e_ + 1 < NE: load_gu(e_ + 1)
                        for ci in range(5):
                            GU(e_, ci)
                            if ci > 0: DN(e_, ci - 1)
                        DN(e_, 4)
                        if e_ + 1 < NE: load_dn(e_ + 1)
                        S.flush()
                    S.barrier()
                with ExitStack() as ph3:
                    modb = sb('modb5', [128, 2, D], F32, ph3)
                    mod_bcast(5, modb)
                    xr = [sb('xq%d' % i, [128, D], F32, ph3) for i in range(2)]
                    bdn = sb('bdn', [32, D], F32, ph3); GT = sb('GT', [32, 128], F32, ph3)
                    S.dma('sp', bdn[:], I['b_down'][li], (), ('bdn',))
                    for t_ in range(NT):
                        b_ = t_ % 2; v = 1 if t_ < 2 else 0
                        pg_, pgk_ = PS()
                        tr(pg_[0:32, 0:128], G[:, t_, :], ident_f[:], ('G', 'ident_f'), (pgk_,))
                        cp('act', GT[:], pg_[0:32, 0:128], (pgk_,), ('GT',))
                        for hf in range(2):
                            pb_, pbk_ = PS()
                            mm(pb_[:, 0:512], GT[:], bdn[:, hf * 512:(hf + 1) * 512], True, True, ('GT', 'bdn'), (pbk_,))
                            tt('dve', macc[:, t_, hf * 512:(hf + 1) * 512], macc[:, t_, hf * 512:(hf + 1) * 512], pb_[:, 0:512], ALU.add, (pbk_, 'macc%d' % t_), ('macc%d' % t_,))
                        S.dma('sp', xr[b_][:], xd[t_ * 128:(t_ + 1) * 128, :], ('xd%d' % t_,), ('xq%d' % b_,))
                        tt('dve', macc[:, t_, :], macc[:, t_, :], modb[:, v, :], ALU.mult, ('macc%d' % t_, 'modb'), ('macc%d' % t_,))
                        tt('pool', xr[b_][:], xr[b_][:], macc[:, t_, :], ALU.add, ('xq%d' % b_, 'macc%d' % t_), ('xq%d' % b_,))
                        if li == nlayers - 1:
                            if t_ >= 2:
                                S.dma('sp', yout[(t_ - 2) * 128:(t_ - 1) * 128, :], xr[b_][:], ('xq%d' % b_,), ('yout%d' % t_,))
                        else:
                            S.dma('sp', xd[t_ * 128:(t_ + 1) * 128, :], xr[b_][:], ('xq%d' % b_,), ('xd%d' % t_,))
                    S.barrier()
        S.barrier()
    return nc, consts, list(I.keys())


def make_in_maps(inputs, cores, nc_consts, names=None):
    maps = []
    big = {k_: np.ascontiguousarray(inputs[k_], dtype=np.float32) for k_ in BIG if names is None or k_ in names}
    for b in cores:
        m = {'x': np.ascontiguousarray(inputs['x'][b]), 'ctx': np.ascontiguousarray(inputs['ctx'][b])}
        m.update(nc_consts)
        m.update(host_layout(inputs, b))
        m.update(big)
        if names is not None: m = {k_: v for k_, v in m.items() if k_ in names}
        maps.append(m)
    return maps


def kernel(**inputs):
    inputs = {k_: np.asarray(v) for k_, v in inputs.items()}
    nc, consts, names = build(DEPTH)
    maps = make_in_maps(inputs, list(range(8)), consts, names)
    res = run_bass_kernel_spmd(nc, maps, core_ids=list(range(8)))
    return np.stack([r['y'] for r in res.results], 0).astype(np.float32)
```

```python
import numpy as np
from contextlib import ExitStack
import concourse.bass as bass
import concourse.mybir as mybir
from concourse.bass_utils import run_bass_kernel_spmd

F32 = mybir.dt.float32
BF = mybir.dt.bfloat16
ALU = mybir.AluOpType
AF = mybir.ActivationFunctionType

D = 1024; L_LAT = 2048; C_CTX = 256; T = 2304; NT = 18; DEPTH = 4
NE = 32; INW = 5888
CH = [(0, 256)] + [(256 + 512 * j, 512) for j in range(4)]
EPS = 1e-6
ND = 12
SAME_ENGINE_SYNC = True


def bl(ap, n):
    return bass.AP(ap.tensor, ap.offset, list(ap.ap) + [(0, n)])


def bmid(ap, n):
    a = list(ap.ap)
    return bass.AP(ap.tensor, ap.offset, [a[0], (0, n)] + a[1:])


class Sched:
    def __init__(s, nc, block, stack):
        s.nc = nc; s.block = block
        s.engs = {'pe': nc.tensor, 'act': nc.scalar, 'dve': nc.vector, 'pool': nc.gpsimd, 'sp': nc.sync}
        s.tl = {e: stack.enter_context(nc.semaphore('tl_' + e)) for e in ['pe', 'act', 'dve', 'pool']}
        s.cnt = {e: 0 for e in s.tl}
        s.dsem = {q: [stack.enter_context(nc.semaphore('d_%s%d' % (q, i))) for i in range(ND)] for q in ['sp', 'pool']}
        s.dval = {q: [0] * ND for q in s.dsem}; s.dnext = {q: 0 for q in s.dsem}
        s.waited = {e: {} for e in s.engs}
        s.lw = {}; s.rd = {}
        s.pend = {e: [] for e in s.engs}
        s.n = 0

    def _waits(s, eng, R, W):
        deps = {}
        def need(tok):
            if tok is None: return
            name, sem, val, src = tok
            if src == eng and (eng in ('pe', 'sp') or not SAME_ENGINE_SYNC): return
            if deps.get(name, (None, 0))[1] < val: deps[name] = (sem, val)
        for k in R: need(s.lw.get(k))
        for k in W:
            need(s.lw.get(k))
            for t in s.rd.get(k, ()): need(t)
        out = []
        for name, (sem, val) in deps.items():
            if s.waited[eng].get(name, 0) < val:
                s.waited[eng][name] = val; out.append((sem, val))
        return out

    def _mark(s, tok, R, W):
        for k in R: s.rd.setdefault(k, []).append(tok)
        for k in W: s.lw[k] = tok; s.rd[k] = []

    def add(s, eng, fn, R=(), W=()):
        waits = s._waits(eng, R, W)
        s.cnt[eng] += 1
        tok = ('tl_' + eng, s.tl[eng], s.cnt[eng], eng)
        s.pend[eng].append((waits, fn, (s.tl[eng], 1)))
        s._mark(tok, R, W); s.n += 1

    def dma(s, q, out, in_, R=(), W=(), **kw):
        waits = s._waits(q, R, W)
        i = s.dnext[q]; s.dnext[q] = (i + 1) % ND
        sem = s.dsem[q][i]; name = 'd_%s%d' % (q, i)
        if s.dval[q][i] > s.waited[q].get(name, 0):
            s.waited[q][name] = s.dval[q][i]; waits.append((sem, s.dval[q][i]))
        s.dval[q][i] += 16
        tok = (name, sem, s.dval[q][i], 'dma')
        s.pend[q].append((waits, lambda e: e.dma_start(out=out, in_=in_, **kw), (sem, 16)))
        s._mark(tok, R, W); s.n += 1

    def barrier(s):
        for eng in s.engs:
            waits = []
            for e2 in s.tl:
                name = 'tl_' + e2
                if s.cnt[e2] > s.waited[eng].get(name, 0) and not (e2 == eng == 'pe'):
                    s.waited[eng][name] = s.cnt[e2]; waits.append((s.tl[e2], s.cnt[e2]))
            for q in s.dsem:
                for i in range(ND):
                    name = 'd_%s%d' % (q, i)
                    if s.dval[q][i] > s.waited[eng].get(name, 0):
                        s.waited[eng][name] = s.dval[q][i]; waits.append((s.dsem[q][i], s.dval[q][i]))
            if waits: s.pend[eng].append((waits, None, None))
        s.flush()

    def wait_all(s, eng, keys):
        waits = s._waits(eng, keys, ())
        s.pend[eng].append((waits, None, None))

    def flush(s):
        for ename, lst in s.pend.items():
            if not lst: continue
            def body(e, lst=lst):
                for waits, fn, inc in lst:
                    for sem, val in waits: e.wait_ge(sem, val)
                    if fn is not None:
                        ins = fn(e)
                        ins.then_inc(inc[0], inc[1])
            getattr(s.block, {'pe': 'tensor', 'act': 'scalar', 'dve': 'vector', 'pool': 'gpsimd', 'sp': 'sync'}[ename])(body)
            s.pend[ename] = []


def na_start(r): return min(max(r - 4, 0), 24)

def na_tiles(i):
    rows = set()
    for r in (2 * i, 2 * i + 1):
        st = na_start(r); rows.update(range(st, st + 8))
    return sorted(set(r // 2 for r in rows))

def na_mask(i, j):
    m = np.zeros((128, 128), np.float32)
    k = np.arange(128); rk = k // 64; ck = k % 64
    for q in range(128):
        rq = q // 64; cq = q % 64
        st = na_start(2 * i + rq)
        row_ok = (2 * j + rk >= st) & (2 * j + rk <= st + 7)
        cs = min(max(cq - 8, 0), 48)
        col_ok = (ck >= cs) & (ck < cs + 16)
        m[:, q] = (row_ok & col_ok)
    return m

_NA_MASKS = None
def na_mask_table():
    global _NA_MASKS
    if _NA_MASKS is None:
        uniq = []; idx = {}
        for i in range(16):
            for j in na_tiles(i):
                m = na_mask(i, j)
                for u, mm in enumerate(uniq):
                    if np.array_equal(mm, m): idx[(i, j)] = u; break
                else:
                    uniq.append(m); idx[(i, j)] = len(uniq) - 1
        _NA_MASKS = (np.stack(uniq), idx)
    return _NA_MASKS


def host_consts():
    bf = mybir.dt.np(BF)
    c = {}
    c['ident_f'] = np.eye(128, dtype=np.float32)
    c['ident_b'] = np.eye(128, dtype=np.float32).astype(bf)
    bo = np.zeros((128, 128), np.float32); bo[:64, :64] = 1; bo[64:, 64:] = 1
    c['blockones'] = bo.astype(bf)
    c['ones_f'] = np.ones((128, 128), np.float32)
    R = np.zeros((64, 64), np.float32)
    for base in (0, 32):
        for i in range(16):
            R[base + i, base + 16 + i] = -1.0
            R[base + 16 + i, base + i] = 1.0
    R2 = np.zeros((128, 128), np.float32); R2[:64, :64] = R; R2[64:, 64:] = R
    c['rotT'] = np.ascontiguousarray(R2.T).astype(bf)
    pos = np.arange(L_LAT); row = pos // 64; col = pos % 64
    inv = (10000.0 ** (-np.arange(16, dtype=np.float32) / 16)).astype(np.float32)
    ang = np.zeros((64, L_LAT), np.float32)
    for dd in range(64):
        p = row if dd < 32 else col
        ang[dd] = p.astype(np.float32) * inv[dd % 16]
    c['cosT'] = np.concatenate([np.cos(ang), np.cos(ang)], 0).astype(bf)
    c['sinT'] = np.concatenate([np.sin(ang), np.sin(ang)], 0).astype(bf)
    b = np.arange(128)[:, None]; a = np.arange(128)[None, :]
    c['wmask'] = np.stack([(a <= b), (b <= a)]).astype(np.float32).astype(bf)
    c['namask'] = na_mask_table()[0].astype(bf)
    mz = np.zeros((4, 128, 128), np.float32); my = np.zeros((4, 128, 128), np.float32)
    for gl in range(4):
        for g2 in range(2):
            mz[gl, 64 * g2:64 * g2 + 64, 32 * gl + 16 * g2: 32 * gl + 16 * g2 + 16] = 1
            my[gl, 32 * gl + 16 * g2: 32 * gl + 16 * g2 + 16, 64 * g2:64 * g2 + 64] = 1
    c['maskZ'] = mz; c['maskY'] = my
    return c


def host_layout(inp, b):
    o = {}
    cb = inp['c'][b].reshape(8, 128).T; cc = inp['c_ctx'].reshape(8, 128).T
    o['cT'] = np.ascontiguousarray(np.stack([cb, cc], -1))
    o['bmodT'] = np.ascontiguousarray(inp['b_mod'].reshape(DEPTH, 48, 128).transpose(0, 2, 1))
    o['gmix'] = np.ascontiguousarray(inp['norm_mix'].reshape(DEPTH, 8, 128).transpose(0, 2, 1))
    o['gffn'] = np.ascontiguousarray(inp['norm_ffn'].reshape(DEPTH, 8, 128).transpose(0, 2, 1))
    def p2(x):
        return np.ascontiguousarray(x.reshape(DEPTH, 2, 16, 2, 64).transpose(0, 3, 4, 1, 2).reshape(DEPTH, 128, 32))
    o['lamre'] = p2(inp['ssm_lam_re']); o['lamim'] = p2(inp['ssm_lam_im'])
    o['logdt'] = p2(np.broadcast_to(inp['ssm_log_dt'][..., None], (DEPTH, 2, 32, 64)))
    def pb(x):
        return np.ascontiguousarray(x.reshape(DEPTH, 2, 16, 2, 64, 16).transpose(0, 3, 4, 1, 2, 5).reshape(DEPTH, 128, 32, 16))
    o['bre'] = pb(inp['ssm_b_re']); o['bim'] = pb(inp['ssm_b_im'])
    def pc(x):
        return np.ascontiguousarray(x.reshape(DEPTH, 2, 4, 8, 16, 64).transpose(0, 3, 4, 1, 2, 5).reshape(DEPTH, 128, 8, 64))
    o['cre'] = pc(inp['ssm_c_re']); o['cim'] = pc(inp['ssm_c_im'])
    o['dskip'] = np.ascontiguousarray(inp['ssm_d'].reshape(DEPTH, 4, 128).transpose(0, 2, 1))
    def hd(x): return np.ascontiguousarray(np.tile(x, (1, 2))[:, :, None])
    o['gwq'] = hd(inp['win_q_norm']); o['gwk'] = hd(inp['win_k_norm'])
    o['gnq'] = hd(inp['na_q_norm']); o['gnk'] = hd(inp['na_k_norm'])
    o['sink'] = np.ascontiguousarray(np.broadcast_to(inp['win_sink'][:, None, :], (DEPTH, 128, 8)))
    k = np.arange(128); rk = k // 64; ck = k % 64
    rpb = inp['na_rpb']
    dc = np.clip(ck[:, None] - ck[None, :], -15, 15) + 15
    B = np.zeros((DEPTH, 7, 128, 8, 128), np.float32)
    for di, dl in enumerate(range(-3, 4)):
        dr = np.clip(2 * dl + rk[:, None] - rk[None, :] + 7, 0, 14)
        dcf = dc[ck[:, None], ck[None, :]]
        B[:, di] = rpb[:, :, dr, dcf].transpose(0, 2, 1, 3)
    o['nabias'] = B
    o['rbias'] = np.ascontiguousarray(np.broadcast_to(inp['router_b'][:, None, :], (DEPTH, 128, NE)))
    o['bguT'] = np.ascontiguousarray(inp['b_gate_up'].reshape(DEPTH, NE, 16, 128).transpose(0, 3, 1, 2))
    return o


SMALL_SHAPES = {
    'cT': [128, 8, 2], 'bmodT': [DEPTH, 128, 48], 'gmix': [DEPTH, 128, 8], 'gffn': [DEPTH, 128, 8],
    'lamre': [DEPTH, 128, 32], 'lamim': [DEPTH, 128, 32], 'logdt': [DEPTH, 128, 32],
    'bre': [DEPTH, 128, 32, 16], 'bim': [DEPTH, 128, 32, 16], 'cre': [DEPTH, 128, 8, 64], 'cim': [DEPTH, 128, 8, 64],
    'dskip': [DEPTH, 128, 4], 'gwq': [DEPTH, 128, 1], 'gwk': [DEPTH, 128, 1], 'gnq': [DEPTH, 128, 1], 'gnk': [DEPTH, 128, 1],
    'sink': [DEPTH, 128, 8], 'nabias': [DEPTH, 7, 128, 8, 128], 'rbias': [DEPTH, 128, NE], 'bguT': [DEPTH, 128, NE, 16],
}
BIG = {'w_mod': [DEPTH, D, 6 * D], 'w_in': [DEPTH, D, INW], 'ssm_w_glu': [DEPTH, 512, 512],
       'w_branch': [DEPTH, 3, 512, D], 'w_out': [DEPTH, D, D], 'router_w': [DEPTH, D, NE],
       'w_gate_up': [DEPTH, NE, D, 2 * D], 'w_down': [DEPTH, NE, D, D], 'b_down': [DEPTH, NE, D]}


def build(nlayers=DEPTH, dbg=None, stop_after=None):
    nc = bass.Bass("TRN2", target_bir_lowering=False, dynamic_dma_scratch_size=4096)
    consts = host_consts()
    shapes = {'x': ([L_LAT, D], F32), 'ctx': ([C_CTX, D], F32)}
    for k_, v in consts.items(): shapes[k_] = (list(v.shape), BF if v.dtype != np.float32 else F32)
    for k_, v in SMALL_SHAPES.items(): shapes[k_] = (v, F32)
    for k_, v in BIG.items(): shapes[k_] = ([nlayers] + v[1:], F32)
    class LazyI(dict):
        def __missing__(self, name):
            shp, dt = shapes[name]
            self[name] = nc.dram_tensor(name, list(shp), dt, kind="ExternalInput").ap()
            return self[name]
    I = LazyI()
    yout = nc.dram_tensor('y', [L_LAT, D], F32, kind="ExternalOutput").ap()
    xd = nc.dram_tensor('xd', [T, D], F32, kind="Internal").ap()
    dbg_out = {}
    if dbg:
        for k_, shp in dbg.items():
            dbg_out[k_] = nc.dram_tensor('dbg_' + k_, list(shp), F32, kind="ExternalOutput").ap()

    with ExitStack() as st:
        sbn = [0]
        def sb(name, shape, dt=F32, stack=None):
            sbn[0] += 1
            return (stack or st).enter_context(nc.sbuf_tensor('s%d_%s' % (sbn[0], name), list(shape), dt))
        ident_f = sb('ident_f', [128, 128]); ident_b = sb('ident_b', [128, 128], BF)
        blockones = sb('blockones', [128, 128], BF); ones_f = sb('ones_f', [128, 128])
        nmk, nidx = na_mask_table(); NM = nmk.shape[0]
        epsT = sb('epsT', [128, 1]); halfpi = sb('halfpi', [128, 1]); hm = sb('hm', [128, 2])
        sc = sb('sc', [128, 8, 2])
        modc = sb('modc', [128, 48, 2])
        s1 = sb('s1', [128, 8, 2]); s0 = sb('s0', [128, 8, 2])
        hT = sb('hT', [128, 8, T], BF)
        ps = [st.enter_context(nc.psum_tensor('ps%d' % i, [128, 512], F32)) for i in range(6)]
        pT = [st.enter_context(nc.psum_tensor('pT%d' % i, [128, 1024], BF)) for i in range(2)]
        block = st.enter_context(nc.Block())
        S = Sched(nc, block, st)
        psn = [0]
        def PS():
            i = psn[0] % 6; psn[0] += 1
            return ps[i], 'ps%d' % i
        ptn = [0]
        def PT_():
            i = ptn[0] % 2; ptn[0] += 1
            return pT[i], 'pT%d' % i

        def act(out, in_, func, R, W, **kw): S.add('act', lambda e: e.activation(out=out, in_=in_, func=func, **kw), R, W)
        def ts(eng, out, in0, s1_, s2_, op0, op1, R, W):
            if op1 is None: S.add(eng, lambda e: e.tensor_scalar(out=out, in0=in0, scalar1=s1_, scalar2=None, op0=op0), R, W)
            else: S.add(eng, lambda e: e.tensor_scalar(out=out, in0=in0, scalar1=s1_, scalar2=s2_, op0=op0, op1=op1), R, W)
        def tt(eng, out, in0, in1, op, R, W): S.add(eng, lambda e: e.tensor_tensor(out=out, in0=in0, in1=in1, op=op), R, W)
        def stt(out, in0, scalar, in1, op0, op1, R, W):
            S.add('dve', lambda e: e.scalar_tensor_tensor(out=out, in0=in0, scalar=scalar, in1=in1, op0=op0, op1=op1), R, W)
        def mm(out, lhsT, rhs, start, stop, R, W): S.add('pe', lambda e: e.matmul(out, lhsT, rhs, start=start, stop=stop), R, W)
        def tr(out, in_, ident, R, W): S.add('pe', lambda e: e.transpose(out, in_, ident), R, W)
        def cp(eng, out, in_, R, W):
            if eng == 'act': S.add('act', lambda e: e.copy(out=out, in_=in_), R, W)
            else: S.add(eng, lambda e: e.tensor_copy(out=out, in_=in_), R, W)
        def ms(eng, ap, val, W): S.add(eng, lambda e: e.memset(ap, val), (), W)
        def rec(out, in_, R, W): S.add('dve', lambda e: e.reciprocal(out=out, in_=in_), R, W)

        for k_, t_ in [('ident_f', ident_f), ('ident_b', ident_b), ('blockones', blockones), ('ones_f', ones_f)]:
            S.dma('sp', t_[:], I[k_], (), (k_,))
        ms('dve', hm[:], 0.0, ('hm',)); ms('dve', hm[0:64, 0:1], 1.0, ('hm',)); ms('dve', hm[64:128, 1:2], 1.0, ('hm',))
        ms('dve', epsT[:], EPS, ('epsT',)); ms('dve', halfpi[:], float(np.pi / 2), ('halfpi',))
        S.dma('sp', xd[0:C_CTX, :], I['ctx'], (), ('xd0', 'xd1'))
        S.dma('sp', xd[C_CTX:T, :], I['x'], (), tuple('xd%d' % t for t in range(2, NT)))
        S.dma('sp', sc[:], I['cT'], (), ('sc',))
        act(sc[:], sc[:], AF.Silu, ('sc',), ('sc',))
        S.flush()

        for li in range(nlayers):
            with ExitStack() as ph:
                wm = [sb('wm%d' % i, [128, 8, 512], F32, ph) for i in range(2)]
                bmod = sb('bmod', [128, 48], F32, ph)
                S.dma('sp', bmod[:], I['bmodT'][li], (), ('bmod',))
                wv = I['w_mod'][li].rearrange("(kc p) n -> p kc n", p=128)
                for blk in range(12):
                    w = wm[blk % 2]; wk = 'wm%d' % (blk % 2)
                    S.dma('sp', w[:], wv[:, :, blk * 512:(blk + 1) * 512], (), (wk,))
                    p_, pk = PS()
                    for j in range(4):
                        for kc in range(8):
                            mm(p_[:, 2 * j:2 * j + 2], w[:, kc, j * 128:(j + 1) * 128], sc[:, kc, :], kc == 0, kc == 7, (wk, 'sc'), (pk,))
                    tt('dve', modc[:, blk * 4:blk * 4 + 4, :], p_[:, 0:8].rearrange("p (j c) -> p j c", c=2),
                       bl(bmod[:, blk * 4:blk * 4 + 4], 2), ALU.add, (pk, 'bmod'), ('modc',))
                S.barrier()
            gcol = sb('gcol', [128, 8], F32, st) if li == 0 else gcol
            if stop_after == 'adaln': break

            def mod_scale_shift(gname, jsc, jsh):
                S.dma('sp', gcol[:], I[gname][li], (), ('gcol',))
                ts('dve', s1[:], modc[:, 8 * jsc:8 * jsc + 8, :], 1.0, None, ALU.add, None, ('modc',), ('s1',))
                tt('dve', s1[:], s1[:], bl(gcol[:], 2), ALU.mult, ('s1', 'gcol'), ('s1',))
                cp('dve', s0[:], modc[:, 8 * jsh:8 * jsh + 8, :], ('modc',), ('s0',))

            def mod_bcast(jg, modb):
                with ExitStack() as ph:
                    dg = sb('dg', [128, 128], F32, ph)
                    for v in range(2):
                        for kc in range(8):
                            ts('dve', dg[:], ident_f[:], modc[:, 8 * jg + kc, v:v + 1], None, ALU.mult, None, ('ident_f', 'modc'), ('dg',))
                            p_, pk = PS()
                            mm(p_[:, 0:128], ones_f[:], dg[:], True, True, ('ones_f', 'dg'), (pk,))
                            cp('act', modb[:, v, kc * 128:(kc + 1) * 128], p_[:, 0:128], (pk,), ('modb',))
                    S.barrier()

            def norm_stage():
                with ExitStack() as ph:
                    xt = [sb('xt%d' % i, [128, D], F32, ph) for i in range(2)]
                    junk = sb('junk', [128, D], BF, ph); xn = [sb('xn%d' % i, [128, D], BF, ph) for i in range(2)]
                    ss = sb('ss', [128, NT], F32, ph)
                    for tt_ in range(NT):
                        b_ = tt_ % 2; v = 1 if tt_ < 2 else 0
                        S.dma('sp', xt[b_][:], xd[tt_ * 128:(tt_ + 1) * 128, :], ('xd%d' % tt_,), ('xt%d' % b_,))
                        act(junk[:], xt[b_][:], AF.Square, ('xt%d' % b_,), ('junk', 'ss%d' % tt_), accum_out=ss[:, tt_:tt_ + 1])
                        act(ss[:, tt_:tt_ + 1], ss[:, tt_:tt_ + 1], AF.Sqrt, ('ss%d' % tt_, 'epsT'), ('ss%d' % tt_,), bias=epsT[:], scale=1.0 / D)
                        rec(ss[:, tt_:tt_ + 1], ss[:, tt_:tt_ + 1], ('ss%d' % tt_,), ('ss%d' % tt_,))
                        ts('dve', xn[b_][:], xt[b_][:], ss[:, tt_:tt_ + 1], None, ALU.mult, None, ('xt%d' % b_, 'ss%d' % tt_), ('xn%d' % b_,))
                        p_, pk = PT_()
                        for kc in range(8):
                            tr(p_[:, kc * 128:(kc + 1) * 128], xn[b_][:, kc * 128:(kc + 1) * 128], ident_b[:], ('xn%d' % b_, 'ident_b'), (pk,))
                        for kc in range(8):
                            ts('dve', hT[:, kc, tt_ * 128:(tt_ + 1) * 128], p_[:, kc * 128:(kc + 1) * 128], s1[:, kc, v:v + 1], s0[:, kc, v:v + 1],
                               ALU.mult, ALU.add, (pk, 's1', 's0'), ('hT',))
                    S.barrier()

            win = I['w_in'][li].rearrange("(kc p) n -> p kc n", p=128)

            def load_w(wt, wkey, view, c0, n, dst0=0):
                S.dma('pool', wt[:, :, dst0:dst0 + n], view[:, :, c0:c0 + n], (), (wkey,))

            def proj_fm(wt, wkey, wc0, consumer, src=None, nk=8, srckey='hT'):
                src = hT if src is None else src
                for (c0, n) in CH:
                    p_, pk = PS()
                    for kc in range(nk):
                        mm(p_[:, 0:n], wt[:, kc, wc0:wc0 + 128], src[:, kc, c0:c0 + n], kc == 0, kc == nk - 1, (wkey,) + (srckey if isinstance(srckey, tuple) else (srckey,)), (pk,))
                    consumer(p_[:, 0:n], pk, c0, n)

            mod_scale_shift('gmix', 1, 0)
            norm_stage()
            if stop_after == 'norm': break
            with ExitStack() as mx:
                rotT = sb('rotT', [128, 128], BF, mx)
                cosT = sb('cosT', [128, L_LAT], BF, mx); sinT = sb('sinT', [128, L_LAT], BF, mx)
                wmask = sb('wmask', [128, 2, 128], BF, mx); namask = sb('namask', [128, NM, 128], BF, mx)
                maskZ = sb('maskZ', [128, 4, 128], F32, mx); maskY = sb('maskY', [128, 4, 128], F32, mx)
                for k_, t_ in [('rotT', rotT), ('cosT', cosT), ('sinT', sinT)]:
                    S.dma('sp', t_[:], I[k_], (), (k_,))
                S.dma('sp', maskZ[:], I['maskZ'].rearrange("g p c -> p g c"), (), ('maskZ',))
                S.dma('sp', maskY[:], I['maskY'].rearrange("g p c -> p g c"), (), ('maskY',))
                S.dma('sp', wmask[:], I['wmask'].rearrange("g p c -> p g c"), (), ('wmask',))
                S.dma('sp', namask[:], I['namask'].rearrange("g p c -> p g c"), (), ('namask',))
                yT0 = sb('yT0', [128, 4, T], BF, mx)
                yTbox = [None]
                class _YT:
                    def __getitem__(self, idx):
                        p, m, sl = idx
                        if isinstance(m, slice):
                            if m.start >= 4: return yTbox[0][p, m.start - 4:m.stop - 4, sl]
                            return yT0[p, m, sl]
                        if m >= 4: return yTbox[0][p, m - 4, sl]
                        return yT0[p, m, sl]
                yT = _YT()
                with ExitStack() as ph:
                    uT = sb('uT', [128, 4, T], BF, ph)
                    acc = sb('acc', [128, 4, T], F32, ph)
                    dsk = sb('dsk', [128, 4], F32, ph)
                    wa_scope = ExitStack()
                    wa = sb('wa', [128, 8, 512], BF, wa_scope)
                    S.dma('sp', dsk[:], I['dskip'][li], (), ('dsk',))
                    load_w(wa, 'wa', win, 0, 512)
                    for m in range(4):
                        def cons(p_, pk, c0, n, m=m):
                            import os
                            dbgv = int(os.environ.get('S5DBG', '0'))
                            if dbgv in (0, 2): cp('act', uT[:, m, c0:c0 + n], p_, (pk,), ('uT%d' % m,))
                            if dbgv in (0, 3): ts('dve', acc[:, m, c0:c0 + n], uT[:, m, c0:c0 + n], dsk[:, m:m + 1], None, ALU.mult, None, ('uT%d' % m, 'dsk'), ('acc%d' % m,))
                        proj_fm(wa, 'wa', m * 128, cons)
                    S.barrier(); wa_scope.close()
                    if stop_after == 's5a': break
                    P = {k_: sb('sp_' + k_, [128, 32], F32, ph) for k_ in ['lr', 'li', 'dt', 'mag', 'c', 's', 't1', 't2', 'ar', 'ai', 'fr', 'fi', 'den']}
                    S.dma('sp', P['lr'][:], I['lamre'][li], (), ('p_lr',)); S.dma('sp', P['li'][:], I['lamim'][li], (), ('p_li',))
                    S.dma('sp', P['dt'][:], I['logdt'][li], (), ('p_dt',))
                    K_ = ('sp',)
                    act(P['dt'][:], P['dt'][:], AF.Exp, ('p_dt',), ('p_dt',))
                    tt('dve', P['t1'][:], P['lr'][:], P['dt'][:], ALU.mult, ('p_lr', 'p_dt'), K_)
                    act(P['mag'][:], P['t1'][:], AF.Exp, K_, K_)
                    tt('dve', P['t2'][:], P['li'][:], P['dt'][:], ALU.mult, ('p_li', 'p_dt'), K_)
                    act(P['s'][:], P['t2'][:], AF.Sin, K_, K_, scale=1.0 / 16)
                    act(P['c'][:], P['t2'][:], AF.Sin, K_ + ('halfpi',), K_, scale=1.0 / 16, bias=halfpi[:])
                    for _ in range(4):
                        tt('dve', P['t1'][:], P['c'][:], P['c'][:], ALU.mult, K_, K_)
                        tt('dve', P['t2'][:], P['s'][:], P['s'][:], ALU.mult, K_, K_)
                        tt('dve', P['s'][:], P['s'][:], P['c'][:], ALU.mult, K_, K_)
                        ts('dve', P['s'][:], P['s'][:], 2.0, None, ALU.mult, None, K_, K_)
                        tt('dve', P['c'][:], P['t1'][:], P['t2'][:], ALU.subtract, K_, K_)
                    tt('dve', P['ar'][:], P['mag'][:], P['c'][:], ALU.mult, K_, K_)
                    tt('dve', P['ai'][:], P['mag'][:], P['s'][:], ALU.mult, K_, K_)
                    tt('dve', P['den'][:], P['lr'][:], P['lr'][:], ALU.mult, K_, K_)
                    tt('dve', P['t1'][:], P['li'][:], P['li'][:], ALU.mult, K_, K_)
                    tt('dve', P['den'][:], P['den'][:], P['t1'][:], ALU.add, K_, K_)
                    rec(P['den'][:], P['den'][:], K_, K_)
                    ts('dve', P['t1'][:], P['ar'][:], -1.0, None, ALU.add, None, K_, K_)
                    tt('dve', P['fr'][:], P['t1'][:], P['lr'][:], ALU.mult, K_, K_)
                    tt('dve', P['t2'][:], P['ai'][:], P['li'][:], ALU.mult, K_, K_)
                    tt('dve', P['fr'][:], P['fr'][:], P['t2'][:], ALU.add, K_, K_)
                    tt('dve', P['fr'][:], P['fr'][:], P['den'][:], ALU.mult, K_, K_)
                    tt('dve', P['fi'][:], P['ai'][:], P['lr'][:], ALU.mult, K_, K_)
                    tt('dve', P['t2'][:], P['t1'][:], P['li'][:], ALU.mult, K_, K_)
                    tt('dve', P['fi'][:], P['fi'][:], P['t2'][:], ALU.subtract, K_, K_)
                    tt('dve', P['fi'][:], P['fi'][:], P['den'][:], ALU.mult, K_, K_)
                    pwr = sb('pwr', [128, 12, 32], F32, ph); pwi = sb('pwi', [128, 12, 32], F32, ph); npwi = sb('npwi', [128, 12, 32], F32, ph)
                    cp('dve', pwr[:, 0, :], P['ar'][:], K_, K_); cp('dve', pwi[:, 0, :], P['ai'][:], K_, K_)
                    for i in range(1, 12):
                        tt('dve', P['t1'][:], pwr[:, i - 1, :], pwr[:, i - 1, :], ALU.mult, K_, K_)
                        tt('dve', P['t2'][:], pwi[:, i - 1, :], pwi[:, i - 1, :], ALU.mult, K_, K_)
                        tt('dve', pwr[:, i, :], P['t1'][:], P['t2'][:], ALU.subtract, K_, K_)
                        tt('dve', P['t1'][:], pwr[:, i - 1, :], pwi[:, i - 1, :], ALU.mult, K_, K_)
                        ts('dve', pwi[:, i, :], P['t1'][:], 2.0, None, ALU.mult, None, K_, K_)
                    ts('dve', npwi[:], pwi[:], -1.0, None, ALU.mult, None, K_, K_)
                    apr = sb('apr', [128, 8, 32], F32, ph); api = sb('api', [128, 8, 32], F32, ph); napi = sb('napi', [128, 8, 32], F32, ph)
                    for p_i, src_i in [(1, 0), (2, 1), (4, 2), (8, 3)]:
                        cp('dve', apr[:, p_i - 1, :], pwr[:, src_i, :], K_, K_); cp('dve', api[:, p_i - 1, :], pwi[:, src_i, :], K_, K_)
                    def cmul(pd, pa, pb):
                        tt('dve', P['t1'][:], apr[:, pa - 1, :], apr[:, pb - 1, :], ALU.mult, K_, K_)
                        tt('dve', P['t2'][:], api[:, pa - 1, :], api[:, pb - 1, :], ALU.mult, K_, K_)
                        tt('dve', apr[:, pd - 1, :], P['t1'][:], P['t2'][:], ALU.subtract, K_, K_)
                        tt('dve', P['t1'][:], apr[:, pa - 1, :], api[:, pb - 1, :], ALU.mult, K_, K_)
                        tt('dve', P['t2'][:], api[:, pa - 1, :], apr[:, pb - 1, :], ALU.mult, K_, K_)
                        tt('dve', api[:, pd - 1, :], P['t1'][:], P['t2'][:], ALU.add, K_, K_)
                    cmul(3, 2, 1); cmul(5, 4, 1); cmul(6, 4, 2); cmul(7, 4, 3)
                    ts('dve', napi[:], api[:], -1.0, None, ALU.mult, None, K_, K_)
                    br = sb('br', [128, 32, 16], F32, ph); bi = sb('bi', [128, 32, 16], F32, ph)
                    bbr = sb('bbr', [128, 32, 16], F32, ph); bbi = sb('bbi', [128, 32, 16], F32, ph); tb = sb('tb', [128, 32, 16], F32, ph)
                    S.dma('sp', br[:], I['bre'][li], (), K_); S.dma('sp', bi[:], I['bim'][li], (), K_)
                    frb = bl(P['fr'][:], 16); fib = bl(P['fi'][:], 16)
                    tt('dve', bbr[:], br[:], frb, ALU.mult, K_, K_); tt('dve', tb[:], bi[:], fib, ALU.mult, K_, K_)
                    tt('dve', bbr[:], bbr[:], tb[:], ALU.subtract, K_, K_)
                    tt('dve', bbi[:], bi[:], frb, ALU.mult, K_, K_); tt('dve', tb[:], br[:], fib, ALU.mult, K_, K_)
                    tt('dve', bbi[:], bbi[:], tb[:], ALU.add, K_, K_)
                    cn_r = sb('cn_r', [128, 8, 64], F32, ph); cn_i = sb('cn_i', [128, 8, 64], F32, ph)
                    S.dma('sp', cn_r[:], I['cre'][li], (), K_); S.dma('sp', cn_i[:], I['cim'][li], (), K_)
                    ts('dve', cn_i[:], cn_i[:], -1.0, None, ALU.mult, None, K_, K_)
                    Zt = sb('Zt', [128, 128], F32, ph)
                    BTr = sb('BTr', [128, 128], BF, ph); BTi = sb('BTi', [128, 128], BF, ph)
                    CTr = sb('CTr', [128, 128], F32, ph); CTi = sb('CTi', [128, 128], F32, ph)
                    KA = [sb('ks%d' % i, [128, T], F32, ph) for i in range(2)]
                    KB = [sb('kb%d' % i, [128, 288], F32, ph) for i in range(4)]
                    S.barrier()
                    if stop_after == 's5b': break
                    for d_ in range(2):
                        if stop_after == 's5c' and d_ == 1: break
                        for gp in range(16):
                            if stop_after == 's5c' and gp == 1: break
                            col = d_ * 16 + gp; tile_ = gp // 4; gl = gp % 4
                            for src_, dst_, dk in [(bbr, BTr, 'BTr'), (bbi, BTi, 'BTi')]:
                                tt('dve', Zt[:].rearrange("p (a q) -> p a q", q=16), bmid(src_[:, col, :], 8),
                                   maskZ[:, gl, :].rearrange("p (a q) -> p a q", q=16), ALU.mult, K_ + ('maskZ',), ('Zt',))
                                p_, pk = PS()
                                tr(p_[:, 0:128], Zt[:], ident_f[:], ('Zt', 'ident_f'), (pk,))
                                cp('act', dst_[:], p_[:, 0:128], (pk,), (dk,))
                            for src_, dst_, dk in [(cn_r, CTr, 'CTr'), (cn_i, CTi, 'CTi')]:
                                tt('dve', Zt[:].rearrange("p (a n) -> p a n", n=64), bmid(src_[:, d_ * 4 + tile_, :], 2),
                                   maskY[:, gl, :].rearrange("p (a n) -> p a n", n=64), ALU.mult, K_ + ('maskY',), ('Zt',))
                                p_, pk = PS()
                                tr(p_[:, 0:128], Zt[:], ident_f[:], ('Zt', 'ident_f'), (pk,))
                                cp('act', dst_[:], p_[:, 0:128], (pk,), (dk,))
                            def pos(c0, n):
                                if d_ == 0: return c0
                                return (c0 - C_CTX) if c0 >= C_CTX else L_LAT
                            for (c0, n) in CH:
                                for lh, dst_, lk, dk in [(BTr, KA[0], 'BTr', 'ka0'), (BTi, KA[1], 'BTi', 'ka1')]:
                                    p_, pk = PS()
                                    mm(p_[:, 0:n], lh[:], uT[:, tile_, c0:c0 + n], True, True, (lk, 'uT%d' % tile_), (pk,))
                                    cp('act', dst_[:, pos(c0, n):pos(c0, n) + n], p_[:, 0:n], (pk,), (dk,))
                            NCH = T // 8
                            Rv = KA[0][:].rearrange("p (j s) -> p j s", s=8); Iv = KA[1][:].rearrange("p (j s) -> p j s", s=8)
                            c1r = apr[:, 0, col:col + 1]; c1i = api[:, 0, col:col + 1]; nc1i = napi[:, 0, col:col + 1]
                            order = range(1, 8) if d_ == 0 else range(6, -1, -1)
                            for s_ in order:
                                q_ = s_ - 1 if d_ == 0 else s_ + 1
                                stt(Rv[:, :, s_], Rv[:, :, q_], c1r, Rv[:, :, s_], ALU.mult, ALU.add, ('ka0',) + K_, ('ka0',))
                                stt(Rv[:, :, s_], Iv[:, :, q_], nc1i, Rv[:, :, s_], ALU.mult, ALU.add, ('ka0', 'ka1') + K_, ('ka0',))
                                stt(Iv[:, :, s_], Iv[:, :, q_], c1r, Iv[:, :, s_], ALU.mult, ALU.add, ('ka1',) + K_, ('ka1',))
                                stt(Iv[:, :, s_], Rv[:, :, q_], c1i, Iv[:, :, s_], ALU.mult, ALU.add, ('ka0', 'ka1') + K_, ('ka1',))
                            es = 7 if d_ == 0 else 0
                            cp('act', KB[0][:], Rv[:, :, es], ('ka0',), ('kb0',))
                            cp('act', KB[1][:], Iv[:, :, es], ('ka1',), ('kb1',))
                            cur = 0
                            for i in range(9):
                                k = 1 << i
                                sr, si = KB[cur], KB[cur + 1]; dr_, di_ = KB[2 - cur], KB[3 - cur]
                                skr, ski = 'kb%d' % cur, 'kb%d' % (cur + 1); dkr, dki = 'kb%d' % (2 - cur), 'kb%d' % (3 - cur)
                                cr = pwr[:, 3 + i, col:col + 1]; ci = pwi[:, 3 + i, col:col + 1]; nci = npwi[:, 3 + i, col:col + 1]
                                if d_ == 0: dst_sl = slice(k, NCH); src_sl = slice(0, NCH - k); keep = slice(0, k)
                                else: dst_sl = slice(0, NCH - k); src_sl = slice(k, NCH); keep = slice(NCH - k, NCH)
                                cp('act', dr_[:, keep], sr[:, keep], (skr,), (dkr + 'k',))
                                cp('act', di_[:, keep], si[:, keep], (ski,), (dki + 'k',))
                                stt(dr_[:, dst_sl], sr[:, src_sl], cr, sr[:, dst_sl], ALU.mult, ALU.add, (skr, skr + 'k') + K_, (dkr,))
                                stt(dr_[:, dst_sl], si[:, src_sl], nci, dr_[:, dst_sl], ALU.mult, ALU.add, (ski, ski + 'k', dkr) + K_, (dkr,))
                                stt(di_[:, dst_sl], si[:, src_sl], cr, si[:, dst_sl], ALU.mult, ALU.add, (ski, ski + 'k') + K_, (dki,))
                                stt(di_[:, dst_sl], sr[:, src_sl], ci, di_[:, dst_sl], ALU.mult, ALU.add, (skr, skr + 'k', dki) + K_, (dki,))
                                cur = 2 - cur
                            Hr, Hi = KB[cur], KB[cur + 1]; hkr_, hki_ = 'kb%d' % cur, 'kb%d' % (cur + 1)
                            for s_ in range(8):
                                pw_ = (s_ + 1) if d_ == 0 else (8 - s_)
                                fr_ = apr[:, pw_ - 1, col:col + 1]; fi_ = api[:, pw_ - 1, col:col + 1]; nfi_ = napi[:, pw_ - 1, col:col + 1]
                                if d_ == 0: osl = slice(1, NCH); hsl = slice(0, NCH - 1)
                                else: osl = slice(0, NCH - 1); hsl = slice(1, NCH)
                                hdeps = (hkr_, hki_, hkr_ + 'k', hki_ + 'k') + K_
                                stt(Rv[:, osl, s_], Hr[:, hsl], fr_, Rv[:, osl, s_], ALU.mult, ALU.add, ('ka0',) + hdeps, ('ka0',))
                                stt(Rv[:, osl, s_], Hi[:, hsl], nfi_, Rv[:, osl, s_], ALU.mult, ALU.add, ('ka0',) + hdeps, ('ka0',))
                                stt(Iv[:, osl, s_], Hi[:, hsl], fr_, Iv[:, osl, s_], ALU.mult, ALU.add, ('ka1',) + hdeps, ('ka1',))
                                stt(Iv[:, osl, s_], Hr[:, hsl], fi_, Iv[:, osl, s_], ALU.mult, ALU.add, ('ka1',) + hdeps, ('ka1',))
                            hr, hi = KA[0], KA[1]; hkr, hki = 'ka0', 'ka1'
                            for (c0, n) in CH:
                                p_, pk = PS()
                                mm(p_[:, 0:n], CTr[:], hr[:, pos(c0, n):pos(c0, n) + n], True, False, ('CTr', hkr), (pk,))
                                mm(p_[:, 0:n], CTi[:], hi[:, pos(c0, n):pos(c0, n) + n], False, True, ('CTi', hki), (pk,))
                                tt('dve', acc[:, tile_, c0:c0 + n], acc[:, tile_, c0:c0 + n], p_[:, 0:n], ALU.add, (pk, 'acc%d' % tile_), ('acc%d' % tile_,))
                            S.flush()
                    if dbg and 'acc' in dbg_out and li == 0:
                        S.dma('sp', dbg_out['acc'].rearrange("(m p) t -> p (m t)", p=128), acc[:].rearrange("p m t -> p (m t)"), ['acc%d' % m for m in range(4)], ())
                    wg = sb('wg', [128, 4, 512], BF, ph)
                    S.dma('pool', wg[:], I['ssm_w_glu'][li].rearrange("(kc p) n -> p kc n", p=128), (), ('wg',))
                    t3 = KA[0]; gT = uT
                    for m in range(4):
                        a_ = acc[:, m, :]
                        act(t3[:], a_, AF.Square, ('acc%d' % m,), ('ka0',))
                        ts('dve', t3[:], t3[:], 0.044715, 1.0, ALU.mult, ALU.add, ('ka0',), ('ka0',))
                        tt('dve', t3[:], t3[:], a_, ALU.mult, ('ka0', 'acc%d' % m), ('ka0',))
                        act(t3[:], t3[:], AF.Sigmoid, ('ka0',), ('ka0',), scale=1.5957691216057308)
                        tt('dve', gT[:, m, :], t3[:], a_, ALU.mult, ('ka0', 'acc%d' % m), ('uT%d' % m,))
                    for m in range(4):
                        def cons(p_, pk, c0, n, m=m):
                            act(KA[1][:, c0:c0 + n], p_, AF.Sigmoid, (pk,), ('ka1',))
                            tt('dve', yT[:, m, c0:c0 + n], gT[:, m, c0:c0 + n], KA[1][:, c0:c0 + n], ALU.mult, ('ka1', 'uT%d' % m), ('yT%d' % m,))
                        proj_fm(wg, 'wg', m * 128, cons, src=gT, nk=4, srckey=('uT0', 'uT1', 'uT2', 'uT3'))
                    S.barrier()

                if stop_after == 's5': break
                yTbox[0] = sb('yT2', [128, 8, T], BF, mx)
                def qk_prep(dst, dkey, wt, wkey, wc0, gcol_ap, gkey, rope, tmp):
                    raw, sq, rs, qn, t1 = tmp
                    for (c0, n) in CH:
                        p_, pk = PS()
                        for kc in range(8):
                            mm(p_[:, 0:n], wt[:, kc, wc0:wc0 + 128], hT[:, kc, c0:c0 + n], kc == 0, kc == 7, (wkey, 'hT'), (pk,))
                        cp('act', raw[:, 0:n], p_[:, 0:n], (pk,), ('raw',))
                        act(sq[:, 0:n], p_[:, 0:n], AF.Square, (pk,), ('sq',))
                        p2, pk2 = PS()
                        mm(p2[:, 0:n], blockones[:], sq[:, 0:n], True, True, ('blockones', 'sq'), (pk2,))
                        act(rs[:, 0:n], p2[:, 0:n], AF.Sqrt, (pk2, 'epsT'), ('rs',), bias=epsT[:], scale=1.0 / 64)
                        rec(rs[:, 0:n], rs[:, 0:n], ('rs',), ('rs',))
                        if not (rope and c0 >= C_CTX):
                            stt(dst[:, c0:c0 + n], raw[:, 0:n], gcol_ap, rs[:, 0:n], ALU.mult, ALU.mult, ('raw', 'rs', gkey), (dkey,))
                        else:
                            l0 = c0 - C_CTX
                            stt(qn[:, 0:n], raw[:, 0:n], gcol_ap, rs[:, 0:n], ALU.mult, ALU.mult, ('raw', 'rs', gkey), ('qn',))
                            p3, pk3 = PS()
                            mm(p3[:, 0:n], rotT[:], qn[:, 0:n], True, True, ('rotT', 'qn'), (pk3,))
                            tt('dve', t1[:, 0:n], qn[:, 0:n], cosT[:, l0:l0 + n], ALU.mult, ('qn', 'cosT'), ('t1',))
                            tt('dve', raw[:, 0:n], p3[:, 0:n], sinT[:, l0:l0 + n], ALU.mult, (pk3, 'sinT'), ('raw',))
                            tt('pool', dst[:, c0:c0 + n], t1[:, 0:n], raw[:, 0:n], ALU.add, ('t1', 'raw'), (dkey,))

                def attention(kind, hg):
                    with ExitStack() as ph:
                        tmp = (sb('raw', [128, 512], F32, ph), sb('sq', [128, 512], BF, ph), sb('rs', [128, 512], F32, ph),
                               sb('qn', [128, 512], BF, ph), sb('t1', [128, 512], F32, ph))
                        gq = sb('gq', [128, 1], F32, ph); gk = sb('gk', [128, 1], F32, ph)
                        S.dma('sp', gq[:], I['gwq' if kind == 'w' else 'gnq'][li], (), ('gq',))
                        S.dma('sp', gk[:], I['gwk' if kind == 'w' else 'gnk'][li], (), ('gk',))
                        ts('dve', gq[:], gq[:], 0.125, None, ALU.mult, None, ('gq',), ('gq',))
                        wq_ = sb('wq_', [128, 8, 256], BF, ph)
                        qT = sb('qT', [128, 2, T], BF, ph)
                        nkt = 1 if kind == 'w' else 2
                        nv = 1 if kind == 'w' else 4
                        wk_ = sb('wk_', [128, 8, 128 * nkt], BF, ph); kT = sb('kT', [128, nkt, T], BF, ph)
                        wv_ = sb('wv_', [128, 8, 64 * nv], BF, ph); vaug = sb('vaug', [128, NT, nv, 65], BF, ph)
                        if kind == 'w':
                            load_w(wq_, 'wq_', win, 512 + 256 * hg, 256)
                            load_w(wk_, 'wk_', win, 1024 + 64 * hg, 64, 0); load_w(wk_, 'wk_', win, 1024 + 64 * hg, 64, 64)
                            load_w(wv_, 'wv_', win, 1152 + 64 * hg, 64)
                        else:
                            load_w(wq_, 'wq_', win, 1280 + 256 * hg, 256)
                            load_w(wk_, 'wk_', win, 1792 + 256 * hg, 256)
                            load_w(wv_, 'wv_', win, 2304 + 256 * hg, 256)
                        for m in range(2):
                            qk_prep(qT[:, m, :], 'qT', wq_, 'wq_', m * 128, gq[:, 0:1], 'gq', kind == 'w', tmp)
                        for m in range(nkt):
                            qk_prep(kT[:, m, :], 'kT', wk_, 'wk_', m * 128, gk[:, 0:1], 'gk', kind == 'w', tmp)
                        kTm = sb('kTm', [128, nkt, 2, T], BF, ph)
                        for m in range(nkt):
                            for par in range(2):
                                ts('dve', kTm[:, m, par, :], kT[:, m, :], hm[:, par:par + 1], None, ALU.mult, None, ('kT', 'hm'), ('kTm',))
                        ms('pool', vaug[:, :, :, 64:65], 1.0, ('vaug',))
                        for t_ in range(NT):
                            p_, pk = PS()
                            for kc in range(8):
                                mm(p_[:, 0:64 * nv], hT[:, kc, t_ * 128:(t_ + 1) * 128], wv_[:, kc, :], kc == 0, kc == 7, ('hT', 'wv_'), (pk,))
                            cp('act', vaug[:, t_, :, 0:64], p_[:, 0:64 * nv].rearrange("p (v d) -> p v d", d=64), (pk,), ('vaug',))
                        esink = sb('esink', [128, 8], F32, ph)
                        if kind == 'w':
                            S.dma('sp', esink[:], I['sink'][li], (), ('esink',))
                            act(esink[:], esink[:], AF.Exp, ('esink',), ('esink',))
                        else:
                            nab = sb('nab', [128, 7, 4, 128], BF, ph)
                            for di in range(7):
                                S.dma('pool', nab[:, di, :, :], I['nabias'][li, di, :, 4 * hg:4 * hg + 4, :], (), ('nab',))
                        import os
                        adbg = int(os.environ.get('ATTDBG', '0'))
                        if adbg == 1:
                            S.barrier(); return
                        NSLOT = 7
                        PTs = sb('PTs', [128, NSLOT, 4, 128], BF, ph)
                        ytok = sb('ytok', [128, 4, 64], BF, ph); tot = sb('tot', [128, 4], F32, ph)
                        S.flush()
                        ybase = (4 if kind == 'w' else 8) + 2 * hg
                        for tq in range(NT):
                            keys = []
                            if tq < 2: keys = [(0, None, None), (1, None, None)]
                            else:
                                i = tq - 2
                                keys = [(0, None, None), (1, None, None)]
                                if kind == 'w':
                                    for j in (i - 1, i, i + 1):
                                        if 0 <= j < 16:
                                            keys.append((2 + j, None if j == i else (wmask[:, 0, :] if j < i else wmask[:, 1, :]), None))
                                else:
                                    for j in na_tiles(i):
                                        keys.append((2 + j, namask[:, nidx[(i, j)], :], nab[:, j - i + 3, :, :]))
                            assert len(keys) <= NSLOT
                            for sl, (tk, mask, bias) in enumerate(keys):
                                p_, pk = PS()
                                if bias is not None:
                                    mm(p_[:, 0:512], ident_b[:], bias.rearrange("p h q -> p (h q)"), True, False, ('ident_b', 'nab'), (pk,))
                                for h in range(4):
                                    if adbg == 4 and h % 2 == 1: continue
                                    half = (h % 2) * 64
                                    kt = kTm[:, 0 if kind == 'w' else h // 2, h % 2, tk * 128:(tk + 1) * 128]
                                    mm(p_[:, h * 128:(h + 1) * 128], kt, qT[:, h // 2, tq * 128:(tq + 1) * 128],
                                       bias is None, h == 3 or bias is None, ('kTm', 'qT'), (pk,))
                                act(PTs[:, sl, :, :].rearrange("p h q -> p (h q)"), p_[:, 0:512], AF.Exp, (pk,), ('PT%d' % sl,))
                                if mask is not None and adbg not in (3, 4):
                                    tt('pool', PTs[:, sl, :, :], PTs[:, sl, :, :], bmid(mask, 4), ALU.mult, ('PT%d' % sl, 'wmask', 'namask'), ('PT%d' % sl,))
                            if adbg in (2, 3, 4):
                                continue
                            po, pok = PS()
                            for h in range(4):
                                for sl, (tk, mask, bias) in enumerate(keys):
                                    mm(po[:, h * 65:(h + 1) * 65], PTs[:, sl, h, :], vaug[:, tk, 0 if kind == 'w' else h, :],
                                       sl == 0, sl == len(keys) - 1, ('PT%d' % sl, 'vaug'), (pok,))
                            pov = po[:, 0:260].rearrange("p (h e) -> p h e", e=65)
                            if kind == 'w':
                                tt('dve', tot[:], pov[:, :, 64], esink[:, 4 * hg:4 * hg + 4], ALU.add, (pok, 'esink'), ('tot',))
                            else:
                                cp('dve', tot[:], pov[:, :, 64], (pok,), ('tot',))
                            rec(tot[:], tot[:], ('tot',), ('tot',))
                            tt('dve', ytok[:], pov[:, :, 0:64], bl(tot[:], 64), ALU.mult, (pok, 'tot'), ('ytok',))
                            pt_, ptk = PT_()
                            for m in range(2):
                                tr(pt_[:, m * 128:(m + 1) * 128], ytok[:, 2 * m:2 * m + 2, :].rearrange("p h d -> p (h d)"), ident_b[:], ('ytok', 'ident_b'), (ptk,))
                            cp('act', yT[:, ybase:ybase + 2, tq * 128:(tq + 1) * 128], pt_[:, 0:256].rearrange("p (m t) -> p m t", t=128), (ptk,), ('yT%d' % ybase, 'yT%d' % (ybase + 1)))
                            if tq % 6 == 5: S.flush()
                        S.barrier()

                for kind in ('w', 'n'):
                    for hg in range(2):
                        attention(kind, hg)
                if dbg and 'yT' in dbg_out and li == 0:
                    with ExitStack() as ph:
                        yf = sb('yf', [128, 12, T], F32, ph) if False else None

                if stop_after == 'attn': break
                with ExitStack() as ph:
                    modb = sb('modb', [128, 2, D], F32, ph)
                    mod_bcast(2, modb)
                    wbr = sb('wbr', [128, 12, D], BF, ph)
                    S.dma('pool', wbr[:], I['w_branch'][li].rearrange("i (kc p) d -> p (i kc) d", p=128), (), ('wbr',))
                    wo = sb('wo', [128, 8, D], BF, ph)
                    S.dma('pool', wo[:], I['w_out'][li].rearrange("(kc p) d -> p kc d", p=128), (), ('wo',))
                    wgt = [sb('wgt%d' % i, [128, 8, 384], BF, ph) for i in range(2)]
                    sT = sb('sT', [128, 8, 512], BF, ph)
                    sg = [sb('sg%d' % i, [128, 512], F32, ph) for i in range(3)]
                    xr = [sb('xr%d' % i, [128, D], F32, ph) for i in range(2)]
                    tmpm = sb('tmpm', [128, 512], F32, ph)
                    it = 0
                    for (c0, n) in CH:
                        for dt_ in range(8):
                            w = wgt[it % 2]; wk = 'wgt%d' % (it % 2); it += 1
                            for i in range(3):
                                load_w(w, wk + '_%d' % i, win, 2816 + i * 1024 + dt_ * 128, 128, i * 128)
                            for i in range(3):
                                pg, pgk = PS()
                                for kc in range(8):
                                    mm(pg[:, 0:n], w[:, kc, i * 128:(i + 1) * 128], hT[:, kc, c0:c0 + n], kc == 0, kc == 7, (wk + '_%d' % i, 'hT'), (pgk,))
                                act(sg[i][:, 0:n], pg[:, 0:n], AF.Sigmoid, (pgk,), ('sg%d' % i,))
                                pp, ppk = PS()
                                for kc in range(4):
                                    mm(pp[:, 0:n], wbr[:, i * 4 + kc, dt_ * 128:(dt_ + 1) * 128], yT[:, i * 4 + kc, c0:c0 + n], kc == 0, kc == 3,
                                       ('wbr',) + tuple('yT%d' % q for q in range(12)), (ppk,))
                                tt('dve', sg[i][:, 0:n], sg[i][:, 0:n], pp[:, 0:n], ALU.mult, (ppk, 'sg%d' % i), ('sg%d' % i,))
                            tt('pool', sg[0][:, 0:n], sg[0][:, 0:n], sg[1][:, 0:n], ALU.add, ('sg0', 'sg1'), ('sg0',))
                            tt('pool', sT[:, dt_, 0:n], sg[0][:, 0:n], sg[2][:, 0:n], ALU.add, ('sg0', 'sg2'), ('sT',))
                        for tl_ in range(n // 128):
                            tt_ = c0 // 128 + tl_; b_ = tt_ % 2; v = 1 if tt_ < 2 else 0
                            S.dma('sp', xr[b_][:], xd[tt_ * 128:(tt_ + 1) * 128, :], ('xd%d' % tt_,), ('xr%d' % b_,))
                            for hf in range(2):
                                p_, pk = PS()
                                for kc in range(8):
                                    mm(p_[:, 0:512], sT[:, kc, tl_ * 128:(tl_ + 1) * 128], wo[:, kc, hf * 512:(hf + 1) * 512], kc == 0, kc == 7, ('sT', 'wo'), (pk,))
                                tt('dve', tmpm[:], p_[:, 0:512], modb[:, v, hf * 512:(hf + 1) * 512], ALU.mult, (pk, 'modb'), ('tmpm',))
                                tt('pool', xr[b_][:, hf * 512:(hf + 1) * 512], xr[b_][:, hf * 512:(hf + 1) * 512], tmpm[:], ALU.add, ('tmpm', 'xr%d' % b_), ('xr%d' % b_,))
                            S.dma('sp', xd[tt_ * 128:(tt_ + 1) * 128, :], xr[b_][:], ('xr%d' % b_,), ('xd%d' % tt_,))
                    S.barrier()

            if stop_after == 'merge': break
            mod_scale_shift('gffn', 4, 3)
            norm_stage()
            with ExitStack() as ph:
                macc = sb('macc', [128, NT, D], F32, ph)
                G = sb('G', [128, NT, NE], F32, ph)
                with ExitStack() as ph1:
                    rw = sb('rw', [128, 8, NE], BF, ph1); rb = sb('rb', [128, NE], F32, ph1)
                    S.dma('pool', rw[:], I['router_w'][li].rearrange("(kc p) e -> p kc e", p=128), (), ('rw',))
                    S.dma('sp', rb[:], I['rbias'][li], (), ('rb',))
                    lg = sb('lg', [128, NE], F32, ph1); m8 = sb('m8', [128, 8], F32, ph1); mk = sb('mk', [128, NE], F32, ph1)
                    nmx = sb('nmx', [128, 1], F32, ph1); sm = sb('sm', [128, 1], F32, ph1)
                    ms('pool', macc[:], 0.0, tuple('macc%d' % t for t in range(NT)))
                    for t_ in range(NT):
                        p_, pk = PS()
                        for kc in range(8):
                            mm(p_[:, 0:NE], hT[:, kc, t_ * 128:(t_ + 1) * 128], rw[:, kc, :], kc == 0, kc == 7, ('hT', 'rw'), (pk,))
                        tt('dve', lg[:], p_[:, 0:NE], rb[:], ALU.add, (pk, 'rb'), ('lg',))
                        S.add('dve', lambda e: e.max(out=m8[:], in_=lg[:]), ('lg',), ('m8',))
                        ts('dve', mk[:], lg[:], m8[:, 3:4], None, ALU.is_ge, None, ('lg', 'm8'), ('mk',))
                        ts('dve', nmx[:], m8[:, 0:1], -1.0, None, ALU.mult, None, ('m8',), ('nmx',))
                        act(lg[:], lg[:], AF.Exp, ('lg', 'nmx'), ('lg',), bias=nmx[:])
                        tt('dve', lg[:], lg[:], mk[:], ALU.mult, ('lg', 'mk'), ('lg',))
                        S.add('dve', lambda e: e.reduce_sum(out=sm[:], in_=lg[:], axis=mybir.AxisListType.X), ('lg',), ('sm',))
                        rec(sm[:], sm[:], ('sm',), ('sm',))
                        ts('dve', G[:, t_, :], lg[:], sm[:, 0:1], None, ALU.mult, None, ('lg', 'sm'), ('G',))
                    S.barrier()
                with ExitStack() as ph2:
                    wgu = [sb('wgu%d' % i, [128, 8, 2 * D], BF, ph2) for i in range(2)]
                    wdn = sb('wdn', [128, 8, D], BF, ph2)
                    bgu = sb('bgu', [128, NE, 16], F32, ph2)
                    S.dma('sp', bgu[:], I['bguT'][li], (), ('bgu',))
                    ts('dve', bgu[:, :, 8:16], bgu[:, :, 8:16], 1.0, None, ALU.add, None, ('bgu',), ('bgu',))
                    actT = [sb('actT%d' % i, [128, 8, 512], BF, ph2) for i in range(2)]
                    g1 = [sb('g1%d' % i, [128, 512], F32, ph2) for i in range(2)]; sgm = sb('sgm', [128, 512], BF, ph2); u1 = sb('u1', [128, 512], F32, ph2)
                    CHM = [(256 + 512 * j, 512) for j in range(4)] + [(0, 256)]

                    def load_gu(e_):
                        wgv = I['w_gate_up'][li, e_].rearrange("(kc p) n -> p kc n", p=128)
                        for kc in range(8):
                            S.dma('pool', wgu[e_ % 2][:, kc, :], wgv[:, kc, :], (), ('wgu%d_%d' % (e_ % 2, kc),))

                    def load_dn(e_):
                        S.dma('pool', wdn[:], I['w_down'][li, e_].rearrange("(kc p) n -> p kc n", p=128), (), ('wdn',))

                    def GU(e_, ci):
                        c0, n = CHM[ci]; a_ = actT[ci % 2]; ak = 'actT%d' % (ci % 2)
                        w = wgu[e_ % 2]; wk = 'wgu%d' % (e_ % 2)
                        for m in range(8):
                            pg, pgk = PS()
                            for kc in range(8):
                                mm(pg[:, 0:n], w[:, kc, m * 128:(m + 1) * 128], hT[:, kc, c0:c0 + n], kc == 0, kc == 7, (wk + '_%d' % kc, 'hT'), (pgk,))
                            pu, puk = PS()
                            for kc in range(8):
                                mm(pu[:, 0:n], w[:, kc, D + m * 128:D + (m + 1) * 128], hT[:, kc, c0:c0 + n], kc == 0, kc == 7, (wk + '_%d' % kc, 'hT'), (puk,))
                            b_ = m % 2
                            ts('dve', g1[b_][:, 0:n], pg[:, 0:n], bgu[:, e_, m:m + 1], 7.0, ALU.add, ALU.min, (pgk, 'bgu'), ('g1%d' % b_,))
                            act(sgm[:, 0:n], g1[b_][:, 0:n], AF.Sigmoid, ('g1%d' % b_,), ('sgm',), scale=1.702)
                            if m > 0:
                                q_ = (m - 1) % 2
                                stt(a_[:, m - 1, 0:n], u1[:, 0:n], -6.0, g1[q_][:, 0:n], ALU.max, ALU.mult, ('u1', 'g1%d' % q_), (ak,))
                            ts('dve', u1[:, 0:n], pu[:, 0:n], bgu[:, e_, 8 + m:9 + m], 8.0, ALU.add, ALU.min, (puk, 'bgu'), ('u1',))
                            tt('pool', g1[b_][:, 0:n], g1[b_][:, 0:n], sgm[:, 0:n], ALU.mult, ('g1%d' % b_, 'sgm'), ('g1%d' % b_,))
                        stt(a_[:, 7, 0:n], u1[:, 0:n], -6.0, g1[1][:, 0:n], ALU.max, ALU.mult, ('u1', 'g11'), (ak,))

                    def DN(e_, ci):
                        c0, n = CHM[ci]; a_ = actT[ci % 2]; ak = 'actT%d' % (ci % 2)
                        for tl_ in range(n // 128):
                            tt_ = c0 // 128 + tl_
                            for hf in range(2):
                                p_, pk = PS()
                                for kc in range(8):
                                    mm(p_[:, 0:512], a_[:, kc, tl_ * 128:(tl_ + 1) * 128], wdn[:, kc, hf * 512:(hf + 1) * 512], kc == 0, kc == 7, (ak, 'wdn'), (pk,))
                                sl_ = macc[:, tt_, hf * 512:(hf + 1) * 512]
                                stt(sl_, p_[:, 0:512], G[:, tt_, e_:e_ + 1], sl_, ALU.mult, ALU.add, (pk, 'G', 'macc%d' % tt_), ('macc%d' % tt_,))

                    load_gu(0); load_dn(0)
                    for e_ in range(NE):
                        if e_ + 1 < NE: load_gu(e_ + 1)
                        for ci in range(5):
                            GU(e_, ci)
                            if ci > 0: DN(e_, ci - 1)
                        DN(e_, 4)
                        if e_ + 1 < NE: load_dn(e_ + 1)
                        S.flush()
                    S.barrier()
                with ExitStack() as ph3:
                    modb = sb('modb5', [128, 2, D], F32, ph3)
                    mod_bcast(5, modb)
                    xr = [sb('xq%d' % i, [128, D], F32, ph3) for i in range(2)]
                    bdn = sb('bdn', [32, D], F32, ph3); GT = sb('GT', [32, 128], F32, ph3)
                    S.dma('sp', bdn[:], I['b_down'][li], (), ('bdn',))
                    for t_ in range(NT):
                        b_ = t_ % 2; v = 1 if t_ < 2 else 0
                        pg_, pgk_ = PS()
                        tr(pg_[0:32, 0:128], G[:, t_, :], ident_f[:], ('G', 'ident_f'), (pgk_,))
                        cp('act', GT[:], pg_[0:32, 0:128], (pgk_,), ('GT',))
                        for hf in range(2):
                            pb_, pbk_ = PS()
                            mm(pb_[:, 0:512], GT[:], bdn[:, hf * 512:(hf + 1) * 512], True, True, ('GT', 'bdn'), (pbk_,))
                            tt('dve', macc[:, t_, hf * 512:(hf + 1) * 512], macc[:, t_, hf * 512:(hf + 1) * 512], pb_[:, 0:512], ALU.add, (pbk_, 'macc%d' % t_), ('macc%d' % t_,))
                        S.dma('sp', xr[b_][:], xd[t_ * 128:(t_ + 1) * 128, :], ('xd%d' % t_,), ('xq%d' % b_,))
                        tt('dve', macc[:, t_, :], macc[:, t_, :], modb[:, v, :], ALU.mult, ('macc%d' % t_, 'modb'), ('macc%d' % t_,))
                        tt('pool', xr[b_][:], xr[b_][:], macc[:, t_, :], ALU.add, ('xq%d' % b_, 'macc%d' % t_), ('xq%d' % b_,))
                        if li == nlayers - 1:
                            if t_ >= 2:
                                S.dma('sp', yout[(t_ - 2) * 128:(t_ - 1) * 128, :], xr[b_][:], ('xq%d' % b_,), ('yout%d' % t_,))
                        else:
                            S.dma('sp', xd[t_ * 128:(t_ + 1) * 128, :], xr[b_][:], ('xq%d' % b_,), ('xd%d' % t_,))
                    S.barrier()
        S.barrier()
    return nc, consts, list(I.keys())


def make_in_maps(inputs, cores, nc_consts, names=None):
    maps = []
    big = {k_: np.ascontiguousarray(inputs[k_], dtype=np.float32) for k_ in BIG if names is None or k_ in names}
    for b in cores:
        m = {'x': np.ascontiguousarray(inputs['x'][b]), 'ctx': np.ascontiguousarray(inputs['ctx'][b])}
        m.update(nc_consts)
        m.update(host_layout(inputs, b))
        m.update(big)
        if names is not None: m = {k_: v for k_, v in m.items() if k_ in names}
        maps.append(m)
    return maps


def kernel(**inputs):
    inputs = {k_: np.asarray(v) for k_, v in inputs.items()}
    nc, consts, names = build(DEPTH)
    maps = make_in_maps(inputs, list(range(8)), consts, names)
    res = run_bass_kernel_spmd(nc, maps, core_ids=list(range(8)))
    return np.stack([r['y'] for r in res.results], 0).astype(np.float32)
```

```python
import numpy as np
from contextlib import ExitStack
import concourse.bass as bass
import concourse.mybir as mybir
from concourse.bass_utils import run_bass_kernel_spmd

F32 = mybir.dt.float32
BF = mybir.dt.bfloat16
ALU = mybir.AluOpType
AF = mybir.ActivationFunctionType

D = 1024; L_LAT = 2048; C_CTX = 256; T = 2304; NT = 18; DEPTH = 4
NE = 32; INW = 5888
CH = [(0, 256)] + [(256 + 512 * j, 512) for j in range(4)]
EPS = 1e-6
ND = 12
SAME_ENGINE_SYNC = True


def bl(ap, n):
    return bass.AP(ap.tensor, ap.offset, list(ap.ap) + [(0, n)])


def bmid(ap, n):
    a = list(ap.ap)
    return bass.AP(ap.tensor, ap.offset, [a[0], (0, n)] + a[1:])


class Sched:
    def __init__(s, nc, block, stack):
        s.nc = nc; s.block = block
        s.engs = {'pe': nc.tensor, 'act': nc.scalar, 'dve': nc.vector, 'pool': nc.gpsimd, 'sp': nc.sync}
        s.tl = {e: stack.enter_context(nc.semaphore('tl_' + e)) for e in ['pe', 'act', 'dve', 'pool']}
        s.cnt = {e: 0 for e in s.tl}
        s.dsem = {q: [stack.enter_context(nc.semaphore('d_%s%d' % (q, i))) for i in range(ND)] for q in ['sp', 'pool']}
        s.dval = {q: [0] * ND for q in s.dsem}; s.dnext = {q: 0 for q in s.dsem}
        s.waited = {e: {} for e in s.engs}
        s.lw = {}; s.rd = {}
        s.pend = {e: [] for e in s.engs}
        s.n = 0

    def _waits(s, eng, R, W, fast=False):
        deps = {}
        def need(tok):
            if tok is None: return
            name, sem, val, src = tok[:4]
            if src == eng and (eng in ('pe', 'sp') or not SAME_ENGINE_SYNC): return
            if src == eng and fast and len(tok) > 4 and tok[4]: return
            if deps.get(name, (None, 0))[1] < val: deps[name] = (sem, val)
        for k in R: need(s.lw.get(k))
        for k in W:
            need(s.lw.get(k))
            for t in s.rd.get(k, ()): need(t)
        out = []
        for name, (sem, val) in deps.items():
            if s.waited[eng].get(name, 0) < val:
                s.waited[eng][name] = val; out.append((sem, val))
        return out

    def _mark(s, tok, R, W):
        for k in R: s.rd.setdefault(k, []).append(tok)
        for k in W: s.lw[k] = tok; s.rd[k] = []

    def add(s, eng, fn, R=(), W=(), fast=False):
        waits = s._waits(eng, R, W, fast)
        s.cnt[eng] += 1
        tok = ('tl_' + eng, s.tl[eng], s.cnt[eng], eng, fast)
        s.pend[eng].append((waits, fn, (s.tl[eng], 1)))
        s._mark(tok, R, W); s.n += 1

    def dma(s, q, out, in_, R=(), W=(), **kw):
        waits = s._waits(q, R, W)
        i = s.dnext[q]; s.dnext[q] = (i + 1) % ND
        sem = s.dsem[q][i]; name = 'd_%s%d' % (q, i)
        if s.dval[q][i] > s.waited[q].get(name, 0):
            s.waited[q][name] = s.dval[q][i]; waits.append((sem, s.dval[q][i]))
        s.dval[q][i] += 16
        tok = (name, sem, s.dval[q][i], 'dma')
        s.pend[q].append((waits, lambda e: e.dma_start(out=out, in_=in_, **kw), (sem, 16)))
        s._mark(tok, R, W); s.n += 1

    def barrier(s):
        for eng in s.engs:
            waits = []
            for e2 in s.tl:
                name = 'tl_' + e2
                if s.cnt[e2] > s.waited[eng].get(name, 0) and not (e2 == eng == 'pe'):
                    s.waited[eng][name] = s.cnt[e2]; waits.append((s.tl[e2], s.cnt[e2]))
            for q in s.dsem:
                for i in range(ND):
                    name = 'd_%s%d' % (q, i)
                    if s.dval[q][i] > s.waited[eng].get(name, 0):
                        s.waited[eng][name] = s.dval[q][i]; waits.append((s.dsem[q][i], s.dval[q][i]))
            if waits: s.pend[eng].append((waits, None, None))
        s.flush()

    def wait_all(s, eng, keys):
        waits = s._waits(eng, keys, ())
        s.pend[eng].append((waits, None, None))

    def flush(s):
        for ename, lst in s.pend.items():
            if not lst: continue
            def body(e, lst=lst):
                for waits, fn, inc in lst:
                    for sem, val in waits: e.wait_ge(sem, val)
                    if fn is not None:
                        ins = fn(e)
                        ins.then_inc(inc[0], inc[1])
            getattr(s.block, {'pe': 'tensor', 'act': 'scalar', 'dve': 'vector', 'pool': 'gpsimd', 'sp': 'sync'}[ename])(body)
            s.pend[ename] = []


def na_start(r): return min(max(r - 4, 0), 24)

def na_tiles(i):
    rows = set()
    for r in (2 * i, 2 * i + 1):
        st = na_start(r); rows.update(range(st, st + 8))
    return sorted(set(r // 2 for r in rows))

def na_mask(i, j):
    m = np.zeros((128, 128), np.float32)
    k = np.arange(128); rk = k // 64; ck = k % 64
    for q in range(128):
        rq = q // 64; cq = q % 64
        st = na_start(2 * i + rq)
        row_ok = (2 * j + rk >= st) & (2 * j + rk <= st + 7)
        cs = min(max(cq - 8, 0), 48)
        col_ok = (ck >= cs) & (ck < cs + 16)
        m[:, q] = (row_ok & col_ok)
    return m

_NA_MASKS = None
def na_mask_table():
    global _NA_MASKS
    if _NA_MASKS is None:
        uniq = []; idx = {}
        for i in range(16):
            for j in na_tiles(i):
                m = na_mask(i, j)
                for u, mm in enumerate(uniq):
                    if np.array_equal(mm, m): idx[(i, j)] = u; break
                else:
                    uniq.append(m); idx[(i, j)] = len(uniq) - 1
        _NA_MASKS = (np.stack(uniq), idx)
    return _NA_MASKS


def host_consts():
    bf = mybir.dt.np(BF)
    c = {}
    c['ident_f'] = np.eye(128, dtype=np.float32)
    c['ident_b'] = np.eye(128, dtype=np.float32).astype(bf)
    bo = np.zeros((128, 128), np.float32); bo[:64, :64] = 1; bo[64:, 64:] = 1
    c['blockones'] = bo.astype(bf)
    c['ones_f'] = np.ones((128, 128), np.float32)
    R = np.zeros((64, 64), np.float32)
    for base in (0, 32):
        for i in range(16):
            R[base + i, base + 16 + i] = -1.0
            R[base + 16 + i, base + i] = 1.0
    R2 = np.zeros((128, 128), np.float32); R2[:64, :64] = R; R2[64:, 64:] = R
    c['rotT'] = np.ascontiguousarray(R2.T).astype(bf)
    pos = np.arange(L_LAT); row = pos // 64; col = pos % 64
    inv = (10000.0 ** (-np.arange(16, dtype=np.float32) / 16)).astype(np.float32)
    ang = np.zeros((64, L_LAT), np.float32)
    for dd in range(64):
        p = row if dd < 32 else col
        ang[dd] = p.astype(np.float32) * inv[dd % 16]
    c['cosT'] = np.concatenate([np.cos(ang), np.cos(ang)], 0).astype(bf)
    c['sinT'] = np.concatenate([np.sin(ang), np.sin(ang)], 0).astype(bf)
    b = np.arange(128)[:, None]; a = np.arange(128)[None, :]
    c['wmask'] = np.stack([(a <= b), (b <= a)]).astype(np.float32).astype(bf)
    c['namask'] = na_mask_table()[0].astype(bf)
    mz = np.zeros((4, 128, 128), np.float32); my = np.zeros((4, 128, 128), np.float32)
    for gl in range(4):
        for g2 in range(2):
            mz[gl, 64 * g2:64 * g2 + 64, 32 * gl + 16 * g2: 32 * gl + 16 * g2 + 16] = 1
            my[gl, 32 * gl + 16 * g2: 32 * gl + 16 * g2 + 16, 64 * g2:64 * g2 + 64] = 1
    c['maskZ'] = mz; c['maskY'] = my
    return c


def host_layout(inp, b):
    o = {}
    cb = inp['c'][b].reshape(8, 128).T; cc = inp['c_ctx'].reshape(8, 128).T
    o['cT'] = np.ascontiguousarray(np.stack([cb, cc], -1))
    o['bmodT'] = np.ascontiguousarray(inp['b_mod'].reshape(DEPTH, 48, 128).transpose(0, 2, 1))
    o['gmix'] = np.ascontiguousarray(inp['norm_mix'].reshape(DEPTH, 8, 128).transpose(0, 2, 1))
    o['gffn'] = np.ascontiguousarray(inp['norm_ffn'].reshape(DEPTH, 8, 128).transpose(0, 2, 1))
    def p2(x):
        return np.ascontiguousarray(x.reshape(DEPTH, 2, 16, 2, 64).transpose(0, 3, 4, 1, 2).reshape(DEPTH, 128, 32))
    o['lamre'] = p2(inp['ssm_lam_re']); o['lamim'] = p2(inp['ssm_lam_im'])
    o['logdt'] = p2(np.broadcast_to(inp['ssm_log_dt'][..., None], (DEPTH, 2, 32, 64)))
    def pb(x):
        return np.ascontiguousarray(x.reshape(DEPTH, 2, 16, 2, 64, 16).transpose(0, 3, 4, 1, 2, 5).reshape(DEPTH, 128, 32, 16))
    o['bre'] = pb(inp['ssm_b_re']); o['bim'] = pb(inp['ssm_b_im'])
    def pc(x):
        return np.ascontiguousarray(x.reshape(DEPTH, 2, 4, 8, 16, 64).transpose(0, 3, 4, 1, 2, 5).reshape(DEPTH, 128, 8, 64))
    o['cre'] = pc(inp['ssm_c_re']); o['cim'] = pc(inp['ssm_c_im'])
    o['dskip'] = np.ascontiguousarray(inp['ssm_d'].reshape(DEPTH, 4, 128).transpose(0, 2, 1))
    def hd(x): return np.ascontiguousarray(np.tile(x, (1, 2))[:, :, None])
    o['gwq'] = hd(inp['win_q_norm']); o['gwk'] = hd(inp['win_k_norm'])
    o['gnq'] = hd(inp['na_q_norm']); o['gnk'] = hd(inp['na_k_norm'])
    o['sink'] = np.ascontiguousarray(np.broadcast_to(inp['win_sink'][:, None, :], (DEPTH, 128, 8)))
    k = np.arange(128); rk = k // 64; ck = k % 64
    rpb = inp['na_rpb']
    dc = np.clip(ck[:, None] - ck[None, :], -15, 15) + 15
    B = np.zeros((DEPTH, 7, 128, 8, 128), np.float32)
    for di, dl in enumerate(range(-3, 4)):
        dr = np.clip(2 * dl + rk[:, None] - rk[None, :] + 7, 0, 14)
        dcf = dc[ck[:, None], ck[None, :]]
        B[:, di] = rpb[:, :, dr, dcf].transpose(0, 2, 1, 3)
    o['nabias'] = B
    o['rbias'] = np.ascontiguousarray(np.broadcast_to(inp['router_b'][:, None, :], (DEPTH, 128, NE)))
    o['bguT'] = np.ascontiguousarray(inp['b_gate_up'].reshape(DEPTH, NE, 16, 128).transpose(0, 3, 1, 2))
    return o


SMALL_SHAPES = {
    'cT': [128, 8, 2], 'bmodT': [DEPTH, 128, 48], 'gmix': [DEPTH, 128, 8], 'gffn': [DEPTH, 128, 8],
    'lamre': [DEPTH, 128, 32], 'lamim': [DEPTH, 128, 32], 'logdt': [DEPTH, 128, 32],
    'bre': [DEPTH, 128, 32, 16], 'bim': [DEPTH, 128, 32, 16], 'cre': [DEPTH, 128, 8, 64], 'cim': [DEPTH, 128, 8, 64],
    'dskip': [DEPTH, 128, 4], 'gwq': [DEPTH, 128, 1], 'gwk': [DEPTH, 128, 1], 'gnq': [DEPTH, 128, 1], 'gnk': [DEPTH, 128, 1],
    'sink': [DEPTH, 128, 8], 'nabias': [DEPTH, 7, 128, 8, 128], 'rbias': [DEPTH, 128, NE], 'bguT': [DEPTH, 128, NE, 16],
}
BIG = {'w_mod': [DEPTH, D, 6 * D], 'w_in': [DEPTH, D, INW], 'ssm_w_glu': [DEPTH, 512, 512],
       'w_branch': [DEPTH, 3, 512, D], 'w_out': [DEPTH, D, D], 'router_w': [DEPTH, D, NE],
       'w_gate_up': [DEPTH, NE, D, 2 * D], 'w_down': [DEPTH, NE, D, D], 'b_down': [DEPTH, NE, D]}


def build(nlayers=DEPTH, dbg=None, stop_after=None):
    nc = bass.Bass("TRN2", target_bir_lowering=False, dynamic_dma_scratch_size=4096)
    consts = host_consts()
    shapes = {'x': ([L_LAT, D], F32), 'ctx': ([C_CTX, D], F32)}
    for k_, v in consts.items(): shapes[k_] = (list(v.shape), BF if v.dtype != np.float32 else F32)
    for k_, v in SMALL_SHAPES.items(): shapes[k_] = (v, F32)
    for k_, v in BIG.items(): shapes[k_] = ([nlayers] + v[1:], F32)
    class LazyI(dict):
        def __missing__(self, name):
            shp, dt = shapes[name]
            self[name] = nc.dram_tensor(name, list(shp), dt, kind="ExternalInput").ap()
            return self[name]
    I = LazyI()
    yout = nc.dram_tensor('y', [L_LAT, D], F32, kind="ExternalOutput").ap()
    xd = nc.dram_tensor('xd', [T, D], F32, kind="Internal").ap()
    dbg_out = {}
    if dbg:
        for k_, shp in dbg.items():
            dbg_out[k_] = nc.dram_tensor('dbg_' + k_, list(shp), F32, kind="ExternalOutput").ap()

    with ExitStack() as st:
        sbn = [0]
        def sb(name, shape, dt=F32, stack=None):
            sbn[0] += 1
            return (stack or st).enter_context(nc.sbuf_tensor('s%d_%s' % (sbn[0], name), list(shape), dt))
        ident_f = sb('ident_f', [128, 128]); ident_b = sb('ident_b', [128, 128], BF)
        blockones = sb('blockones', [128, 128], BF); ones_f = sb('ones_f', [128, 128])
        nmk, nidx = na_mask_table(); NM = nmk.shape[0]
        epsT = sb('epsT', [128, 1]); halfpi = sb('halfpi', [128, 1]); hm = sb('hm', [128, 2])
        sc = sb('sc', [128, 8, 2])
        modc = sb('modc', [128, 48, 2])
        s1 = sb('s1', [128, 8, 2]); s0 = sb('s0', [128, 8, 2])
        hT = sb('hT', [128, 8, T], BF)
        ps = [st.enter_context(nc.psum_tensor('ps%d' % i, [128, 512], F32)) for i in range(6)]
        pT = [st.enter_context(nc.psum_tensor('pT%d' % i, [128, 1024], BF)) for i in range(2)]
        block = st.enter_context(nc.Block())
        S = Sched(nc, block, st)
        psn = [0]
        def PS():
            i = psn[0] % 6; psn[0] += 1
            return ps[i], 'ps%d' % i
        ptn = [0]
        def PT_():
            i = ptn[0] % 2; ptn[0] += 1
            return pT[i], 'pT%d' % i

        def act(out, in_, func, R, W, **kw): S.add('act', lambda e: e.activation(out=out, in_=in_, func=func, **kw), R, W)
        def ts(eng, out, in0, s1_, s2_, op0, op1, R, W):
            if op1 is None: S.add(eng, lambda e: e.tensor_scalar(out=out, in0=in0, scalar1=s1_, scalar2=None, op0=op0), R, W)
            else: S.add(eng, lambda e: e.tensor_scalar(out=out, in0=in0, scalar1=s1_, scalar2=s2_, op0=op0, op1=op1), R, W)
        def tt(eng, out, in0, in1, op, R, W): S.add(eng, lambda e: e.tensor_tensor(out=out, in0=in0, in1=in1, op=op), R, W)
        def stt(out, in0, scalar, in1, op0, op1, R, W, fast=False):
            S.add('dve', lambda e: e.scalar_tensor_tensor(out=out, in0=in0, scalar=scalar, in1=in1, op0=op0, op1=op1), R, W, fast)
        def mm(out, lhsT, rhs, start, stop, R, W): S.add('pe', lambda e: e.matmul(out, lhsT, rhs, start=start, stop=stop), R, W)
        def tr(out, in_, ident, R, W): S.add('pe', lambda e: e.transpose(out, in_, ident), R, W)
        def cp(eng, out, in_, R, W):
            if eng == 'act': S.add('act', lambda e: e.copy(out=out, in_=in_), R, W)
            else: S.add(eng, lambda e: e.tensor_copy(out=out, in_=in_), R, W)
        def ms(eng, ap, val, W): S.add(eng, lambda e: e.memset(ap, val), (), W)
        def rec(out, in_, R, W): S.add('dve', lambda e: e.reciprocal(out=out, in_=in_), R, W)

        for k_, t_ in [('ident_f', ident_f), ('ident_b', ident_b), ('blockones', blockones), ('ones_f', ones_f)]:
            S.dma('sp', t_[:], I[k_], (), (k_,))
        ms('dve', hm[:], 0.0, ('hm',)); ms('dve', hm[0:64, 0:1], 1.0, ('hm',)); ms('dve', hm[64:128, 1:2], 1.0, ('hm',))
        ms('dve', epsT[:], EPS, ('epsT',)); ms('dve', halfpi[:], float(np.pi / 2), ('halfpi',))
        S.dma('sp', xd[0:C_CTX, :], I['ctx'], (), ('xd0', 'xd1'))
        S.dma('sp', xd[C_CTX:T, :], I['x'], (), tuple('xd%d' % t for t in range(2, NT)))
        S.dma('sp', sc[:], I['cT'], (), ('sc',))
        act(sc[:], sc[:], AF.Silu, ('sc',), ('sc',))
        S.flush()

        for li in range(nlayers):
            with ExitStack() as ph:
                wm = [sb('wm%d' % i, [128, 8, 512], F32, ph) for i in range(2)]
                bmod = sb('bmod', [128, 48], F32, ph)
                S.dma('sp', bmod[:], I['bmodT'][li], (), ('bmod',))
                wv = I['w_mod'][li].rearrange("(kc p) n -> p kc n", p=128)
                for blk in range(12):
                    w = wm[blk % 2]; wk = 'wm%d' % (blk % 2)
                    S.dma('sp', w[:], wv[:, :, blk * 512:(blk + 1) * 512], (), (wk,))
                    p_, pk = PS()
                    for j in range(4):
                        for kc in range(8):
                            mm(p_[:, 2 * j:2 * j + 2], w[:, kc, j * 128:(j + 1) * 128], sc[:, kc, :], kc == 0, kc == 7, (wk, 'sc'), (pk,))
                    tt('dve', modc[:, blk * 4:blk * 4 + 4, :], p_[:, 0:8].rearrange("p (j c) -> p j c", c=2),
                       bl(bmod[:, blk * 4:blk * 4 + 4], 2), ALU.add, (pk, 'bmod'), ('modc',))
                S.barrier()
            gcol = sb('gcol', [128, 8], F32, st) if li == 0 else gcol
            if stop_after == 'adaln': break

            def mod_scale_shift(gname, jsc, jsh):
                S.dma('sp', gcol[:], I[gname][li], (), ('gcol',))
                ts('dve', s1[:], modc[:, 8 * jsc:8 * jsc + 8, :], 1.0, None, ALU.add, None, ('modc',), ('s1',))
                tt('dve', s1[:], s1[:], bl(gcol[:], 2), ALU.mult, ('s1', 'gcol'), ('s1',))
                cp('dve', s0[:], modc[:, 8 * jsh:8 * jsh + 8, :], ('modc',), ('s0',))

            def mod_bcast(jg, modb):
                with ExitStack() as ph:
                    dg = sb('dg', [128, 128], F32, ph)
                    for v in range(2):
                        for kc in range(8):
                            ts('dve', dg[:], ident_f[:], modc[:, 8 * jg + kc, v:v + 1], None, ALU.mult, None, ('ident_f', 'modc'), ('dg',))
                            p_, pk = PS()
                            mm(p_[:, 0:128], ones_f[:], dg[:], True, True, ('ones_f', 'dg'), (pk,))
                            cp('act', modb[:, v, kc * 128:(kc + 1) * 128], p_[:, 0:128], (pk,), ('modb',))
                    S.barrier()

            def norm_stage():
                with ExitStack() as ph:
                    xt = [sb('xt%d' % i, [128, D], F32, ph) for i in range(2)]
                    junk = sb('junk', [128, D], BF, ph); xn = [sb('xn%d' % i, [128, D], BF, ph) for i in range(2)]
                    ss = sb('ss', [128, NT], F32, ph)
                    for tt_ in range(NT):
                        b_ = tt_ % 2; v = 1 if tt_ < 2 else 0
                        S.dma('sp', xt[b_][:], xd[tt_ * 128:(tt_ + 1) * 128, :], ('xd%d' % tt_,), ('xt%d' % b_,))
                        act(junk[:], xt[b_][:], AF.Square, ('xt%d' % b_,), ('junk', 'ss%d' % tt_), accum_out=ss[:, tt_:tt_ + 1])
                        act(ss[:, tt_:tt_ + 1], ss[:, tt_:tt_ + 1], AF.Sqrt, ('ss%d' % tt_, 'epsT'), ('ss%d' % tt_,), bias=epsT[:], scale=1.0 / D)
                        rec(ss[:, tt_:tt_ + 1], ss[:, tt_:tt_ + 1], ('ss%d' % tt_,), ('ss%d' % tt_,))
                        ts('dve', xn[b_][:], xt[b_][:], ss[:, tt_:tt_ + 1], None, ALU.mult, None, ('xt%d' % b_, 'ss%d' % tt_), ('xn%d' % b_,))
                        p_, pk = PT_()
                        for kc in range(8):
                            tr(p_[:, kc * 128:(kc + 1) * 128], xn[b_][:, kc * 128:(kc + 1) * 128], ident_b[:], ('xn%d' % b_, 'ident_b'), (pk,))
                        for kc in range(8):
                            ts('dve', hT[:, kc, tt_ * 128:(tt_ + 1) * 128], p_[:, kc * 128:(kc + 1) * 128], s1[:, kc, v:v + 1], s0[:, kc, v:v + 1],
                               ALU.mult, ALU.add, (pk, 's1', 's0'), ('hT',))
                    S.barrier()

            win = I['w_in'][li].rearrange("(kc p) n -> p kc n", p=128)

            def load_w(wt, wkey, view, c0, n, dst0=0):
                S.dma('pool', wt[:, :, dst0:dst0 + n], view[:, :, c0:c0 + n], (), (wkey,))

            def proj_fm(wt, wkey, wc0, consumer, src=None, nk=8, srckey='hT'):
                src = hT if src is None else src
                for (c0, n) in CH:
                    p_, pk = PS()
                    for kc in range(nk):
                        mm(p_[:, 0:n], wt[:, kc, wc0:wc0 + 128], src[:, kc, c0:c0 + n], kc == 0, kc == nk - 1, (wkey,) + (srckey if isinstance(srckey, tuple) else (srckey,)), (pk,))
                    consumer(p_[:, 0:n], pk, c0, n)

            mod_scale_shift('gmix', 1, 0)
            norm_stage()
            if stop_after == 'norm': break
            with ExitStack() as mx:
                rotT = sb('rotT', [128, 128], BF, mx)
                cosT = sb('cosT', [128, L_LAT], BF, mx); sinT = sb('sinT', [128, L_LAT], BF, mx)
                wmask = sb('wmask', [128, 2, 128], BF, mx); namask = sb('namask', [128, NM, 128], BF, mx)
                maskZ = sb('maskZ', [128, 4, 128], F32, mx); maskY = sb('maskY', [128, 4, 128], F32, mx)
                for k_, t_ in [('rotT', rotT), ('cosT', cosT), ('sinT', sinT)]:
                    S.dma('sp', t_[:], I[k_], (), (k_,))
                S.dma('sp', maskZ[:], I['maskZ'].rearrange("g p c -> p g c"), (), ('maskZ',))
                S.dma('sp', maskY[:], I['maskY'].rearrange("g p c -> p g c"), (), ('maskY',))
                S.dma('sp', wmask[:], I['wmask'].rearrange("g p c -> p g c"), (), ('wmask',))
                S.dma('sp', namask[:], I['namask'].rearrange("g p c -> p g c"), (), ('namask',))
                yT0 = sb('yT0', [128, 4, T], BF, mx)
                yTbox = [None]
                class _YT:
                    def __getitem__(self, idx):
                        p, m, sl = idx
                        if isinstance(m, slice):
                            if m.start >= 4: return yTbox[0][p, m.start - 4:m.stop - 4, sl]
                            return yT0[p, m, sl]
                        if m >= 4: return yTbox[0][p, m - 4, sl]
                        return yT0[p, m, sl]
                yT = _YT()
                with ExitStack() as ph:
                    uT = sb('uT', [128, 4, T], BF, ph)
                    acc = sb('acc', [128, 4, T], F32, ph)
                    dsk = sb('dsk', [128, 4], F32, ph)
                    wa_scope = ExitStack()
                    wa = sb('wa', [128, 8, 512], BF, wa_scope)
                    S.dma('sp', dsk[:], I['dskip'][li], (), ('dsk',))
                    load_w(wa, 'wa', win, 0, 512)
                    for m in range(4):
                        def cons(p_, pk, c0, n, m=m):
                            import os
                            dbgv = int(os.environ.get('S5DBG', '0'))
                            if dbgv in (0, 2): cp('act', uT[:, m, c0:c0 + n], p_, (pk,), ('uT%d' % m,))
                            if dbgv in (0, 3): ts('dve', acc[:, m, c0:c0 + n], uT[:, m, c0:c0 + n], dsk[:, m:m + 1], None, ALU.mult, None, ('uT%d' % m, 'dsk'), ('acc%d' % m,))
                        proj_fm(wa, 'wa', m * 128, cons)
                    S.barrier(); wa_scope.close()
                    if stop_after == 's5a': break
                    P = {k_: sb('sp_' + k_, [128, 32], F32, ph) for k_ in ['lr', 'li', 'dt', 'mag', 'c', 's', 't1', 't2', 'ar', 'ai', 'fr', 'fi', 'den']}
                    S.dma('sp', P['lr'][:], I['lamre'][li], (), ('p_lr',)); S.dma('sp', P['li'][:], I['lamim'][li], (), ('p_li',))
                    S.dma('sp', P['dt'][:], I['logdt'][li], (), ('p_dt',))
                    K_ = ('sp',)
                    act(P['dt'][:], P['dt'][:], AF.Exp, ('p_dt',), ('p_dt',))
                    tt('dve', P['t1'][:], P['lr'][:], P['dt'][:], ALU.mult, ('p_lr', 'p_dt'), K_)
                    act(P['mag'][:], P['t1'][:], AF.Exp, K_, K_)
                    tt('dve', P['t2'][:], P['li'][:], P['dt'][:], ALU.mult, ('p_li', 'p_dt'), K_)
                    act(P['s'][:], P['t2'][:], AF.Sin, K_, K_, scale=1.0 / 16)
                    act(P['c'][:], P['t2'][:], AF.Sin, K_ + ('halfpi',), K_, scale=1.0 / 16, bias=halfpi[:])
                    for _ in range(4):
                        tt('dve', P['t1'][:], P['c'][:], P['c'][:], ALU.mult, K_, K_)
                        tt('dve', P['t2'][:], P['s'][:], P['s'][:], ALU.mult, K_, K_)
                        tt('dve', P['s'][:], P['s'][:], P['c'][:], ALU.mult, K_, K_)
                        ts('dve', P['s'][:], P['s'][:], 2.0, None, ALU.mult, None, K_, K_)
                        tt('dve', P['c'][:], P['t1'][:], P['t2'][:], ALU.subtract, K_, K_)
                    tt('dve', P['ar'][:], P['mag'][:], P['c'][:], ALU.mult, K_, K_)
                    tt('dve', P['ai'][:], P['mag'][:], P['s'][:], ALU.mult, K_, K_)
                    tt('dve', P['den'][:], P['lr'][:], P['lr'][:], ALU.mult, K_, K_)
                    tt('dve', P['t1'][:], P['li'][:], P['li'][:], ALU.mult, K_, K_)
                    tt('dve', P['den'][:], P['den'][:], P['t1'][:], ALU.add, K_, K_)
                    rec(P['den'][:], P['den'][:], K_, K_)
                    ts('dve', P['t1'][:], P['ar'][:], -1.0, None, ALU.add, None, K_, K_)
                    tt('dve', P['fr'][:], P['t1'][:], P['lr'][:], ALU.mult, K_, K_)
                    tt('dve', P['t2'][:], P['ai'][:], P['li'][:], ALU.mult, K_, K_)
                    tt('dve', P['fr'][:], P['fr'][:], P['t2'][:], ALU.add, K_, K_)
                    tt('dve', P['fr'][:], P['fr'][:], P['den'][:], ALU.mult, K_, K_)
                    tt('dve', P['fi'][:], P['ai'][:], P['lr'][:], ALU.mult, K_, K_)
                    tt('dve', P['t2'][:], P['t1'][:], P['li'][:], ALU.mult, K_, K_)
                    tt('dve', P['fi'][:], P['fi'][:], P['t2'][:], ALU.subtract, K_, K_)
                    tt('dve', P['fi'][:], P['fi'][:], P['den'][:], ALU.mult, K_, K_)
                    pwr = sb('pwr', [128, 12, 32], F32, ph); pwi = sb('pwi', [128, 12, 32], F32, ph); npwi = sb('npwi', [128, 12, 32], F32, ph)
                    cp('dve', pwr[:, 0, :], P['ar'][:], K_, K_); cp('dve', pwi[:, 0, :], P['ai'][:], K_, K_)
                    for i in range(1, 12):
                        tt('dve', P['t1'][:], pwr[:, i - 1, :], pwr[:, i - 1, :], ALU.mult, K_, K_)
                        tt('dve', P['t2'][:], pwi[:, i - 1, :], pwi[:, i - 1, :], ALU.mult, K_, K_)
                        tt('dve', pwr[:, i, :], P['t1'][:], P['t2'][:], ALU.subtract, K_, K_)
                        tt('dve', P['t1'][:], pwr[:, i - 1, :], pwi[:, i - 1, :], ALU.mult, K_, K_)
                        ts('dve', pwi[:, i, :], P['t1'][:], 2.0, None, ALU.mult, None, K_, K_)
                    ts('dve', npwi[:], pwi[:], -1.0, None, ALU.mult, None, K_, K_)
                    apr = sb('apr', [128, 8, 32], F32, ph); api = sb('api', [128, 8, 32], F32, ph); napi = sb('napi', [128, 8, 32], F32, ph)
                    for p_i, src_i in [(1, 0), (2, 1), (4, 2), (8, 3)]:
                        cp('dve', apr[:, p_i - 1, :], pwr[:, src_i, :], K_, K_); cp('dve', api[:, p_i - 1, :], pwi[:, src_i, :], K_, K_)
                    def cmul(pd, pa, pb):
                        tt('dve', P['t1'][:], apr[:, pa - 1, :], apr[:, pb - 1, :], ALU.mult, K_, K_)
                        tt('dve', P['t2'][:], api[:, pa - 1, :], api[:, pb - 1, :], ALU.mult, K_, K_)
                        tt('dve', apr[:, pd - 1, :], P['t1'][:], P['t2'][:], ALU.subtract, K_, K_)
                        tt('dve', P['t1'][:], apr[:, pa - 1, :], api[:, pb - 1, :], ALU.mult, K_, K_)
                        tt('dve', P['t2'][:], api[:, pa - 1, :], apr[:, pb - 1, :], ALU.mult, K_, K_)
                        tt('dve', api[:, pd - 1, :], P['t1'][:], P['t2'][:], ALU.add, K_, K_)
                    cmul(3, 2, 1); cmul(5, 4, 1); cmul(6, 4, 2); cmul(7, 4, 3)
                    ts('dve', napi[:], api[:], -1.0, None, ALU.mult, None, K_, K_)
                    br = sb('br', [128, 32, 16], F32, ph); bi = sb('bi', [128, 32, 16], F32, ph)
                    bbr = sb('bbr', [128, 32, 16], F32, ph); bbi = sb('bbi', [128, 32, 16], F32, ph); tb = sb('tb', [128, 32, 16], F32, ph)
                    S.dma('sp', br[:], I['bre'][li], (), K_); S.dma('sp', bi[:], I['bim'][li], (), K_)
                    frb = bl(P['fr'][:], 16); fib = bl(P['fi'][:], 16)
                    tt('dve', bbr[:], br[:], frb, ALU.mult, K_, K_); tt('dve', tb[:], bi[:], fib, ALU.mult, K_, K_)
                    tt('dve', bbr[:], bbr[:], tb[:], ALU.subtract, K_, K_)
                    tt('dve', bbi[:], bi[:], frb, ALU.mult, K_, K_); tt('dve', tb[:], br[:], fib, ALU.mult, K_, K_)
                    tt('dve', bbi[:], bbi[:], tb[:], ALU.add, K_, K_)
                    cn_r = sb('cn_r', [128, 8, 64], F32, ph); cn_i = sb('cn_i', [128, 8, 64], F32, ph)
                    S.dma('sp', cn_r[:], I['cre'][li], (), K_); S.dma('sp', cn_i[:], I['cim'][li], (), K_)
                    ts('dve', cn_i[:], cn_i[:], -1.0, None, ALU.mult, None, K_, K_)
                    Zt = [sb('Zt%d' % i, [128, 128], F32, ph) for i in range(2)]
                    BTr = [sb('BTr%d' % i, [128, 128], BF, ph) for i in range(2)]; BTi = [sb('BTi%d' % i, [128, 128], BF, ph) for i in range(2)]
                    CTr = [sb('CTr%d' % i, [128, 128], F32, ph) for i in range(2)]; CTi = [sb('CTi%d' % i, [128, 128], F32, ph) for i in range(2)]
                    KAr = [sb('kar%d' % i, [128, T], F32, ph) for i in range(2)]; KAi = [sb('kai%d' % i, [128, T], F32, ph) for i in range(2)]
                    KBs = [[sb('kb%d_%d' % (i, j), [128, 288], F32, ph) for j in range(4)] for i in range(2)]
                    S.barrier()
                    if stop_after == 's5b': break
                    NCH = T // 8

                    def gparams(idx):
                        d_ = idx // 16; gp = idx % 16
                        return d_, gp, d_ * 16 + gp, gp // 4, gp % 4, idx % 2

                    def pos(d_, c0):
                        if d_ == 0: return c0
                        return (c0 - C_CTX) if c0 >= C_CTX else L_LAT

                    def stageA(idx):
                        d_, gp, col, tile_, gl, b = gparams(idx)
                        zk = 'Zt%d' % b
                        for src_, dst_, dk in [(bbr, BTr[b], 'BTr%d' % b), (bbi, BTi[b], 'BTi%d' % b)]:
                            tt('dve', Zt[b][:].rearrange("p (a q) -> p a q", q=16), bmid(src_[:, col, :], 8),
                               maskZ[:, gl, :].rearrange("p (a q) -> p a q", q=16), ALU.mult, K_ + ('maskZ',), (zk,))
                            p_, pk = PS()
                            tr(p_[:, 0:128], Zt[b][:], ident_f[:], (zk, 'ident_f'), (pk,))
                            cp('act', dst_[:], p_[:, 0:128], (pk,), (dk,))
                        for src_, dst_, dk in [(cn_r, CTr[b], 'CTr%d' % b), (cn_i, CTi[b], 'CTi%d' % b)]:
                            tt('dve', Zt[b][:].rearrange("p (a n) -> p a n", n=64), bmid(src_[:, d_ * 4 + tile_, :], 2),
                               maskY[:, gl, :].rearrange("p (a n) -> p a n", n=64), ALU.mult, K_ + ('maskY',), (zk,))
                            p_, pk = PS()
                            tr(p_[:, 0:128], Zt[b][:], ident_f[:], (zk, 'ident_f'), (pk,))
                            cp('act', dst_[:], p_[:, 0:128], (pk,), (dk,))
                        for (c0, n) in CH:
                            for lh, dst_, lk, dk in [(BTr[b], KAr[b], 'BTr%d' % b, 'kar%d' % b), (BTi[b], KAi[b], 'BTi%d' % b, 'kai%d' % b)]:
                                p_, pk = PS()
                                mm(p_[:, 0:n], lh[:], uT[:, tile_, c0:c0 + n], True, True, (lk, 'uT%d' % tile_), (pk,))
                                cp('act', dst_[:, pos(d_, c0):pos(d_, c0) + n], p_[:, 0:n], (pk,), (dk,))

                    def stageB(idx):
                        d_, gp, col, tile_, gl, b = gparams(idx)
                        kr, ki = 'kar%d' % b, 'kai%d' % b
                        KB = KBs[b]
                        Rv = KAr[b][:].rearrange("p (j s) -> p j s", s=8); Iv = KAi[b][:].rearrange("p (j s) -> p j s", s=8)
                        c1r = apr[:, 0, col:col + 1]; c1i = api[:, 0, col:col + 1]; nc1i = napi[:, 0, col:col + 1]
                        order = range(1, 8) if d_ == 0 else range(6, -1, -1)
                        for s_ in order:
                            q_ = s_ - 1 if d_ == 0 else s_ + 1
                            stt(Rv[:, :, s_], Rv[:, :, q_], c1r, Rv[:, :, s_], ALU.mult, ALU.add, (kr,) + K_, (kr,), fast=True)
                            stt(Rv[:, :, s_], Iv[:, :, q_], nc1i, Rv[:, :, s_], ALU.mult, ALU.add, (kr, ki) + K_, (kr,), fast=True)
                            stt(Iv[:, :, s_], Iv[:, :, q_], c1r, Iv[:, :, s_], ALU.mult, ALU.add, (ki,) + K_, (ki,), fast=True)
                            stt(Iv[:, :, s_], Rv[:, :, q_], c1i, Iv[:, :, s_], ALU.mult, ALU.add, (kr, ki) + K_, (ki,), fast=True)
                        es = 7 if d_ == 0 else 0
                        kbn = ['kb%d_%d' % (b, j) for j in range(4)]
                        cp('act', KB[0][:], Rv[:, :, es], (kr,), (kbn[0],))
                        cp('act', KB[1][:], Iv[:, :, es], (ki,), (kbn[1],))
                        cur = 0
                        for i in range(9):
                            k = 1 << i
                            sr, si = KB[cur], KB[cur + 1]; dr_, di_ = KB[2 - cur], KB[3 - cur]
                            skr, ski = kbn[cur], kbn[cur + 1]; dkr, dki = kbn[2 - cur], kbn[3 - cur]
                            cr = pwr[:, 3 + i, col:col + 1]; ci = pwi[:, 3 + i, col:col + 1]; nci = npwi[:, 3 + i, col:col + 1]
                            if d_ == 0: dst_sl = slice(k, NCH); src_sl = slice(0, NCH - k); keep = slice(0, k)
                            else: dst_sl = slice(0, NCH - k); src_sl = slice(k, NCH); keep = slice(NCH - k, NCH)
                            cp('act', dr_[:, keep], sr[:, keep], (skr, skr + 'k'), (dkr + 'k',))
                            cp('act', di_[:, keep], si[:, keep], (ski, ski + 'k'), (dki + 'k',))
                            stt(dr_[:, dst_sl], sr[:, src_sl], cr, sr[:, dst_sl], ALU.mult, ALU.add, (skr, skr + 'k') + K_, (dkr,), fast=True)
                            stt(dr_[:, dst_sl], si[:, src_sl], nci, dr_[:, dst_sl], ALU.mult, ALU.add, (ski, ski + 'k', dkr) + K_, (dkr,), fast=True)
                            stt(di_[:, dst_sl], si[:, src_sl], cr, si[:, dst_sl], ALU.mult, ALU.add, (ski, ski + 'k') + K_, (dki,), fast=True)
                            stt(di_[:, dst_sl], sr[:, src_sl], ci, di_[:, dst_sl], ALU.mult, ALU.add, (skr, skr + 'k', dki) + K_, (dki,), fast=True)
                            cur = 2 - cur
                        Hr, Hi = KB[cur], KB[cur + 1]; hkr_, hki_ = kbn[cur], kbn[cur + 1]
                        for s_ in range(8):
                            pw_ = (s_ + 1) if d_ == 0 else (8 - s_)
                            fr_ = apr[:, pw_ - 1, col:col + 1]; fi_ = api[:, pw_ - 1, col:col + 1]; nfi_ = napi[:, pw_ - 1, col:col + 1]
                            if d_ == 0: osl = slice(1, NCH); hsl = slice(0, NCH - 1)
                            else: osl = slice(0, NCH - 1); hsl = slice(1, NCH)
                            hdeps = (hkr_, hki_, hkr_ + 'k', hki_ + 'k') + K_
                            stt(Rv[:, osl, s_], Hr[:, hsl], fr_, Rv[:, osl, s_], ALU.mult, ALU.add, (kr,) + hdeps, (kr,), fast=True)
                            stt(Rv[:, osl, s_], Hi[:, hsl], nfi_, Rv[:, osl, s_], ALU.mult, ALU.add, (kr,) + hdeps, (kr,), fast=True)
                            stt(Iv[:, osl, s_], Hi[:, hsl], fr_, Iv[:, osl, s_], ALU.mult, ALU.add, (ki,) + hdeps, (ki,), fast=True)
                            stt(Iv[:, osl, s_], Hr[:, hsl], fi_, Iv[:, osl, s_], ALU.mult, ALU.add, (ki,) + hdeps, (ki,), fast=True)

                    def stageC(idx):
                        d_, gp, col, tile_, gl, b = gparams(idx)
                        for (c0, n) in CH:
                            p_, pk = PS()
                            mm(p_[:, 0:n], CTr[b][:], KAr[b][:, pos(d_, c0):pos(d_, c0) + n], True, False, ('CTr%d' % b, 'kar%d' % b), (pk,))
                            mm(p_[:, 0:n], CTi[b][:], KAi[b][:, pos(d_, c0):pos(d_, c0) + n], False, True, ('CTi%d' % b, 'kai%d' % b), (pk,))
                            tt('dve', acc[:, tile_, c0:c0 + n], acc[:, tile_, c0:c0 + n], p_[:, 0:n], ALU.add, (pk, 'acc%d' % tile_), ('acc%d' % tile_,))

                    NG = 32 if stop_after != 's5c' else 2
                    stageA(0)
                    for idx in range(NG):
                        if idx + 1 < NG: stageA(idx + 1)
                        stageB(idx)
                        stageC(idx)
                        S.flush()
                    if dbg and 'acc' in dbg_out and li == 0:
                        S.dma('sp', dbg_out['acc'].rearrange("(m p) t -> p (m t)", p=128), acc[:].rearrange("p m t -> p (m t)"), ['acc%d' % m for m in range(4)], ())
                    wg = sb('wg', [128, 4, 512], BF, ph)
                    S.dma('pool', wg[:], I['ssm_w_glu'][li].rearrange("(kc p) n -> p kc n", p=128), (), ('wg',))
                    t3 = KAr[0]; gT = uT
                    for m in range(4):
                        a_ = acc[:, m, :]
                        act(t3[:], a_, AF.Square, ('acc%d' % m,), ('kar0',))
                        ts('dve', t3[:], t3[:], 0.044715, 1.0, ALU.mult, ALU.add, ('kar0',), ('kar0',))
                        tt('dve', t3[:], t3[:], a_, ALU.mult, ('kar0', 'acc%d' % m), ('kar0',))
                        act(t3[:], t3[:], AF.Sigmoid, ('kar0',), ('kar0',), scale=1.5957691216057308)
                        tt('dve', gT[:, m, :], t3[:], a_, ALU.mult, ('kar0', 'acc%d' % m), ('uT%d' % m,))
                    for m in range(4):
                        def cons(p_, pk, c0, n, m=m):
                            act(KAi[0][:, c0:c0 + n], p_, AF.Sigmoid, (pk,), ('kai0',))
                            tt('dve', yT[:, m, c0:c0 + n], gT[:, m, c0:c0 + n], KAi[0][:, c0:c0 + n], ALU.mult, ('kai0', 'uT%d' % m), ('yT%d' % m,))
                        proj_fm(wg, 'wg', m * 128, cons, src=gT, nk=4, srckey=('uT0', 'uT1', 'uT2', 'uT3'))
                    S.barrier()

                if stop_after == 's5': break
                yTbox[0] = sb('yT2', [128, 8, T], BF, mx)
                def qk_prep(dst, dkey, wt, wkey, wc0, gcol_ap, gkey, rope, tmp):
                    raw, sq, rs, qn, t1 = tmp
                    for (c0, n) in CH:
                        p_, pk = PS()
                        for kc in range(8):
                            mm(p_[:, 0:n], wt[:, kc, wc0:wc0 + 128], hT[:, kc, c0:c0 + n], kc == 0, kc == 7, (wkey, 'hT'), (pk,))
                        cp('act', raw[:, 0:n], p_[:, 0:n], (pk,), ('raw',))
                        act(sq[:, 0:n], p_[:, 0:n], AF.Square, (pk,), ('sq',))
                        p2, pk2 = PS()
                        mm(p2[:, 0:n], blockones[:], sq[:, 0:n], True, True, ('blockones', 'sq'), (pk2,))
                        act(rs[:, 0:n], p2[:, 0:n], AF.Sqrt, (pk2, 'epsT'), ('rs',), bias=epsT[:], scale=1.0 / 64)
                        rec(rs[:, 0:n], rs[:, 0:n], ('rs',), ('rs',))
                        if not (rope and c0 >= C_CTX):
                            stt(dst[:, c0:c0 + n], raw[:, 0:n], gcol_ap, rs[:, 0:n], ALU.mult, ALU.mult, ('raw', 'rs', gkey), (dkey,))
                        else:
                            l0 = c0 - C_CTX
                            stt(qn[:, 0:n], raw[:, 0:n], gcol_ap, rs[:, 0:n], ALU.mult, ALU.mult, ('raw', 'rs', gkey), ('qn',))
                            p3, pk3 = PS()
                            mm(p3[:, 0:n], rotT[:], qn[:, 0:n], True, True, ('rotT', 'qn'), (pk3,))
                            tt('dve', t1[:, 0:n], qn[:, 0:n], cosT[:, l0:l0 + n], ALU.mult, ('qn', 'cosT'), ('t1',))
                            tt('dve', raw[:, 0:n], p3[:, 0:n], sinT[:, l0:l0 + n], ALU.mult, (pk3, 'sinT'), ('raw',))
                            tt('dve', dst[:, c0:c0 + n], t1[:, 0:n], raw[:, 0:n], ALU.add, ('t1', 'raw'), (dkey,))

                def attention(kind, hg):
                    with ExitStack() as ph:
                        tmp = (sb('raw', [128, 512], F32, ph), sb('sq', [128, 512], BF, ph), sb('rs', [128, 512], F32, ph),
                               sb('qn', [128, 512], BF, ph), sb('t1', [128, 512], F32, ph))
                        gq = sb('gq', [128, 1], F32, ph); gk = sb('gk', [128, 1], F32, ph)
                        S.dma('sp', gq[:], I['gwq' if kind == 'w' else 'gnq'][li], (), ('gq',))
                        S.dma('sp', gk[:], I['gwk' if kind == 'w' else 'gnk'][li], (), ('gk',))
                        ts('dve', gq[:], gq[:], 0.125, None, ALU.mult, None, ('gq',), ('gq',))
                        wq_ = sb('wq_', [128, 8, 256], BF, ph)
                        qT = sb('qT', [128, 2, T], BF, ph)
                        nkt = 1 if kind == 'w' else 2
                        nv = 1 if kind == 'w' else 4
                        wk_ = sb('wk_', [128, 8, 128 * nkt], BF, ph); kT = sb('kT', [128, nkt, T], BF, ph)
                        wv_ = sb('wv_', [128, 8, 64 * nv], BF, ph); vaug = sb('vaug', [128, NT, nv, 65], BF, ph)
                        if kind == 'w':
                            load_w(wq_, 'wq_', win, 512 + 256 * hg, 256)
                            load_w(wk_, 'wk_', win, 1024 + 64 * hg, 64, 0); load_w(wk_, 'wk_', win, 1024 + 64 * hg, 64, 64)
                            load_w(wv_, 'wv_', win, 1152 + 64 * hg, 64)
                        else:
                            load_w(wq_, 'wq_', win, 1280 + 256 * hg, 256)
                            load_w(wk_, 'wk_', win, 1792 + 256 * hg, 256)
                            load_w(wv_, 'wv_', win, 2304 + 256 * hg, 256)
                        for m in range(2):
                            qk_prep(qT[:, m, :], 'qT', wq_, 'wq_', m * 128, gq[:, 0:1], 'gq', kind == 'w', tmp)
                        for m in range(nkt):
                            qk_prep(kT[:, m, :], 'kT', wk_, 'wk_', m * 128, gk[:, 0:1], 'gk', kind == 'w', tmp)
                        kTm = sb('kTm', [128, nkt, 2, T], BF, ph)
                        for m in range(nkt):
                            for par in range(2):
                                ts('dve', kTm[:, m, par, :], kT[:, m, :], hm[:, par:par + 1], None, ALU.mult, None, ('kT', 'hm'), ('kTm',))
                        ms('pool', vaug[:, :, :, 64:65], 1.0, ('vaug',))
                        for t_ in range(NT):
                            p_, pk = PS()
                            for kc in range(8):
                                mm(p_[:, 0:64 * nv], hT[:, kc, t_ * 128:(t_ + 1) * 128], wv_[:, kc, :], kc == 0, kc == 7, ('hT', 'wv_'), (pk,))
                            cp('act', vaug[:, t_, :, 0:64], p_[:, 0:64 * nv].rearrange("p (v d) -> p v d", d=64), (pk,), ('vaug',))
                        esink = sb('esink', [128, 8], F32, ph)
                        if kind == 'w':
                            S.dma('sp', esink[:], I['sink'][li], (), ('esink',))
                            act(esink[:], esink[:], AF.Exp, ('esink',), ('esink',))
                        else:
                            nab = sb('nab', [128, 7, 4, 128], BF, ph)
                            for di in range(7):
                                S.dma('pool', nab[:, di, :, :], I['nabias'][li, di, :, 4 * hg:4 * hg + 4, :], (), ('nab',))
                        import os
                        adbg = int(os.environ.get('ATTDBG', '0'))
                        if adbg == 1:
                            S.barrier(); return
                        NSLOT = 7
                        PTs = sb('PTs', [128, NSLOT, 4, 128], BF, ph)
                        ytok = sb('ytok', [128, 4, 64], BF, ph); tot = sb('tot', [128, 4], F32, ph)
                        S.flush()
                        ybase = (4 if kind == 'w' else 8) + 2 * hg
                        for tq in range(NT):
                            keys = []
                            if tq < 2: keys = [(0, None, None), (1, None, None)]
                            else:
                                i = tq - 2
                                keys = [(0, None, None), (1, None, None)]
                                if kind == 'w':
                                    for j in (i - 1, i, i + 1):
                                        if 0 <= j < 16:
                                            keys.append((2 + j, None if j == i else (wmask[:, 0, :] if j < i else wmask[:, 1, :]), None))
                                else:
                                    for j in na_tiles(i):
                                        keys.append((2 + j, namask[:, nidx[(i, j)], :], nab[:, j - i + 3, :, :]))
                            assert len(keys) <= NSLOT
                            for sl, (tk, mask, bias) in enumerate(keys):
                                p_, pk = PS()
                                if bias is not None:
                                    mm(p_[:, 0:512], ident_b[:], bias.rearrange("p h q -> p (h q)"), True, False, ('ident_b', 'nab'), (pk,))
                                for h in range(4):
                                    if adbg == 4 and h % 2 == 1: continue
                                    half = (h % 2) * 64
                                    kt = kTm[:, 0 if kind == 'w' else h // 2, h % 2, tk * 128:(tk + 1) * 128]
                                    mm(p_[:, h * 128:(h + 1) * 128], kt, qT[:, h // 2, tq * 128:(tq + 1) * 128],
                                       bias is None, h == 3 or bias is None, ('kTm', 'qT'), (pk,))
                                act(PTs[:, sl, :, :].rearrange("p h q -> p (h q)"), p_[:, 0:512], AF.Exp, (pk,), ('PT%d' % sl,))
                                if mask is not None and adbg not in (3, 4):
                                    tt('dve', PTs[:, sl, :, :], PTs[:, sl, :, :], bmid(mask, 4), ALU.mult, ('PT%d' % sl, 'wmask', 'namask'), ('PT%d' % sl,))
                            if adbg in (2, 3, 4):
                                continue
                            po, pok = PS()
                            for h in range(4):
                                for sl, (tk, mask, bias) in enumerate(keys):
                                    mm(po[:, h * 65:(h + 1) * 65], PTs[:, sl, h, :], vaug[:, tk, 0 if kind == 'w' else h, :],
                                       sl == 0, sl == len(keys) - 1, ('PT%d' % sl, 'vaug'), (pok,))
                            pov = po[:, 0:260].rearrange("p (h e) -> p h e", e=65)
                            if kind == 'w':
                                tt('dve', tot[:], pov[:, :, 64], esink[:, 4 * hg:4 * hg + 4], ALU.add, (pok, 'esink'), ('tot',))
                            else:
                                cp('dve', tot[:], pov[:, :, 64], (pok,), ('tot',))
                            rec(tot[:], tot[:], ('tot',), ('tot',))
                            tt('dve', ytok[:], pov[:, :, 0:64], bl(tot[:], 64), ALU.mult, (pok, 'tot'), ('ytok',))
                            pt_, ptk = PT_()
                            for m in range(2):
                                tr(pt_[:, m * 128:(m + 1) * 128], ytok[:, 2 * m:2 * m + 2, :].rearrange("p h d -> p (h d)"), ident_b[:], ('ytok', 'ident_b'), (ptk,))
                            cp('act', yT[:, ybase:ybase + 2, tq * 128:(tq + 1) * 128], pt_[:, 0:256].rearrange("p (m t) -> p m t", t=128), (ptk,), ('yT%d' % ybase, 'yT%d' % (ybase + 1)))
                            if tq % 6 == 5: S.flush()
                        S.barrier()

                for kind in ('w', 'n'):
                    for hg in range(2):
                        attention(kind, hg)
                if dbg and 'yT' in dbg_out and li == 0:
                    with ExitStack() as ph:
                        yf = sb('yf', [128, 12, T], F32, ph) if False else None

                if stop_after == 'attn': break
                with ExitStack() as ph:
                    modb = sb('modb', [128, 2, D], F32, ph)
                    mod_bcast(2, modb)
                    wbr = sb('wbr', [128, 12, D], BF, ph)
                    S.dma('pool', wbr[:], I['w_branch'][li].rearrange("i (kc p) d -> p (i kc) d", p=128), (), ('wbr',))
                    wo = sb('wo', [128, 8, D], BF, ph)
                    S.dma('pool', wo[:], I['w_out'][li].rearrange("(kc p) d -> p kc d", p=128), (), ('wo',))
                    wgt = [sb('wgt%d' % i, [128, 8, 384], BF, ph) for i in range(2)]
                    sT = sb('sT', [128, 8, 512], BF, ph)
                    sg = [sb('sg%d' % i, [128, 512], F32, ph) for i in range(3)]
                    xr = [sb('xr%d' % i, [128, D], F32, ph) for i in range(2)]
                    tmpm = sb('tmpm', [128, 512], F32, ph)
                    it = 0
                    for (c0, n) in CH:
                        for dt_ in range(8):
                            w = wgt[it % 2]; wk = 'wgt%d' % (it % 2); it += 1
                            for i in range(3):
                                load_w(w, wk + '_%d' % i, win, 2816 + i * 1024 + dt_ * 128, 128, i * 128)
                            for i in range(3):
                                pg, pgk = PS()
                                for kc in range(8):
                                    mm(pg[:, 0:n], w[:, kc, i * 128:(i + 1) * 128], hT[:, kc, c0:c0 + n], kc == 0, kc == 7, (wk + '_%d' % i, 'hT'), (pgk,))
                                act(sg[i][:, 0:n], pg[:, 0:n], AF.Sigmoid, (pgk,), ('sg%d' % i,))
                                pp, ppk = PS()
                                for kc in range(4):
                                    mm(pp[:, 0:n], wbr[:, i * 4 + kc, dt_ * 128:(dt_ + 1) * 128], yT[:, i * 4 + kc, c0:c0 + n], kc == 0, kc == 3,
                                       ('wbr',) + tuple('yT%d' % q for q in range(12)), (ppk,))
                                tt('dve', sg[i][:, 0:n], sg[i][:, 0:n], pp[:, 0:n], ALU.mult, (ppk, 'sg%d' % i), ('sg%d' % i,))
                            tt('dve', sg[0][:, 0:n], sg[0][:, 0:n], sg[1][:, 0:n], ALU.add, ('sg0', 'sg1'), ('sg0',))
                            tt('dve', sT[:, dt_, 0:n], sg[0][:, 0:n], sg[2][:, 0:n], ALU.add, ('sg0', 'sg2'), ('sT',))
                        for tl_ in range(n // 128):
                            tt_ = c0 // 128 + tl_; b_ = tt_ % 2; v = 1 if tt_ < 2 else 0
                            S.dma('sp', xr[b_][:], xd[tt_ * 128:(tt_ + 1) * 128, :], ('xd%d' % tt_,), ('xr%d' % b_,))
                            for hf in range(2):
                                p_, pk = PS()
                                for kc in range(8):
                                    mm(p_[:, 0:512], sT[:, kc, tl_ * 128:(tl_ + 1) * 128], wo[:, kc, hf * 512:(hf + 1) * 512], kc == 0, kc == 7, ('sT', 'wo'), (pk,))
                                tt('dve', tmpm[:], p_[:, 0:512], modb[:, v, hf * 512:(hf + 1) * 512], ALU.mult, (pk, 'modb'), ('tmpm',))
                                tt('dve', xr[b_][:, hf * 512:(hf + 1) * 512], xr[b_][:, hf * 512:(hf + 1) * 512], tmpm[:], ALU.add, ('tmpm', 'xr%d' % b_), ('xr%d' % b_,))
                            S.dma('sp', xd[tt_ * 128:(tt_ + 1) * 128, :], xr[b_][:], ('xr%d' % b_,), ('xd%d' % tt_,))
                    S.barrier()

            if stop_after == 'merge': break
            mod_scale_shift('gffn', 4, 3)
            norm_stage()
            with ExitStack() as ph:
                macc = sb('macc', [128, NT, D], F32, ph)
                G = sb('G', [128, NT, NE], F32, ph)
                with ExitStack() as ph1:
                    rw = sb('rw', [128, 8, NE], BF, ph1); rb = sb('rb', [128, NE], F32, ph1)
                    S.dma('pool', rw[:], I['router_w'][li].rearrange("(kc p) e -> p kc e", p=128), (), ('rw',))
                    S.dma('sp', rb[:], I['rbias'][li], (), ('rb',))
                    lg = sb('lg', [128, NE], F32, ph1); m8 = sb('m8', [128, 8], F32, ph1); mk = sb('mk', [128, NE], F32, ph1)
                    nmx = sb('nmx', [128, 1], F32, ph1); sm = sb('sm', [128, 1], F32, ph1)
                    ms('pool', macc[:], 0.0, tuple('macc%d' % t for t in range(NT)))
                    for t_ in range(NT):
                        p_, pk = PS()
                        for kc in range(8):
                            mm(p_[:, 0:NE], hT[:, kc, t_ * 128:(t_ + 1) * 128], rw[:, kc, :], kc == 0, kc == 7, ('hT', 'rw'), (pk,))
                        tt('dve', lg[:], p_[:, 0:NE], rb[:], ALU.add, (pk, 'rb'), ('lg',))
                        S.add('dve', lambda e: e.max(out=m8[:], in_=lg[:]), ('lg',), ('m8',))
                        ts('dve', mk[:], lg[:], m8[:, 3:4], None, ALU.is_ge, None, ('lg', 'm8'), ('mk',))
                        ts('dve', nmx[:], m8[:, 0:1], -1.0, None, ALU.mult, None, ('m8',), ('nmx',))
                        act(lg[:], lg[:], AF.Exp, ('lg', 'nmx'), ('lg',), bias=nmx[:])
                        tt('dve', lg[:], lg[:], mk[:], ALU.mult, ('lg', 'mk'), ('lg',))
                        S.add('dve', lambda e: e.reduce_sum(out=sm[:], in_=lg[:], axis=mybir.AxisListType.X), ('lg',), ('sm',))
                        rec(sm[:], sm[:], ('sm',), ('sm',))
                        ts('dve', G[:, t_, :], lg[:], sm[:, 0:1], None, ALU.mult, None, ('lg', 'sm'), ('G',))
                    S.barrier()
                with ExitStack() as ph2:
                    wgu = [sb('wgu%d' % i, [128, 8, 2 * D], BF, ph2) for i in range(2)]
                    wdn = sb('wdn', [128, 8, D], BF, ph2)
                    bgu = sb('bgu', [128, NE, 16], F32, ph2)
                    S.dma('sp', bgu[:], I['bguT'][li], (), ('bgu',))
                    ts('dve', bgu[:, :, 8:16], bgu[:, :, 8:16], 1.0, None, ALU.add, None, ('bgu',), ('bgu',))
                    actT = [sb('actT%d' % i, [128, 8, 512], BF, ph2) for i in range(2)]
                    g1 = [sb('g1%d' % i, [128, 512], F32, ph2) for i in range(2)]; sgm = sb('sgm', [128, 512], BF, ph2); u1 = sb('u1', [128, 512], F32, ph2)
                    CHM = [(256 + 512 * j, 512) for j in range(4)] + ([(0, 256)] if li < DEPTH - 1 or nlayers < DEPTH else [])
                    NCM = len(CHM)

                    def load_gu(e_):
                        wgv = I['w_gate_up'][li, e_].rearrange("(kc p) n -> p kc n", p=128)
                        for kc in range(8):
                            S.dma('pool', wgu[e_ % 2][:, kc, :], wgv[:, kc, :], (), ('wgu%d_%d' % (e_ % 2, kc),))

                    def load_dn(e_):
                        S.dma('pool', wdn[:], I['w_down'][li, e_].rearrange("(kc p) n -> p kc n", p=128), (), ('wdn',))

                    def GU(e_, ci):
                        c0, n = CHM[ci]; a_ = actT[ci % 2]; ak = 'actT%d' % (ci % 2)
                        w = wgu[e_ % 2]; wk = 'wgu%d' % (e_ % 2)
                        for m in range(8):
                            pg, pgk = PS()
                            for kc in range(8):
                                mm(pg[:, 0:n], w[:, kc, m * 128:(m + 1) * 128], hT[:, kc, c0:c0 + n], kc == 0, kc == 7, (wk + '_%d' % kc, 'hT'), (pgk,))
                            pu, puk = PS()
                            for kc in range(8):
                                mm(pu[:, 0:n], w[:, kc, D + m * 128:D + (m + 1) * 128], hT[:, kc, c0:c0 + n], kc == 0, kc == 7, (wk + '_%d' % kc, 'hT'), (puk,))
                            b_ = m % 2
                            ts('dve', g1[b_][:, 0:n], pg[:, 0:n], bgu[:, e_, m:m + 1], 7.0, ALU.add, ALU.min, (pgk, 'bgu'), ('g1%d' % b_,))
                            act(sgm[:, 0:n], g1[b_][:, 0:n], AF.Sigmoid, ('g1%d' % b_,), ('sgm',), scale=1.702)
                            if m > 0:
                                q_ = (m - 1) % 2
                                stt(a_[:, m - 1, 0:n], u1[:, 0:n], -6.0, g1[q_][:, 0:n], ALU.max, ALU.mult, ('u1', 'g1%d' % q_), (ak,))
                            ts('dve', u1[:, 0:n], pu[:, 0:n], bgu[:, e_, 8 + m:9 + m], 8.0, ALU.add, ALU.min, (puk, 'bgu'), ('u1',))
                            tt('pool', g1[b_][:, 0:n], g1[b_][:, 0:n], sgm[:, 0:n], ALU.mult, ('g1%d' % b_, 'sgm'), ('g1%d' % b_,))
                        stt(a_[:, 7, 0:n], u1[:, 0:n], -6.0, g1[1][:, 0:n], ALU.max, ALU.mult, ('u1', 'g11'), (ak,))

                    def DN(e_, ci):
                        c0, n = CHM[ci]; a_ = actT[ci % 2]; ak = 'actT%d' % (ci % 2)
                        for tl_ in range(n // 128):
                            tt_ = c0 // 128 + tl_
                            for hf in range(2):
                                p_, pk = PS()
                                for kc in range(8):
                                    mm(p_[:, 0:512], a_[:, kc, tl_ * 128:(tl_ + 1) * 128], wdn[:, kc, hf * 512:(hf + 1) * 512], kc == 0, kc == 7, (ak, 'wdn'), (pk,))
                                sl_ = macc[:, tt_, hf * 512:(hf + 1) * 512]
                                stt(sl_, p_[:, 0:512], G[:, tt_, e_:e_ + 1], sl_, ALU.mult, ALU.add, (pk, 'G', 'macc%d' % tt_), ('macc%d' % tt_,))

                    load_gu(0); load_dn(0)
                    for e_ in range(NE):
                        if e_ + 1 < NE: load_gu(e_ + 1)
                        for ci in range(NCM):
                            GU(e_, ci)
                            if ci > 0: DN(e_, ci - 1)
                        DN(e_, NCM - 1)
                        if e_ + 1 < NE: load_dn(e_ + 1)
                        S.flush()
                    S.barrier()
                with ExitStack() as ph3:
                    modb = sb('modb5', [128, 2, D], F32, ph3)
                    mod_bcast(5, modb)
                    xr = [sb('xq%d' % i, [128, D], F32, ph3) for i in range(2)]
                    bdn = sb('bdn', [32, D], F32, ph3); GT = sb('GT', [32, 128], F32, ph3)
                    S.dma('sp', bdn[:], I['b_down'][li], (), ('bdn',))
                    for t_ in range(NT):
                        b_ = t_ % 2; v = 1 if t_ < 2 else 0
                        pg_, pgk_ = PS()
                        tr(pg_[0:32, 0:128], G[:, t_, :], ident_f[:], ('G', 'ident_f'), (pgk_,))
                        cp('act', GT[:], pg_[0:32, 0:128], (pgk_,), ('GT',))
                        for hf in range(2):
                            pb_, pbk_ = PS()
                            mm(pb_[:, 0:512], GT[:], bdn[:, hf * 512:(hf + 1) * 512], True, True, ('GT', 'bdn'), (pbk_,))
                            tt('dve', macc[:, t_, hf * 512:(hf + 1) * 512], macc[:, t_, hf * 512:(hf + 1) * 512], pb_[:, 0:512], ALU.add, (pbk_, 'macc%d' % t_), ('macc%d' % t_,))
                        S.dma('sp', xr[b_][:], xd[t_ * 128:(t_ + 1) * 128, :], ('xd%d' % t_,), ('xq%d' % b_,))
                        tt('dve', macc[:, t_, :], macc[:, t_, :], modb[:, v, :], ALU.mult, ('macc%d' % t_, 'modb'), ('macc%d' % t_,))
                        tt('pool', xr[b_][:], xr[b_][:], macc[:, t_, :], ALU.add, ('xq%d' % b_, 'macc%d' % t_), ('xq%d' % b_,))
                        if li == nlayers - 1:
                            if t_ >= 2:
                                S.dma('sp', yout[(t_ - 2) * 128:(t_ - 1) * 128, :], xr[b_][:], ('xq%d' % b_,), ('yout%d' % t_,))
                        else:
                            S.dma('sp', xd[t_ * 128:(t_ + 1) * 128, :], xr[b_][:], ('xq%d' % b_,), ('xd%d' % t_,))
                    S.barrier()
        S.barrier()
    return nc, consts, list(I.keys())


def make_in_maps(inputs, cores, nc_consts, names=None):
    maps = []
    big = {k_: np.ascontiguousarray(inputs[k_], dtype=np.float32) for k_ in BIG if names is None or k_ in names}
    for b in cores:
        m = {'x': np.ascontiguousarray(inputs['x'][b]), 'ctx': np.ascontiguousarray(inputs['ctx'][b])}
        m.update(nc_consts)
        m.update(host_layout(inputs, b))
        m.update(big)
        if names is not None: m = {k_: v for k_, v in m.items() if k_ in names}
        maps.append(m)
    return maps


def kernel(**inputs):
    inputs = {k_: np.asarray(v) for k_, v in inputs.items()}
    nc, consts, names = build(DEPTH)
    maps = make_in_maps(inputs, list(range(8)), consts, names)
    res = run_bass_kernel_spmd(nc, maps, core_ids=list(range(8)))
    return np.stack([r['y'] for r in res.results], 0).astype(np.float32)
```

```python
import numpy as np
from contextlib import ExitStack
import concourse.bass as bass
import concourse.mybir as mybir
from concourse.bass_utils import run_bass_kernel_spmd

F32 = mybir.dt.float32
BF = mybir.dt.bfloat16
ALU = mybir.AluOpType
AF = mybir.ActivationFunctionType

D = 1024; L_LAT = 2048; C_CTX = 256; T = 2304; NT = 18; DEPTH = 4
NE = 32; INW = 5888
CH = [(0, 256)] + [(256 + 512 * j, 512) for j in range(4)]
EPS = 1e-6
ND = 12
SAME_ENGINE_SYNC = True


def bl(ap, n):
    return bass.AP(ap.tensor, ap.offset, list(ap.ap) + [(0, n)])


def bmid(ap, n):
    a = list(ap.ap)
    return bass.AP(ap.tensor, ap.offset, [a[0], (0, n)] + a[1:])


class Sched:
    def __init__(s, nc, block, stack):
        s.nc = nc; s.block = block
        s.engs = {'pe': nc.tensor, 'act': nc.scalar, 'dve': nc.vector, 'pool': nc.gpsimd, 'sp': nc.sync}
        s.tl = {e: stack.enter_context(nc.semaphore('tl_' + e)) for e in ['pe', 'act', 'dve', 'pool']}
        s.cnt = {e: 0 for e in s.tl}
        s.dsem = {q: [stack.enter_context(nc.semaphore('d_%s%d' % (q, i))) for i in range(ND)] for q in ['sp', 'pool']}
        s.dval = {q: [0] * ND for q in s.dsem}; s.dnext = {q: 0 for q in s.dsem}
        s.waited = {e: {} for e in s.engs}
        s.lw = {}; s.rd = {}
        s.pend = {e: [] for e in s.engs}
        s.n = 0

    def _waits(s, eng, R, W, fast=False):
        deps = {}
        def need(tok):
            if tok is None: return
            name, sem, val, src = tok[:4]
            if src == eng and (eng in ('pe', 'sp') or not SAME_ENGINE_SYNC): return
            if src == eng and fast and len(tok) > 4 and tok[4]: return
            if deps.get(name, (None, 0))[1] < val: deps[name] = (sem, val)
        for k in R: need(s.lw.get(k))
        for k in W:
            need(s.lw.get(k))
            for t in s.rd.get(k, ()): need(t)
        out = []
        for name, (sem, val) in deps.items():
            if s.waited[eng].get(name, 0) < val:
                s.waited[eng][name] = val; out.append((sem, val))
        return out

    def _mark(s, tok, R, W):
        for k in R: s.rd.setdefault(k, []).append(tok)
        for k in W: s.lw[k] = tok; s.rd[k] = []

    def add(s, eng, fn, R=(), W=(), fast=False):
        waits = s._waits(eng, R, W, fast)
        s.cnt[eng] += 1
        tok = ('tl_' + eng, s.tl[eng], s.cnt[eng], eng, fast)
        s.pend[eng].append((waits, fn, (s.tl[eng], 1)))
        s._mark(tok, R, W); s.n += 1

    def dma(s, q, out, in_, R=(), W=(), **kw):
        waits = s._waits(q, R, W)
        i = s.dnext[q]; s.dnext[q] = (i + 1) % ND
        sem = s.dsem[q][i]; name = 'd_%s%d' % (q, i)
        if s.dval[q][i] > s.waited[q].get(name, 0):
            s.waited[q][name] = s.dval[q][i]; waits.append((sem, s.dval[q][i]))
        s.dval[q][i] += 16
        tok = (name, sem, s.dval[q][i], 'dma')
        s.pend[q].append((waits, lambda e: e.dma_start(out=out, in_=in_, **kw), (sem, 16)))
        s._mark(tok, R, W); s.n += 1

    def barrier(s):
        for eng in s.engs:
            waits = []
            for e2 in s.tl:
                name = 'tl_' + e2
                if s.cnt[e2] > s.waited[eng].get(name, 0) and not (e2 == eng == 'pe'):
                    s.waited[eng][name] = s.cnt[e2]; waits.append((s.tl[e2], s.cnt[e2]))
            for q in s.dsem:
                for i in range(ND):
                    name = 'd_%s%d' % (q, i)
                    if s.dval[q][i] > s.waited[eng].get(name, 0):
                        s.waited[eng][name] = s.dval[q][i]; waits.append((s.dsem[q][i], s.dval[q][i]))
            if waits: s.pend[eng].append((waits, None, None))
        s.flush()

    def wait_all(s, eng, keys):
        waits = s._waits(eng, keys, ())
        s.pend[eng].append((waits, None, None))

    def flush(s):
        for ename, lst in s.pend.items():
            if not lst: continue
            def body(e, lst=lst):
                for waits, fn, inc in lst:
                    for sem, val in waits: e.wait_ge(sem, val)
                    if fn is not None:
                        ins = fn(e)
                        ins.then_inc(inc[0], inc[1])
            getattr(s.block, {'pe': 'tensor', 'act': 'scalar', 'dve': 'vector', 'pool': 'gpsimd', 'sp': 'sync'}[ename])(body)
            s.pend[ename] = []


def na_start(r): return min(max(r - 4, 0), 24)

def na_tiles(i):
    rows = set()
    for r in (2 * i, 2 * i + 1):
        st = na_start(r); rows.update(range(st, st + 8))
    return sorted(set(r // 2 for r in rows))

def na_mask(i, j):
    m = np.zeros((128, 128), np.float32)
    k = np.arange(128); rk = k // 64; ck = k % 64
    for q in range(128):
        rq = q // 64; cq = q % 64
        st = na_start(2 * i + rq)
        row_ok = (2 * j + rk >= st) & (2 * j + rk <= st + 7)
        cs = min(max(cq - 8, 0), 48)
        col_ok = (ck >= cs) & (ck < cs + 16)
        m[:, q] = (row_ok & col_ok)
    return m

_NA_MASKS = None
def na_mask_table():
    global _NA_MASKS
    if _NA_MASKS is None:
        uniq = []; idx = {}
        for i in range(16):
            for j in na_tiles(i):
                m = na_mask(i, j)
                for u, mm in enumerate(uniq):
                    if np.array_equal(mm, m): idx[(i, j)] = u; break
                else:
                    uniq.append(m); idx[(i, j)] = len(uniq) - 1
        _NA_MASKS = (np.stack(uniq), idx)
    return _NA_MASKS


def host_consts():
    bf = mybir.dt.np(BF)
    c = {}
    c['ident_f'] = np.eye(128, dtype=np.float32)
    c['ident_b'] = np.eye(128, dtype=np.float32).astype(bf)
    bo = np.zeros((128, 128), np.float32); bo[:64, :64] = 1; bo[64:, 64:] = 1
    c['blockones'] = bo.astype(bf)
    c['ones_f'] = np.ones((128, 128), np.float32)
    R = np.zeros((64, 64), np.float32)
    for base in (0, 32):
        for i in range(16):
            R[base + i, base + 16 + i] = -1.0
            R[base + 16 + i, base + i] = 1.0
    R2 = np.zeros((128, 128), np.float32); R2[:64, :64] = R; R2[64:, 64:] = R
    c['rotT'] = np.ascontiguousarray(R2.T).astype(bf)
    pos = np.arange(L_LAT); row = pos // 64; col = pos % 64
    inv = (10000.0 ** (-np.arange(16, dtype=np.float32) / 16)).astype(np.float32)
    ang = np.zeros((64, L_LAT), np.float32)
    for dd in range(64):
        p = row if dd < 32 else col
        ang[dd] = p.astype(np.float32) * inv[dd % 16]
    c['cosT'] = np.concatenate([np.cos(ang), np.cos(ang)], 0).astype(bf)
    c['sinT'] = np.concatenate([np.sin(ang), np.sin(ang)], 0).astype(bf)
    b = np.arange(128)[:, None]; a = np.arange(128)[None, :]
    c['wmask'] = np.stack([(a <= b), (b <= a)]).astype(np.float32).astype(bf)
    c['namask'] = na_mask_table()[0].astype(bf)
    mz = np.zeros((4, 128, 128), np.float32); my = np.zeros((4, 128, 128), np.float32)
    for gl in range(4):
        for g2 in range(2):
            mz[gl, 64 * g2:64 * g2 + 64, 32 * gl + 16 * g2: 32 * gl + 16 * g2 + 16] = 1
            my[gl, 32 * gl + 16 * g2: 32 * gl + 16 * g2 + 16, 64 * g2:64 * g2 + 64] = 1
    c['maskZ'] = mz; c['maskY'] = my
    return c


def host_layout(inp, b):
    o = {}
    cb = inp['c'][b].reshape(8, 128).T; cc = inp['c_ctx'].reshape(8, 128).T
    o['cT'] = np.ascontiguousarray(np.stack([cb, cc], -1))
    o['bmodT'] = np.ascontiguousarray(inp['b_mod'].reshape(DEPTH, 48, 128).transpose(0, 2, 1))
    o['gmix'] = np.ascontiguousarray(inp['norm_mix'].reshape(DEPTH, 8, 128).transpose(0, 2, 1))
    o['gffn'] = np.ascontiguousarray(inp['norm_ffn'].reshape(DEPTH, 8, 128).transpose(0, 2, 1))
    def p2(x):
        return np.ascontiguousarray(x.reshape(DEPTH, 2, 16, 2, 64).transpose(0, 3, 4, 1, 2).reshape(DEPTH, 128, 32))
    o['lamre'] = p2(inp['ssm_lam_re']); o['lamim'] = p2(inp['ssm_lam_im'])
    o['logdt'] = p2(np.broadcast_to(inp['ssm_log_dt'][..., None], (DEPTH, 2, 32, 64)))
    def pb(x):
        return np.ascontiguousarray(x.reshape(DEPTH, 2, 16, 2, 64, 16).transpose(0, 3, 4, 1, 2, 5).reshape(DEPTH, 128, 32, 16))
    o['bre'] = pb(inp['ssm_b_re']); o['bim'] = pb(inp['ssm_b_im'])
    def pc(x):
        return np.ascontiguousarray(x.reshape(DEPTH, 2, 4, 8, 16, 64).transpose(0, 3, 4, 1, 2, 5).reshape(DEPTH, 128, 8, 64))
    o['cre'] = pc(inp['ssm_c_re']); o['cim'] = pc(inp['ssm_c_im'])
    o['dskip'] = np.ascontiguousarray(inp['ssm_d'].reshape(DEPTH, 4, 128).transpose(0, 2, 1))
    def hd(x): return np.ascontiguousarray(np.tile(x, (1, 2))[:, :, None])
    o['gwq'] = hd(inp['win_q_norm']); o['gwk'] = hd(inp['win_k_norm'])
    o['gnq'] = hd(inp['na_q_norm']); o['gnk'] = hd(inp['na_k_norm'])
    o['sink'] = np.ascontiguousarray(np.broadcast_to(inp['win_sink'][:, None, :], (DEPTH, 128, 8)))
    k = np.arange(128); rk = k // 64; ck = k % 64
    rpb = inp['na_rpb']
    dc = np.clip(ck[:, None] - ck[None, :], -15, 15) + 15
    B = np.zeros((DEPTH, 7, 128, 8, 128), np.float32)
    for di, dl in enumerate(range(-3, 4)):
        dr = np.clip(2 * dl + rk[:, None] - rk[None, :] + 7, 0, 14)
        dcf = dc[ck[:, None], ck[None, :]]
        B[:, di] = rpb[:, :, dr, dcf].transpose(0, 2, 1, 3)
    o['nabias'] = B
    o['rbias'] = np.ascontiguousarray(np.broadcast_to(inp['router_b'][:, None, :], (DEPTH, 128, NE)))
    o['bguT'] = np.ascontiguousarray(inp['b_gate_up'].reshape(DEPTH, NE, 16, 128).transpose(0, 3, 1, 2))
    return o


SMALL_SHAPES = {
    'cT': [128, 8, 2], 'bmodT': [DEPTH, 128, 48], 'gmix': [DEPTH, 128, 8], 'gffn': [DEPTH, 128, 8],
    'lamre': [DEPTH, 128, 32], 'lamim': [DEPTH, 128, 32], 'logdt': [DEPTH, 128, 32],
    'bre': [DEPTH, 128, 32, 16], 'bim': [DEPTH, 128, 32, 16], 'cre': [DEPTH, 128, 8, 64], 'cim': [DEPTH, 128, 8, 64],
    'dskip': [DEPTH, 128, 4], 'gwq': [DEPTH, 128, 1], 'gwk': [DEPTH, 128, 1], 'gnq': [DEPTH, 128, 1], 'gnk': [DEPTH, 128, 1],
    'sink': [DEPTH, 128, 8], 'nabias': [DEPTH, 7, 128, 8, 128], 'rbias': [DEPTH, 128, NE], 'bguT': [DEPTH, 128, NE, 16],
}
BIG = {'w_mod': [DEPTH, D, 6 * D], 'w_in': [DEPTH, D, INW], 'ssm_w_glu': [DEPTH, 512, 512],
       'w_branch': [DEPTH, 3, 512, D], 'w_out': [DEPTH, D, D], 'router_w': [DEPTH, D, NE],
       'w_gate_up': [DEPTH, NE, D, 2 * D], 'w_down': [DEPTH, NE, D, D], 'b_down': [DEPTH, NE, D]}


def build(nlayers=DEPTH, dbg=None, stop_after=None):
    nc = bass.Bass("TRN2", target_bir_lowering=False, dynamic_dma_scratch_size=4096)
    consts = host_consts()
    shapes = {'x': ([L_LAT, D], F32), 'ctx': ([C_CTX, D], F32)}
    for k_, v in consts.items(): shapes[k_] = (list(v.shape), BF if v.dtype != np.float32 else F32)
    for k_, v in SMALL_SHAPES.items(): shapes[k_] = (v, F32)
    for k_, v in BIG.items(): shapes[k_] = ([nlayers] + v[1:], F32)
    class LazyI(dict):
        def __missing__(self, name):
            shp, dt = shapes[name]
            self[name] = nc.dram_tensor(name, list(shp), dt, kind="ExternalInput").ap()
            return self[name]
    I = LazyI()
    yout = nc.dram_tensor('y', [L_LAT, D], F32, kind="ExternalOutput").ap()
    xd = nc.dram_tensor('xd', [T, D], F32, kind="Internal").ap()
    dbg_out = {}
    if dbg:
        for k_, shp in dbg.items():
            dbg_out[k_] = nc.dram_tensor('dbg_' + k_, list(shp), F32, kind="ExternalOutput").ap()

    with ExitStack() as st:
        sbn = [0]
        def sb(name, shape, dt=F32, stack=None):
            sbn[0] += 1
            return (stack or st).enter_context(nc.sbuf_tensor('s%d_%s' % (sbn[0], name), list(shape), dt))
        ident_f = sb('ident_f', [128, 128]); ident_b = sb('ident_b', [128, 128], BF)
        blockones = sb('blockones', [128, 128], BF); ones_f = sb('ones_f', [128, 128])
        nmk, nidx = na_mask_table(); NM = nmk.shape[0]
        epsT = sb('epsT', [128, 1]); halfpi = sb('halfpi', [128, 1]); hm = sb('hm', [128, 2])
        sc = sb('sc', [128, 8, 2])
        modc = sb('modc', [128, 48, 2])
        s1 = sb('s1', [128, 8, 2]); s0 = sb('s0', [128, 8, 2])
        hT = sb('hT', [128, 8, T], BF)
        ps = [st.enter_context(nc.psum_tensor('ps%d' % i, [128, 512], F32)) for i in range(6)]
        pT = [st.enter_context(nc.psum_tensor('pT%d' % i, [128, 1024], BF)) for i in range(2)]
        block = st.enter_context(nc.Block())
        S = Sched(nc, block, st)
        psn = [0]
        def PS():
            i = psn[0] % 6; psn[0] += 1
            return ps[i], 'ps%d' % i
        ptn = [0]
        def PT_():
            i = ptn[0] % 2; ptn[0] += 1
            return pT[i], 'pT%d' % i

        def act(out, in_, func, R, W, **kw): S.add('act', lambda e: e.activation(out=out, in_=in_, func=func, **kw), R, W)
        def ts(eng, out, in0, s1_, s2_, op0, op1, R, W):
            if op1 is None: S.add(eng, lambda e: e.tensor_scalar(out=out, in0=in0, scalar1=s1_, scalar2=None, op0=op0), R, W)
            else: S.add(eng, lambda e: e.tensor_scalar(out=out, in0=in0, scalar1=s1_, scalar2=s2_, op0=op0, op1=op1), R, W)
        def tt(eng, out, in0, in1, op, R, W): S.add(eng, lambda e: e.tensor_tensor(out=out, in0=in0, in1=in1, op=op), R, W)
        def stt(out, in0, scalar, in1, op0, op1, R, W, fast=False):
            S.add('dve', lambda e: e.scalar_tensor_tensor(out=out, in0=in0, scalar=scalar, in1=in1, op0=op0, op1=op1), R, W, fast)
        def mm(out, lhsT, rhs, start, stop, R, W): S.add('pe', lambda e: e.matmul(out, lhsT, rhs, start=start, stop=stop), R, W)
        def tr(out, in_, ident, R, W): S.add('pe', lambda e: e.transpose(out, in_, ident), R, W)
        def cp(eng, out, in_, R, W):
            if eng == 'act': S.add('act', lambda e: e.copy(out=out, in_=in_), R, W)
            else: S.add(eng, lambda e: e.tensor_copy(out=out, in_=in_), R, W)
        def ms(eng, ap, val, W): S.add(eng, lambda e: e.memset(ap, val), (), W)
        def rec(out, in_, R, W): S.add('dve', lambda e: e.reciprocal(out=out, in_=in_), R, W)

        for k_, t_ in [('ident_f', ident_f), ('ident_b', ident_b), ('blockones', blockones), ('ones_f', ones_f)]:
            S.dma('sp', t_[:], I[k_], (), (k_,))
        ms('dve', hm[:], 0.0, ('hm',)); ms('dve', hm[0:64, 0:1], 1.0, ('hm',)); ms('dve', hm[64:128, 1:2], 1.0, ('hm',))
        ms('dve', epsT[:], EPS, ('epsT',)); ms('dve', halfpi[:], float(np.pi / 2), ('halfpi',))
        S.dma('sp', xd[0:C_CTX, :], I['ctx'], (), ('xd0', 'xd1'))
        S.dma('sp', xd[C_CTX:T, :], I['x'], (), tuple('xd%d' % t for t in range(2, NT)))
        S.dma('sp', sc[:], I['cT'], (), ('sc',))
        act(sc[:], sc[:], AF.Silu, ('sc',), ('sc',))
        S.flush()

        for li in range(nlayers):
            with ExitStack() as ph:
                wm = [sb('wm%d' % i, [128, 8, 512], F32, ph) for i in range(2)]
                bmod = sb('bmod', [128, 48], F32, ph)
                S.dma('sp', bmod[:], I['bmodT'][li], (), ('bmod',))
                wv = I['w_mod'][li].rearrange("(kc p) n -> p kc n", p=128)
                for blk in range(12):
                    w = wm[blk % 2]; wk = 'wm%d' % (blk % 2)
                    S.dma('sp', w[:], wv[:, :, blk * 512:(blk + 1) * 512], (), (wk,))
                    p_, pk = PS()
                    for j in range(4):
                        for kc in range(8):
                            mm(p_[:, 2 * j:2 * j + 2], w[:, kc, j * 128:(j + 1) * 128], sc[:, kc, :], kc == 0, kc == 7, (wk, 'sc'), (pk,))
                    tt('dve', modc[:, blk * 4:blk * 4 + 4, :], p_[:, 0:8].rearrange("p (j c) -> p j c", c=2),
                       bl(bmod[:, blk * 4:blk * 4 + 4], 2), ALU.add, (pk, 'bmod'), ('modc',))
                S.barrier()
            gcol = sb('gcol', [128, 8], F32, st) if li == 0 else gcol
            if stop_after == 'adaln': break

            def mod_scale_shift(gname, jsc, jsh):
                S.dma('sp', gcol[:], I[gname][li], (), ('gcol',))
                ts('dve', s1[:], modc[:, 8 * jsc:8 * jsc + 8, :], 1.0, None, ALU.add, None, ('modc',), ('s1',))
                tt('dve', s1[:], s1[:], bl(gcol[:], 2), ALU.mult, ('s1', 'gcol'), ('s1',))
                cp('dve', s0[:], modc[:, 8 * jsh:8 * jsh + 8, :], ('modc',), ('s0',))

            def mod_bcast(jg, modb):
                with ExitStack() as ph:
                    dg = sb('dg', [128, 128], F32, ph)
                    for v in range(2):
                        for kc in range(8):
                            ts('dve', dg[:], ident_f[:], modc[:, 8 * jg + kc, v:v + 1], None, ALU.mult, None, ('ident_f', 'modc'), ('dg',))
                            p_, pk = PS()
                            mm(p_[:, 0:128], ones_f[:], dg[:], True, True, ('ones_f', 'dg'), (pk,))
                            cp('act', modb[:, v, kc * 128:(kc + 1) * 128], p_[:, 0:128], (pk,), ('modb',))
                    S.barrier()

            def norm_stage():
                with ExitStack() as ph:
                    xt = [sb('xt%d' % i, [128, D], F32, ph) for i in range(2)]
                    junk = sb('junk', [128, D], BF, ph); xn = [sb('xn%d' % i, [128, D], BF, ph) for i in range(2)]
                    ss = sb('ss', [128, NT], F32, ph)
                    for tt_ in range(NT):
                        b_ = tt_ % 2; v = 1 if tt_ < 2 else 0
                        S.dma('sp', xt[b_][:], xd[tt_ * 128:(tt_ + 1) * 128, :], ('xd%d' % tt_,), ('xt%d' % b_,))
                        act(junk[:], xt[b_][:], AF.Square, ('xt%d' % b_,), ('junk', 'ss%d' % tt_), accum_out=ss[:, tt_:tt_ + 1])
                        act(ss[:, tt_:tt_ + 1], ss[:, tt_:tt_ + 1], AF.Sqrt, ('ss%d' % tt_, 'epsT'), ('ss%d' % tt_,), bias=epsT[:], scale=1.0 / D)
                        rec(ss[:, tt_:tt_ + 1], ss[:, tt_:tt_ + 1], ('ss%d' % tt_,), ('ss%d' % tt_,))
                        ts('dve', xn[b_][:], xt[b_][:], ss[:, tt_:tt_ + 1], None, ALU.mult, None, ('xt%d' % b_, 'ss%d' % tt_), ('xn%d' % b_,))
                        p_, pk = PT_()
                        for kc in range(8):
                            tr(p_[:, kc * 128:(kc + 1) * 128], xn[b_][:, kc * 128:(kc + 1) * 128], ident_b[:], ('xn%d' % b_, 'ident_b'), (pk,))
                        for kc in range(8):
                            ts('dve', hT[:, kc, tt_ * 128:(tt_ + 1) * 128], p_[:, kc * 128:(kc + 1) * 128], s1[:, kc, v:v + 1], s0[:, kc, v:v + 1],
                               ALU.mult, ALU.add, (pk, 's1', 's0'), ('hT',))
                    S.barrier()

            win = I['w_in'][li].rearrange("(kc p) n -> p kc n", p=128)

            def load_w(wt, wkey, view, c0, n, dst0=0):
                S.dma('pool', wt[:, :, dst0:dst0 + n], view[:, :, c0:c0 + n], (), (wkey,))

            def proj_fm(wt, wkey, wc0, consumer, src=None, nk=8, srckey='hT'):
                src = hT if src is None else src
                for (c0, n) in CH:
                    p_, pk = PS()
                    for kc in range(nk):
                        mm(p_[:, 0:n], wt[:, kc, wc0:wc0 + 128], src[:, kc, c0:c0 + n], kc == 0, kc == nk - 1, (wkey,) + (srckey if isinstance(srckey, tuple) else (srckey,)), (pk,))
                    consumer(p_[:, 0:n], pk, c0, n)

            mod_scale_shift('gmix', 1, 0)
            norm_stage()
            if stop_after == 'norm': break
            with ExitStack() as mx:
                rotT = sb('rotT', [128, 128], BF, mx)
                cosT = sb('cosT', [128, L_LAT], BF, mx); sinT = sb('sinT', [128, L_LAT], BF, mx)
                wmask = sb('wmask', [128, 2, 128], BF, mx); namask = sb('namask', [128, NM, 128], BF, mx)
                maskZ = sb('maskZ', [128, 4, 128], F32, mx); maskY = sb('maskY', [128, 4, 128], F32, mx)
                for k_, t_ in [('rotT', rotT), ('cosT', cosT), ('sinT', sinT)]:
                    S.dma('sp', t_[:], I[k_], (), (k_,))
                S.dma('sp', maskZ[:], I['maskZ'].rearrange("g p c -> p g c"), (), ('maskZ',))
                S.dma('sp', maskY[:], I['maskY'].rearrange("g p c -> p g c"), (), ('maskY',))
                S.dma('sp', wmask[:], I['wmask'].rearrange("g p c -> p g c"), (), ('wmask',))
                S.dma('sp', namask[:], I['namask'].rearrange("g p c -> p g c"), (), ('namask',))
                yT0 = sb('yT0', [128, 4, T], BF, mx)
                yTbox = [None]
                class _YT:
                    def __getitem__(self, idx):
                        p, m, sl = idx
                        if isinstance(m, slice):
                            if m.start >= 4: return yTbox[0][p, m.start - 4:m.stop - 4, sl]
                            return yT0[p, m, sl]
                        if m >= 4: return yTbox[0][p, m - 4, sl]
                        return yT0[p, m, sl]
                yT = _YT()
                with ExitStack() as ph:
                    uT = sb('uT', [128, 4, T], BF, ph)
                    acc = sb('acc', [128, 4, T], F32, ph)
                    dsk = sb('dsk', [128, 4], F32, ph)
                    wa_scope = ExitStack()
                    wa = sb('wa', [128, 8, 512], BF, wa_scope)
                    S.dma('sp', dsk[:], I['dskip'][li], (), ('dsk',))
                    load_w(wa, 'wa', win, 0, 512)
                    for m in range(4):
                        def cons(p_, pk, c0, n, m=m):
                            import os
                            dbgv = int(os.environ.get('S5DBG', '0'))
                            if dbgv in (0, 2): cp('act', uT[:, m, c0:c0 + n], p_, (pk,), ('uT%d' % m,))
                            if dbgv in (0, 3): ts('dve', acc[:, m, c0:c0 + n], uT[:, m, c0:c0 + n], dsk[:, m:m + 1], None, ALU.mult, None, ('uT%d' % m, 'dsk'), ('acc%d' % m,))
                        proj_fm(wa, 'wa', m * 128, cons)
                    S.barrier(); wa_scope.close()
                    if stop_after == 's5a': break
                    P = {k_: sb('sp_' + k_, [128, 32], F32, ph) for k_ in ['lr', 'li', 'dt', 'mag', 'c', 's', 't1', 't2', 'ar', 'ai', 'fr', 'fi', 'den']}
                    S.dma('sp', P['lr'][:], I['lamre'][li], (), ('p_lr',)); S.dma('sp', P['li'][:], I['lamim'][li], (), ('p_li',))
                    S.dma('sp', P['dt'][:], I['logdt'][li], (), ('p_dt',))
                    K_ = ('sp',)
                    act(P['dt'][:], P['dt'][:], AF.Exp, ('p_dt',), ('p_dt',))
                    tt('dve', P['t1'][:], P['lr'][:], P['dt'][:], ALU.mult, ('p_lr', 'p_dt'), K_)
                    act(P['mag'][:], P['t1'][:], AF.Exp, K_, K_)
                    tt('dve', P['t2'][:], P['li'][:], P['dt'][:], ALU.mult, ('p_li', 'p_dt'), K_)
                    act(P['s'][:], P['t2'][:], AF.Sin, K_, K_, scale=1.0 / 16)
                    act(P['c'][:], P['t2'][:], AF.Sin, K_ + ('halfpi',), K_, scale=1.0 / 16, bias=halfpi[:])
                    for _ in range(4):
                        tt('dve', P['t1'][:], P['c'][:], P['c'][:], ALU.mult, K_, K_)
                        tt('dve', P['t2'][:], P['s'][:], P['s'][:], ALU.mult, K_, K_)
                        tt('dve', P['s'][:], P['s'][:], P['c'][:], ALU.mult, K_, K_)
                        ts('dve', P['s'][:], P['s'][:], 2.0, None, ALU.mult, None, K_, K_)
                        tt('dve', P['c'][:], P['t1'][:], P['t2'][:], ALU.subtract, K_, K_)
                    tt('dve', P['ar'][:], P['mag'][:], P['c'][:], ALU.mult, K_, K_)
                    tt('dve', P['ai'][:], P['mag'][:], P['s'][:], ALU.mult, K_, K_)
                    tt('dve', P['den'][:], P['lr'][:], P['lr'][:], ALU.mult, K_, K_)
                    tt('dve', P['t1'][:], P['li'][:], P['li'][:], ALU.mult, K_, K_)
                    tt('dve', P['den'][:], P['den'][:], P['t1'][:], ALU.add, K_, K_)
                    rec(P['den'][:], P['den'][:], K_, K_)
                    ts('dve', P['t1'][:], P['ar'][:], -1.0, None, ALU.add, None, K_, K_)
                    tt('dve', P['fr'][:], P['t1'][:], P['lr'][:], ALU.mult, K_, K_)
                    tt('dve', P['t2'][:], P['ai'][:], P['li'][:], ALU.mult, K_, K_)
                    tt('dve', P['fr'][:], P['fr'][:], P['t2'][:], ALU.add, K_, K_)
                    tt('dve', P['fr'][:], P['fr'][:], P['den'][:], ALU.mult, K_, K_)
                    tt('dve', P['fi'][:], P['ai'][:], P['lr'][:], ALU.mult, K_, K_)
                    tt('dve', P['t2'][:], P['t1'][:], P['li'][:], ALU.mult, K_, K_)
                    tt('dve', P['fi'][:], P['fi'][:], P['t2'][:], ALU.subtract, K_, K_)
                    tt('dve', P['fi'][:], P['fi'][:], P['den'][:], ALU.mult, K_, K_)
                    pwr = sb('pwr', [128, 12, 32], F32, ph); pwi = sb('pwi', [128, 12, 32], F32, ph); npwi = sb('npwi', [128, 12, 32], F32, ph)
                    cp('dve', pwr[:, 0, :], P['ar'][:], K_, K_); cp('dve', pwi[:, 0, :], P['ai'][:], K_, K_)
                    for i in range(1, 12):
                        tt('dve', P['t1'][:], pwr[:, i - 1, :], pwr[:, i - 1, :], ALU.mult, K_, K_)
                        tt('dve', P['t2'][:], pwi[:, i - 1, :], pwi[:, i - 1, :], ALU.mult, K_, K_)
                        tt('dve', pwr[:, i, :], P['t1'][:], P['t2'][:], ALU.subtract, K_, K_)
                        tt('dve', P['t1'][:], pwr[:, i - 1, :], pwi[:, i - 1, :], ALU.mult, K_, K_)
                        ts('dve', pwi[:, i, :], P['t1'][:], 2.0, None, ALU.mult, None, K_, K_)
                    ts('dve', npwi[:], pwi[:], -1.0, None, ALU.mult, None, K_, K_)
                    apr = sb('apr', [128, 8, 32], F32, ph); api = sb('api', [128, 8, 32], F32, ph); napi = sb('napi', [128, 8, 32], F32, ph)
                    for p_i, src_i in [(1, 0), (2, 1), (4, 2), (8, 3)]:
                        cp('dve', apr[:, p_i - 1, :], pwr[:, src_i, :], K_, K_); cp('dve', api[:, p_i - 1, :], pwi[:, src_i, :], K_, K_)
                    def cmul(pd, pa, pb):
                        tt('dve', P['t1'][:], apr[:, pa - 1, :], apr[:, pb - 1, :], ALU.mult, K_, K_)
                        tt('dve', P['t2'][:], api[:, pa - 1, :], api[:, pb - 1, :], ALU.mult, K_, K_)
                        tt('dve', apr[:, pd - 1, :], P['t1'][:], P['t2'][:], ALU.subtract, K_, K_)
                        tt('dve', P['t1'][:], apr[:, pa - 1, :], api[:, pb - 1, :], ALU.mult, K_, K_)
                        tt('dve', P['t2'][:], api[:, pa - 1, :], apr[:, pb - 1, :], ALU.mult, K_, K_)
                        tt('dve', api[:, pd - 1, :], P['t1'][:], P['t2'][:], ALU.add, K_, K_)
                    cmul(3, 2, 1); cmul(5, 4, 1); cmul(6, 4, 2); cmul(7, 4, 3)
                    ts('dve', napi[:], api[:], -1.0, None, ALU.mult, None, K_, K_)
                    br = sb('br', [128, 32, 16], F32, ph); bi = sb('bi', [128, 32, 16], F32, ph)
                    bbr = sb('bbr', [128, 32, 16], F32, ph); bbi = sb('bbi', [128, 32, 16], F32, ph); tb = sb('tb', [128, 32, 16], F32, ph)
                    S.dma('sp', br[:], I['bre'][li], (), K_); S.dma('sp', bi[:], I['bim'][li], (), K_)
                    frb = bl(P['fr'][:], 16); fib = bl(P['fi'][:], 16)
                    tt('dve', bbr[:], br[:], frb, ALU.mult, K_, K_); tt('dve', tb[:], bi[:], fib, ALU.mult, K_, K_)
                    tt('dve', bbr[:], bbr[:], tb[:], ALU.subtract, K_, K_)
                    tt('dve', bbi[:], bi[:], frb, ALU.mult, K_, K_); tt('dve', tb[:], br[:], fib, ALU.mult, K_, K_)
                    tt('dve', bbi[:], bbi[:], tb[:], ALU.add, K_, K_)
                    cn_r = sb('cn_r', [128, 8, 64], F32, ph); cn_i = sb('cn_i', [128, 8, 64], F32, ph)
                    S.dma('sp', cn_r[:], I['cre'][li], (), K_); S.dma('sp', cn_i[:], I['cim'][li], (), K_)
                    ts('dve', cn_i[:], cn_i[:], -1.0, None, ALU.mult, None, K_, K_)
                    Zt = [sb('Zt%d' % i, [128, 128], F32, ph) for i in range(2)]
                    BTr = [sb('BTr%d' % i, [128, 128], BF, ph) for i in range(2)]; BTi = [sb('BTi%d' % i, [128, 128], BF, ph) for i in range(2)]
                    CTr = [sb('CTr%d' % i, [128, 128], F32, ph) for i in range(2)]; CTi = [sb('CTi%d' % i, [128, 128], F32, ph) for i in range(2)]
                    KAr = [sb('kar%d' % i, [128, T], F32, ph) for i in range(2)]; KAi = [sb('kai%d' % i, [128, T], F32, ph) for i in range(2)]
                    KBs = [[sb('kb%d_%d' % (i, j), [128, 288], F32, ph) for j in range(4)] for i in range(2)]
                    S.barrier()
                    if stop_after == 's5b': break
                    NCH = T // 8

                    def gparams(idx):
                        d_ = idx // 16; gp = idx % 16
                        return d_, gp, d_ * 16 + gp, gp // 4, gp % 4, idx % 2

                    def pos(d_, c0):
                        if d_ == 0: return c0
                        return (c0 - C_CTX) if c0 >= C_CTX else L_LAT

                    def stageA(idx):
                        d_, gp, col, tile_, gl, b = gparams(idx)
                        zk = 'Zt%d' % b
                        for src_, dst_, dk in [(bbr, BTr[b], 'BTr%d' % b), (bbi, BTi[b], 'BTi%d' % b)]:
                            tt('dve', Zt[b][:].rearrange("p (a q) -> p a q", q=16), bmid(src_[:, col, :], 8),
                               maskZ[:, gl, :].rearrange("p (a q) -> p a q", q=16), ALU.mult, K_ + ('maskZ',), (zk,))
                            p_, pk = PS()
                            tr(p_[:, 0:128], Zt[b][:], ident_f[:], (zk, 'ident_f'), (pk,))
                            cp('act', dst_[:], p_[:, 0:128], (pk,), (dk,))
                        for src_, dst_, dk in [(cn_r, CTr[b], 'CTr%d' % b), (cn_i, CTi[b], 'CTi%d' % b)]:
                            tt('dve', Zt[b][:].rearrange("p (a n) -> p a n", n=64), bmid(src_[:, d_ * 4 + tile_, :], 2),
                               maskY[:, gl, :].rearrange("p (a n) -> p a n", n=64), ALU.mult, K_ + ('maskY',), (zk,))
                            p_, pk = PS()
                            tr(p_[:, 0:128], Zt[b][:], ident_f[:], (zk, 'ident_f'), (pk,))
                            cp('act', dst_[:], p_[:, 0:128], (pk,), (dk,))
                        for (c0, n) in CH:
                            for lh, dst_, lk, dk in [(BTr[b], KAr[b], 'BTr%d' % b, 'kar%d' % b), (BTi[b], KAi[b], 'BTi%d' % b, 'kai%d' % b)]:
                                p_, pk = PS()
                                mm(p_[:, 0:n], lh[:], uT[:, tile_, c0:c0 + n], True, True, (lk, 'uT%d' % tile_), (pk,))
                                j0 = pos(d_, c0) // 8
                                cp('act', dst_[:].rearrange("p (s j) -> p j s", j=NCH)[:, j0:j0 + n // 8, :], p_[:, 0:n].rearrange("p (j s) -> p j s", s=8), (pk,), (dk,))

                    def stageB(idx):
                        d_, gp, col, tile_, gl, b = gparams(idx)
                        kr, ki = 'kar%d' % b, 'kai%d' % b
                        KB = KBs[b]
                        Rv = KAr[b][:].rearrange("p (s j) -> p j s", j=NCH); Iv = KAi[b][:].rearrange("p (s j) -> p j s", j=NCH)
                        c1r = apr[:, 0, col:col + 1]; c1i = api[:, 0, col:col + 1]; nc1i = napi[:, 0, col:col + 1]
                        order = range(1, 8) if d_ == 0 else range(6, -1, -1)
                        for s_ in order:
                            q_ = s_ - 1 if d_ == 0 else s_ + 1
                            stt(Rv[:, :, s_], Rv[:, :, q_], c1r, Rv[:, :, s_], ALU.mult, ALU.add, (kr,) + K_, (kr,), fast=True)
                            stt(Rv[:, :, s_], Iv[:, :, q_], nc1i, Rv[:, :, s_], ALU.mult, ALU.add, (kr, ki) + K_, (kr,), fast=True)
                            stt(Iv[:, :, s_], Iv[:, :, q_], c1r, Iv[:, :, s_], ALU.mult, ALU.add, (ki,) + K_, (ki,), fast=True)
                            stt(Iv[:, :, s_], Rv[:, :, q_], c1i, Iv[:, :, s_], ALU.mult, ALU.add, (kr, ki) + K_, (ki,), fast=True)
                        es = 7 if d_ == 0 else 0
                        kbn = ['kb%d_%d' % (b, j) for j in range(4)]
                        cp('act', KB[0][:], Rv[:, :, es], (kr,), (kbn[0],))
                        cp('act', KB[1][:], Iv[:, :, es], (ki,), (kbn[1],))
                        cur = 0
                        for i in range(9):
                            k = 1 << i
                            sr, si = KB[cur], KB[cur + 1]; dr_, di_ = KB[2 - cur], KB[3 - cur]
                            skr, ski = kbn[cur], kbn[cur + 1]; dkr, dki = kbn[2 - cur], kbn[3 - cur]
                            cr = pwr[:, 3 + i, col:col + 1]; ci = pwi[:, 3 + i, col:col + 1]; nci = npwi[:, 3 + i, col:col + 1]
                            if d_ == 0: dst_sl = slice(k, NCH); src_sl = slice(0, NCH - k); keep = slice(0, k)
                            else: dst_sl = slice(0, NCH - k); src_sl = slice(k, NCH); keep = slice(NCH - k, NCH)
                            cp('act', dr_[:, keep], sr[:, keep], (skr, skr + 'k'), (dkr + 'k',))
                            cp('act', di_[:, keep], si[:, keep], (ski, ski + 'k'), (dki + 'k',))
                            stt(dr_[:, dst_sl], sr[:, src_sl], cr, sr[:, dst_sl], ALU.mult, ALU.add, (skr, skr + 'k') + K_, (dkr,), fast=True)
                            stt(dr_[:, dst_sl], si[:, src_sl], nci, dr_[:, dst_sl], ALU.mult, ALU.add, (ski, ski + 'k', dkr) + K_, (dkr,), fast=True)
                            stt(di_[:, dst_sl], si[:, src_sl], cr, si[:, dst_sl], ALU.mult, ALU.add, (ski, ski + 'k') + K_, (dki,), fast=True)
                            stt(di_[:, dst_sl], sr[:, src_sl], ci, di_[:, dst_sl], ALU.mult, ALU.add, (skr, skr + 'k', dki) + K_, (dki,), fast=True)
                            cur = 2 - cur
                        Hr, Hi = KB[cur], KB[cur + 1]; hkr_, hki_ = kbn[cur], kbn[cur + 1]
                        for s_ in range(8):
                            pw_ = (s_ + 1) if d_ == 0 else (8 - s_)
                            fr_ = apr[:, pw_ - 1, col:col + 1]; fi_ = api[:, pw_ - 1, col:col + 1]; nfi_ = napi[:, pw_ - 1, col:col + 1]
                            if d_ == 0: osl = slice(1, NCH); hsl = slice(0, NCH - 1)
                            else: osl = slice(0, NCH - 1); hsl = slice(1, NCH)
                            hdeps = (hkr_, hki_, hkr_ + 'k', hki_ + 'k') + K_
                            stt(Rv[:, osl, s_], Hr[:, hsl], fr_, Rv[:, osl, s_], ALU.mult, ALU.add, (kr,) + hdeps, (kr,), fast=True)
                            stt(Rv[:, osl, s_], Hi[:, hsl], nfi_, Rv[:, osl, s_], ALU.mult, ALU.add, (kr,) + hdeps, (kr,), fast=True)
                            stt(Iv[:, osl, s_], Hi[:, hsl], fr_, Iv[:, osl, s_], ALU.mult, ALU.add, (ki,) + hdeps, (ki,), fast=True)
                            stt(Iv[:, osl, s_], Hr[:, hsl], fi_, Iv[:, osl, s_], ALU.mult, ALU.add, (ki,) + hdeps, (ki,), fast=True)

                    def stageC(idx):
                        d_, gp, col, tile_, gl, b = gparams(idx)
                        for (c0, n) in CH:
                            p_, pk = PS()
                            j0 = pos(d_, c0) // 8
                            rr = KAr[b][:].rearrange("p (s j) -> p j s", j=NCH)[:, j0:j0 + n // 8, :]
                            ri = KAi[b][:].rearrange("p (s j) -> p j s", j=NCH)[:, j0:j0 + n // 8, :]
                            mm(p_[:, 0:n], CTr[b][:], rr, True, False, ('CTr%d' % b, 'kar%d' % b), (pk,))
                            mm(p_[:, 0:n], CTi[b][:], ri, False, True, ('CTi%d' % b, 'kai%d' % b), (pk,))
                            tt('dve', acc[:, tile_, c0:c0 + n], acc[:, tile_, c0:c0 + n], p_[:, 0:n], ALU.add, (pk, 'acc%d' % tile_), ('acc%d' % tile_,))

                    NG = 32 if stop_after != 's5c' else 2
                    stageA(0)
                    for idx in range(NG):
                        if idx + 1 < NG: stageA(idx + 1)
                        stageB(idx)
                        stageC(idx)
                        S.flush()
                    if dbg and 'acc' in dbg_out and li == 0:
                        S.dma('sp', dbg_out['acc'].rearrange("(m p) t -> p (m t)", p=128), acc[:].rearrange("p m t -> p (m t)"), ['acc%d' % m for m in range(4)], ())
                    wg = sb('wg', [128, 4, 512], BF, ph)
                    S.dma('pool', wg[:], I['ssm_w_glu'][li].rearrange("(kc p) n -> p kc n", p=128), (), ('wg',))
                    t3 = KAr[0]; gT = uT
                    for m in range(4):
                        a_ = acc[:, m, :]
                        act(t3[:], a_, AF.Square, ('acc%d' % m,), ('kar0',))
                        ts('dve', t3[:], t3[:], 0.044715, 1.0, ALU.mult, ALU.add, ('kar0',), ('kar0',))
                        tt('dve', t3[:], t3[:], a_, ALU.mult, ('kar0', 'acc%d' % m), ('kar0',))
                        act(t3[:], t3[:], AF.Sigmoid, ('kar0',), ('kar0',), scale=1.5957691216057308)
                        tt('dve', gT[:, m, :], t3[:], a_, ALU.mult, ('kar0', 'acc%d' % m), ('uT%d' % m,))
                    for m in range(4):
                        def cons(p_, pk, c0, n, m=m):
                            act(KAi[0][:, c0:c0 + n], p_, AF.Sigmoid, (pk,), ('kai0',))
                            tt('dve', yT[:, m, c0:c0 + n], gT[:, m, c0:c0 + n], KAi[0][:, c0:c0 + n], ALU.mult, ('kai0', 'uT%d' % m), ('yT%d' % m,))
                        proj_fm(wg, 'wg', m * 128, cons, src=gT, nk=4, srckey=('uT0', 'uT1', 'uT2', 'uT3'))
                    S.barrier()

                if stop_after == 's5': break
                yTbox[0] = sb('yT2', [128, 8, T], BF, mx)
                def qk_prep(dst, dkey, wt, wkey, wc0, gcol_ap, gkey, rope, tmp):
                    raw, sq, rs, qn, t1 = tmp
                    for (c0, n) in CH:
                        p_, pk = PS()
                        for kc in range(8):
                            mm(p_[:, 0:n], wt[:, kc, wc0:wc0 + 128], hT[:, kc, c0:c0 + n], kc == 0, kc == 7, (wkey, 'hT'), (pk,))
                        cp('act', raw[:, 0:n], p_[:, 0:n], (pk,), ('raw',))
                        act(sq[:, 0:n], p_[:, 0:n], AF.Square, (pk,), ('sq',))
                        p2, pk2 = PS()
                        mm(p2[:, 0:n], blockones[:], sq[:, 0:n], True, True, ('blockones', 'sq'), (pk2,))
                        act(rs[:, 0:n], p2[:, 0:n], AF.Sqrt, (pk2, 'epsT'), ('rs',), bias=epsT[:], scale=1.0 / 64)
                        rec(rs[:, 0:n], rs[:, 0:n], ('rs',), ('rs',))
                        if not (rope and c0 >= C_CTX):
                            stt(dst[:, c0:c0 + n], raw[:, 0:n], gcol_ap, rs[:, 0:n], ALU.mult, ALU.mult, ('raw', 'rs', gkey), (dkey,))
                        else:
                            l0 = c0 - C_CTX
                            stt(qn[:, 0:n], raw[:, 0:n], gcol_ap, rs[:, 0:n], ALU.mult, ALU.mult, ('raw', 'rs', gkey), ('qn',))
                            p3, pk3 = PS()
                            mm(p3[:, 0:n], rotT[:], qn[:, 0:n], True, True, ('rotT', 'qn'), (pk3,))
                            tt('dve', t1[:, 0:n], qn[:, 0:n], cosT[:, l0:l0 + n], ALU.mult, ('qn', 'cosT'), ('t1',))
                            tt('dve', raw[:, 0:n], p3[:, 0:n], sinT[:, l0:l0 + n], ALU.mult, (pk3, 'sinT'), ('raw',))
                            tt('dve', dst[:, c0:c0 + n], t1[:, 0:n], raw[:, 0:n], ALU.add, ('t1', 'raw'), (dkey,))

                def attention(kind, hg):
                    with ExitStack() as ph:
                        tmp = (sb('raw', [128, 512], F32, ph), sb('sq', [128, 512], BF, ph), sb('rs', [128, 512], F32, ph),
                               sb('qn', [128, 512], BF, ph), sb('t1', [128, 512], F32, ph))
                        gq = sb('gq', [128, 1], F32, ph); gk = sb('gk', [128, 1], F32, ph)
                        S.dma('sp', gq[:], I['gwq' if kind == 'w' else 'gnq'][li], (), ('gq',))
                        S.dma('sp', gk[:], I['gwk' if kind == 'w' else 'gnk'][li], (), ('gk',))
                        ts('dve', gq[:], gq[:], 0.125, None, ALU.mult, None, ('gq',), ('gq',))
                        wq_ = sb('wq_', [128, 8, 256], BF, ph)
                        qT = sb('qT', [128, 2, T], BF, ph)
                        nkt = 1 if kind == 'w' else 2
                        nv = 1 if kind == 'w' else 4
                        wk_ = sb('wk_', [128, 8, 128 * nkt], BF, ph); kT = sb('kT', [128, nkt, T], BF, ph)
                        wv_ = sb('wv_', [128, 8, 64 * nv], BF, ph); vaug = sb('vaug', [128, NT, nv, 65], BF, ph)
                        if kind == 'w':
                            load_w(wq_, 'wq_', win, 512 + 256 * hg, 256)
                            load_w(wk_, 'wk_', win, 1024 + 64 * hg, 64, 0); load_w(wk_, 'wk_', win, 1024 + 64 * hg, 64, 64)
                            load_w(wv_, 'wv_', win, 1152 + 64 * hg, 64)
                        else:
                            load_w(wq_, 'wq_', win, 1280 + 256 * hg, 256)
                            load_w(wk_, 'wk_', win, 1792 + 256 * hg, 256)
                            load_w(wv_, 'wv_', win, 2304 + 256 * hg, 256)
                        for m in range(2):
                            qk_prep(qT[:, m, :], 'qT', wq_, 'wq_', m * 128, gq[:, 0:1], 'gq', kind == 'w', tmp)
                        for m in range(nkt):
                            qk_prep(kT[:, m, :], 'kT', wk_, 'wk_', m * 128, gk[:, 0:1], 'gk', kind == 'w', tmp)
                        kTm = sb('kTm', [128, nkt, 2, T], BF, ph)
                        for m in range(nkt):
                            for par in range(2):
                                ts('dve', kTm[:, m, par, :], kT[:, m, :], hm[:, par:par + 1], None, ALU.mult, None, ('kT', 'hm'), ('kTm',))
                        ms('pool', vaug[:, :, :, 64:65], 1.0, ('vaug',))
                        for t_ in range(NT):
                            p_, pk = PS()
                            for kc in range(8):
                                mm(p_[:, 0:64 * nv], hT[:, kc, t_ * 128:(t_ + 1) * 128], wv_[:, kc, :], kc == 0, kc == 7, ('hT', 'wv_'), (pk,))
                            cp('act', vaug[:, t_, :, 0:64], p_[:, 0:64 * nv].rearrange("p (v d) -> p v d", d=64), (pk,), ('vaug',))
                        esink = sb('esink', [128, 8], F32, ph)
                        if kind == 'w':
                            S.dma('sp', esink[:], I['sink'][li], (), ('esink',))
                            act(esink[:], esink[:], AF.Exp, ('esink',), ('esink',))
                        else:
                            nab = sb('nab', [128, 7, 4, 128], BF, ph)
                            for di in range(7):
                                S.dma('pool', nab[:, di, :, :], I['nabias'][li, di, :, 4 * hg:4 * hg + 4, :], (), ('nab',))
                        import os
                        adbg = int(os.environ.get('ATTDBG', '0'))
                        if adbg == 1:
                            S.barrier(); return
                        NSLOT = 7
                        PTs = sb('PTs', [128, NSLOT, 4, 128], BF, ph)
                        ytok = sb('ytok', [128, 4, 64], BF, ph); tot = sb('tot', [128, 4], F32, ph)
                        S.flush()
                        ybase = (4 if kind == 'w' else 8) + 2 * hg
                        for tq in range(NT):
                            keys = []
                            if tq < 2: keys = [(0, None, None), (1, None, None)]
                            else:
                                i = tq - 2
                                keys = [(0, None, None), (1, None, None)]
                                if kind == 'w':
                                    for j in (i - 1, i, i + 1):
                                        if 0 <= j < 16:
                                            keys.append((2 + j, None if j == i else (wmask[:, 0, :] if j < i else wmask[:, 1, :]), None))
                                else:
                                    for j in na_tiles(i):
                                        keys.append((2 + j, namask[:, nidx[(i, j)], :], nab[:, j - i + 3, :, :]))
                            assert len(keys) <= NSLOT
                            for sl, (tk, mask, bias) in enumerate(keys):
                                p_, pk = PS()
                                if bias is not None:
                                    mm(p_[:, 0:512], ident_b[:], bias.rearrange("p h q -> p (h q)"), True, False, ('ident_b', 'nab'), (pk,))
                                for h in range(4):
                                    if adbg == 4 and h % 2 == 1: continue
                                    half = (h % 2) * 64
                                    kt = kTm[:, 0 if kind == 'w' else h // 2, h % 2, tk * 128:(tk + 1) * 128]
                                    mm(p_[:, h * 128:(h + 1) * 128], kt, qT[:, h // 2, tq * 128:(tq + 1) * 128],
                                       bias is None, h == 3 or bias is None, ('kTm', 'qT'), (pk,))
                                act(PTs[:, sl, :, :].rearrange("p h q -> p (h q)"), p_[:, 0:512], AF.Exp, (pk,), ('PT%d' % sl,))
                                if mask is not None and adbg not in (3, 4):
                                    tt('dve', PTs[:, sl, :, :], PTs[:, sl, :, :], bmid(mask, 4), ALU.mult, ('PT%d' % sl, 'wmask', 'namask'), ('PT%d' % sl,))
                            if adbg in (2, 3, 4):
                                continue
                            po, pok = PS()
                            for h in range(4):
                                for sl, (tk, mask, bias) in enumerate(keys):
                                    mm(po[:, h * 65:(h + 1) * 65], PTs[:, sl, h, :], vaug[:, tk, 0 if kind == 'w' else h, :],
                                       sl == 0, sl == len(keys) - 1, ('PT%d' % sl, 'vaug'), (pok,))
                            pov = po[:, 0:260].rearrange("p (h e) -> p h e", e=65)
                            if kind == 'w':
                                tt('dve', tot[:], pov[:, :, 64], esink[:, 4 * hg:4 * hg + 4], ALU.add, (pok, 'esink'), ('tot',))
                            else:
                                cp('dve', tot[:], pov[:, :, 64], (pok,), ('tot',))
                            rec(tot[:], tot[:], ('tot',), ('tot',))
                            tt('dve', ytok[:], pov[:, :, 0:64], bl(tot[:], 64), ALU.mult, (pok, 'tot'), ('ytok',))
                            pt_, ptk = PT_()
                            for m in range(2):
                                tr(pt_[:, m * 128:(m + 1) * 128], ytok[:, 2 * m:2 * m + 2, :].rearrange("p h d -> p (h d)"), ident_b[:], ('ytok', 'ident_b'), (ptk,))
                            cp('act', yT[:, ybase:ybase + 2, tq * 128:(tq + 1) * 128], pt_[:, 0:256].rearrange("p (m t) -> p m t", t=128), (ptk,), ('yT%d' % ybase, 'yT%d' % (ybase + 1)))
                            if tq % 6 == 5: S.flush()
                        S.barrier()

                for kind in ('w', 'n'):
                    for hg in range(2):
                        attention(kind, hg)
                if dbg and 'yT' in dbg_out and li == 0:
                    with ExitStack() as ph:
                        yf = sb('yf', [128, 12, T], F32, ph) if False else None

                if stop_after == 'attn': break
                with ExitStack() as ph:
                    modb = sb('modb', [128, 2, D], F32, ph)
                    mod_bcast(2, modb)
                    wbr = sb('wbr', [128, 12, D], BF, ph)
                    S.dma('pool', wbr[:], I['w_branch'][li].rearrange("i (kc p) d -> p (i kc) d", p=128), (), ('wbr',))
                    wo = sb('wo', [128, 8, D], BF, ph)
                    S.dma('pool', wo[:], I['w_out'][li].rearrange("(kc p) d -> p kc d", p=128), (), ('wo',))
                    wgt = [sb('wgt%d' % i, [128, 8, 384], BF, ph) for i in range(2)]
                    sT = sb('sT', [128, 8, 512], BF, ph)
                    sg = [sb('sg%d' % i, [128, 512], F32, ph) for i in range(3)]
                    xr = [sb('xr%d' % i, [128, D], F32, ph) for i in range(2)]
                    tmpm = sb('tmpm', [128, 512], F32, ph)
                    it = 0
                    for (c0, n) in CH:
                        for dt_ in range(8):
                            w = wgt[it % 2]; wk = 'wgt%d' % (it % 2); it += 1
                            for i in range(3):
                                load_w(w, wk + '_%d' % i, win, 2816 + i * 1024 + dt_ * 128, 128, i * 128)
                            for i in range(3):
                                pg, pgk = PS()
                                for kc in range(8):
                                    mm(pg[:, 0:n], w[:, kc, i * 128:(i + 1) * 128], hT[:, kc, c0:c0 + n], kc == 0, kc == 7, (wk + '_%d' % i, 'hT'), (pgk,))
                                act(sg[i][:, 0:n], pg[:, 0:n], AF.Sigmoid, (pgk,), ('sg%d' % i,))
                                pp, ppk = PS()
                                for kc in range(4):
                                    mm(pp[:, 0:n], wbr[:, i * 4 + kc, dt_ * 128:(dt_ + 1) * 128], yT[:, i * 4 + kc, c0:c0 + n], kc == 0, kc == 3,
                                       ('wbr',) + tuple('yT%d' % q for q in range(12)), (ppk,))
                                tt('dve', sg[i][:, 0:n], sg[i][:, 0:n], pp[:, 0:n], ALU.mult, (ppk, 'sg%d' % i), ('sg%d' % i,))
                            tt('dve', sg[0][:, 0:n], sg[0][:, 0:n], sg[1][:, 0:n], ALU.add, ('sg0', 'sg1'), ('sg0',))
                            tt('dve', sT[:, dt_, 0:n], sg[0][:, 0:n], sg[2][:, 0:n], ALU.add, ('sg0', 'sg2'), ('sT',))
                        for tl_ in range(n // 128):
                            tt_ = c0 // 128 + tl_; b_ = tt_ % 2; v = 1 if tt_ < 2 else 0
                            S.dma('sp', xr[b_][:], xd[tt_ * 128:(tt_ + 1) * 128, :], ('xd%d' % tt_,), ('xr%d' % b_,))
                            for hf in range(2):
                                p_, pk = PS()
                                for kc in range(8):
                                    mm(p_[:, 0:512], sT[:, kc, tl_ * 128:(tl_ + 1) * 128], wo[:, kc, hf * 512:(hf + 1) * 512], kc == 0, kc == 7, ('sT', 'wo'), (pk,))
                                tt('dve', tmpm[:], p_[:, 0:512], modb[:, v, hf * 512:(hf + 1) * 512], ALU.mult, (pk, 'modb'), ('tmpm',))
                                tt('dve', xr[b_][:, hf * 512:(hf + 1) * 512], xr[b_][:, hf * 512:(hf + 1) * 512], tmpm[:], ALU.add, ('tmpm', 'xr%d' % b_), ('xr%d' % b_,))
                            S.dma('sp', xd[tt_ * 128:(tt_ + 1) * 128, :], xr[b_][:], ('xr%d' % b_,), ('xd%d' % tt_,))
                    S.barrier()

            if stop_after == 'merge': break
            mod_scale_shift('gffn', 4, 3)
            norm_stage()
            with ExitStack() as ph:
                macc = sb('macc', [128, NT, D], F32, ph)
                G = sb('G', [128, NT, NE], F32, ph)
                with ExitStack() as ph1:
                    rw = sb('rw', [128, 8, NE], BF, ph1); rb = sb('rb', [128, NE], F32, ph1)
                    S.dma('pool', rw[:], I['router_w'][li].rearrange("(kc p) e -> p kc e", p=128), (), ('rw',))
                    S.dma('sp', rb[:], I['rbias'][li], (), ('rb',))
                    lg = sb('lg', [128, NE], F32, ph1); m8 = sb('m8', [128, 8], F32, ph1); mk = sb('mk', [128, NE], F32, ph1)
                    nmx = sb('nmx', [128, 1], F32, ph1); sm = sb('sm', [128, 1], F32, ph1)
                    ms('pool', macc[:], 0.0, tuple('macc%d' % t for t in range(NT)))
                    for t_ in range(NT):
                        p_, pk = PS()
                        for kc in range(8):
                            mm(p_[:, 0:NE], hT[:, kc, t_ * 128:(t_ + 1) * 128], rw[:, kc, :], kc == 0, kc == 7, ('hT', 'rw'), (pk,))
                        tt('dve', lg[:], p_[:, 0:NE], rb[:], ALU.add, (pk, 'rb'), ('lg',))
                        S.add('dve', lambda e: e.max(out=m8[:], in_=lg[:]), ('lg',), ('m8',))
                        ts('dve', mk[:], lg[:], m8[:, 3:4], None, ALU.is_ge, None, ('lg', 'm8'), ('mk',))
                        ts('dve', nmx[:], m8[:, 0:1], -1.0, None, ALU.mult, None, ('m8',), ('nmx',))
                        act(lg[:], lg[:], AF.Exp, ('lg', 'nmx'), ('lg',), bias=nmx[:])
                        tt('dve', lg[:], lg[:], mk[:], ALU.mult, ('lg', 'mk'), ('lg',))
                        S.add('dve', lambda e: e.reduce_sum(out=sm[:], in_=lg[:], axis=mybir.AxisListType.X), ('lg',), ('sm',))
                        rec(sm[:], sm[:], ('sm',), ('sm',))
                        ts('dve', G[:, t_, :], lg[:], sm[:, 0:1], None, ALU.mult, None, ('lg', 'sm'), ('G',))
                    S.barrier()
                with ExitStack() as ph2:
                    wgu = [sb('wgu%d' % i, [128, 8, 2 * D], BF, ph2) for i in range(2)]
                    wdn = sb('wdn', [128, 8, D], BF, ph2)
                    bgu = sb('bgu', [128, NE, 16], F32, ph2)
                    S.dma('sp', bgu[:], I['bguT'][li], (), ('bgu',))
                    ts('dve', bgu[:, :, 8:16], bgu[:, :, 8:16], 1.0, None, ALU.add, None, ('bgu',), ('bgu',))
                    actT = [sb('actT%d' % i, [128, 8, 512], BF, ph2) for i in range(2)]
                    g1 = [sb('g1%d' % i, [128, 512], F32, ph2) for i in range(2)]; sgm = sb('sgm', [128, 512], BF, ph2); u1 = sb('u1', [128, 512], F32, ph2)
                    CHM = [(256 + 512 * j, 512) for j in range(4)] + ([(0, 256)] if li < DEPTH - 1 or nlayers < DEPTH else [])
                    NCM = len(CHM)

                    def load_gu(e_):
                        wgv = I['w_gate_up'][li, e_].rearrange("(kc p) n -> p kc n", p=128)
                        for kc in range(8):
                            S.dma('pool', wgu[e_ % 2][:, kc, :], wgv[:, kc, :], (), ('wgu%d_%d' % (e_ % 2, kc),))

                    def load_dn(e_):
                        S.dma('pool', wdn[:], I['w_down'][li, e_].rearrange("(kc p) n -> p kc n", p=128), (), ('wdn',))

                    def GU(e_, ci):
                        c0, n = CHM[ci]; a_ = actT[ci % 2]; ak = 'actT%d' % (ci % 2)
                        w = wgu[e_ % 2]; wk = 'wgu%d' % (e_ % 2)
                        for m in range(8):
                            pg, pgk = PS()
                            for kc in range(8):
                                mm(pg[:, 0:n], w[:, kc, m * 128:(m + 1) * 128], hT[:, kc, c0:c0 + n], kc == 0, kc == 7, (wk + '_%d' % kc, 'hT'), (pgk,))
                            pu, puk = PS()
                            for kc in range(8):
                                mm(pu[:, 0:n], w[:, kc, D + m * 128:D + (m + 1) * 128], hT[:, kc, c0:c0 + n], kc == 0, kc == 7, (wk + '_%d' % kc, 'hT'), (puk,))
                            b_ = m % 2
                            ts('dve', g1[b_][:, 0:n], pg[:, 0:n], bgu[:, e_, m:m + 1], 7.0, ALU.add, ALU.min, (pgk, 'bgu'), ('g1%d' % b_,))
                            act(sgm[:, 0:n], g1[b_][:, 0:n], AF.Sigmoid, ('g1%d' % b_,), ('sgm',), scale=1.702)
                            if m > 0:
                                q_ = (m - 1) % 2
                                stt(a_[:, m - 1, 0:n], u1[:, 0:n], -6.0, g1[q_][:, 0:n], ALU.max, ALU.mult, ('u1', 'g1%d' % q_), (ak,))
                            ts('dve', u1[:, 0:n], pu[:, 0:n], bgu[:, e_, 8 + m:9 + m], 8.0, ALU.add, ALU.min, (puk, 'bgu'), ('u1',))
                            tt('pool', g1[b_][:, 0:n], g1[b_][:, 0:n], sgm[:, 0:n], ALU.mult, ('g1%d' % b_, 'sgm'), ('g1%d' % b_,))
                        stt(a_[:, 7, 0:n], u1[:, 0:n], -6.0, g1[1][:, 0:n], ALU.max, ALU.mult, ('u1', 'g11'), (ak,))

                    def DN(e_, ci):
                        c0, n = CHM[ci]; a_ = actT[ci % 2]; ak = 'actT%d' % (ci % 2)
                        for tl_ in range(n // 128):
                            tt_ = c0 // 128 + tl_
                            for hf in range(2):
                                p_, pk = PS()
                                for kc in range(8):
                                    mm(p_[:, 0:512], a_[:, kc, tl_ * 128:(tl_ + 1) * 128], wdn[:, kc, hf * 512:(hf + 1) * 512], kc == 0, kc == 7, (ak, 'wdn'), (pk,))
                                sl_ = macc[:, tt_, hf * 512:(hf + 1) * 512]
                                stt(sl_, p_[:, 0:512], G[:, tt_, e_:e_ + 1], sl_, ALU.mult, ALU.add, (pk, 'G', 'macc%d' % tt_), ('macc%d' % tt_,))

                    load_gu(0); load_dn(0)
                    for e_ in range(NE):
                        if e_ + 1 < NE: load_gu(e_ + 1)
                        for ci in range(NCM):
                            GU(e_, ci)
                            if ci > 0: DN(e_, ci - 1)
                        DN(e_, NCM - 1)
                        if e_ + 1 < NE: load_dn(e_ + 1)
                        S.flush()
                    S.barrier()
                with ExitStack() as ph3:
                    modb = sb('modb5', [128, 2, D], F32, ph3)
                    mod_bcast(5, modb)
                    xr = [sb('xq%d' % i, [128, D], F32, ph3) for i in range(2)]
                    bdn = sb('bdn', [32, D], F32, ph3); GT = sb('GT', [32, 128], F32, ph3)
                    S.dma('sp', bdn[:], I['b_down'][li], (), ('bdn',))
                    for t_ in range(NT):
                        b_ = t_ % 2; v = 1 if t_ < 2 else 0
                        pg_, pgk_ = PS()
                        tr(pg_[0:32, 0:128], G[:, t_, :], ident_f[:], ('G', 'ident_f'), (pgk_,))
                        cp('act', GT[:], pg_[0:32, 0:128], (pgk_,), ('GT',))
                        for hf in range(2):
                            pb_, pbk_ = PS()
                            mm(pb_[:, 0:512], GT[:], bdn[:, hf * 512:(hf + 1) * 512], True, True, ('GT', 'bdn'), (pbk_,))
                            tt('dve', macc[:, t_, hf * 512:(hf + 1) * 512], macc[:, t_, hf * 512:(hf + 1) * 512], pb_[:, 0:512], ALU.add, (pbk_, 'macc%d' % t_), ('macc%d' % t_,))
                        S.dma('sp', xr[b_][:], xd[t_ * 128:(t_ + 1) * 128, :], ('xd%d' % t_,), ('xq%d' % b_,))
                        tt('dve', macc[:, t_, :], macc[:, t_, :], modb[:, v, :], ALU.mult, ('macc%d' % t_, 'modb'), ('macc%d' % t_,))
                        tt('pool', xr[b_][:], xr[b_][:], macc[:, t_, :], ALU.add, ('xq%d' % b_, 'macc%d' % t_), ('xq%d' % b_,))
                        if li == nlayers - 1:
                            if t_ >= 2:
                                S.dma('sp', yout[(t_ - 2) * 128:(t_ - 1) * 128, :], xr[b_][:], ('xq%d' % b_,), ('yout%d' % t_,))
                        else:
                            S.dma('sp', xd[t_ * 128:(t_ + 1) * 128, :], xr[b_][:], ('xq%d' % b_,), ('xd%d' % t_,))
                    S.barrier()
        S.barrier()
    return nc, consts, list(I.keys())


def make_in_maps(inputs, cores, nc_consts, names=None):
    maps = []
    big = {k_: np.ascontiguousarray(inputs[k_], dtype=np.float32) for k_ in BIG if names is None or k_ in names}
    for b in cores:
        m = {'x': np.ascontiguousarray(inputs['x'][b]), 'ctx': np.ascontiguousarray(inputs['ctx'][b])}
        m.update(nc_consts)
        m.update(host_layout(inputs, b))
        m.update(big)
        if names is not None: m = {k_: v for k_, v in m.items() if k_ in names}
        maps.append(m)
    return maps


def kernel(**inputs):
    inputs = {k_: np.asarray(v) for k_, v in inputs.items()}
    nc, consts, names = build(DEPTH)
    maps = make_in_maps(inputs, list(range(8)), consts, names)
    res = run_bass_kernel_spmd(nc, maps, core_ids=list(range(8)))
    return np.stack([r['y'] for r in res.results], 0).astype(np.float32)
```
